# Optimizing a Trainium2 kernel written in Bass

```python
import jax, jax.numpy as jnp
from jax import lax
import numpy as np

D_MODEL = 1024
BATCH = 8
SEQ = 4096
DEPTH = 2

D_MIX = 2 * D_MODEL
ATTN_HEAD_DIM = 128
N_ATTN_HEADS = D_MODEL // ATTN_HEAD_DIM
D_ATTN = N_ATTN_HEADS * ATTN_HEAD_DIM
MOBA_BLOCK = 256
MOBA_TOPK = 3
Q_BLOCK = 128
SSD_HEAD_DIM = 64
D_SSD = D_MIX - D_ATTN
N_SSD_HEADS = D_SSD // SSD_HEAD_DIM
SSD_GROUPS = 2
SSD_STATE = 128
SSD_CONV = 4
SSD_CHUNK = 256
D_CONV_CH = D_SSD + 2 * SSD_GROUPS * SSD_STATE
D_IN_PROJ = 4 * D_ATTN + D_SSD + D_CONV_CH + N_SSD_HEADS
SEQ_ALIGN = 256
EPS = 1e-6

kernel_name = "hymba_moba_ssd_hybrid"


def rms_norm(x, w):
    xf = x.astype(jnp.float32)
    y = xf * lax.rsqrt(jnp.mean(xf * xf, axis=-1, keepdims=True) + EPS)
    return (y * w.astype(jnp.float32)).astype(x.dtype)


def alibi_slopes():
    h = jnp.arange(1, N_ATTN_HEADS + 1, dtype=jnp.float32)
    return jnp.exp2(-8.0 * h / N_ATTN_HEADS)


def moba_attention(q, k, v):
    b, s, h, dh = q.shape
    nb = s // MOBA_BLOCK
    nqc = s // Q_BLOCK
    n_sel = min(MOBA_TOPK, nb)
    scale = dh ** -0.5
    slopes = alibi_slopes()
    qh = q.transpose(0, 2, 1, 3)
    kblk = k.transpose(0, 2, 1, 3).reshape(b, h, nb, MOBA_BLOCK, dh)
    vblk = v.transpose(0, 2, 1, 3).reshape(b, h, nb, MOBA_BLOCK, dh)
    k_mean = jnp.mean(kblk.astype(jnp.float32), axis=3)
    gate = jnp.einsum('bhsd,bhnd->bhsn', qh.astype(jnp.float32), k_mean)
    q_blk = jnp.arange(s) // MOBA_BLOCK
    past = jnp.arange(nb)[None, :] < q_blk[:, None]
    gate = jnp.where(past, gate, -jnp.inf)
    _, sel = lax.top_k(gate, n_sel)
    q_c = qh.reshape(b, h, nqc, Q_BLOCK, dh).transpose(0, 2, 1, 3, 4).reshape(b * nqc, h, Q_BLOCK, dh)
    sel_c = sel.reshape(b, h, nqc, Q_BLOCK, n_sel).transpose(0, 2, 1, 3, 4).reshape(b * nqc, h, Q_BLOCK, n_sel)
    b_id = jnp.repeat(jnp.arange(b), nqc)
    c_id = jnp.tile(jnp.arange(nqc), b)
    head_ix = jnp.arange(h)[:, None, None]
    key_off = jnp.arange(MOBA_BLOCK)

    def one_block(args):
        qc, selc, bi, ci = args
        kb = lax.dynamic_index_in_dim(kblk, bi, 0, keepdims=False)
        vb = lax.dynamic_index_in_dim(vblk, bi, 0, keepdims=False)
        t = ci * Q_BLOCK + jnp.arange(Q_BLOCK)
        own = (ci * Q_BLOCK) // MOBA_BLOCK
        k_sel = kb[head_ix, selc]
        v_sel = vb[head_ix, selc]
        s_sel = jnp.einsum('hqd,hqkcd->hqkc', qc, k_sel, preferred_element_type=jnp.float32) * scale
        pos_sel = selc[..., None] * MOBA_BLOCK + key_off
        s_sel = s_sel - slopes[:, None, None, None] * (t[None, :, None, None] - pos_sel).astype(jnp.float32)
        valid = jnp.arange(n_sel)[None, :] < (t // MOBA_BLOCK)[:, None]
        s_sel = jnp.where(valid[None, :, :, None], s_sel, -jnp.inf)
        k_own = lax.dynamic_index_in_dim(kb, own, 1, keepdims=False)
        v_own = lax.dynamic_index_in_dim(vb, own, 1, keepdims=False)
        s_own = jnp.einsum('hqd,hcd->hqc', qc, k_own, preferred_element_type=jnp.float32) * scale
        dist = t[:, None] - (own * MOBA_BLOCK + key_off)[None, :]
        s_own = s_own - slopes[:, None, None] * dist.astype(jnp.float32)[None]
        s_own = jnp.where((dist >= 0)[None], s_own, -jnp.inf)
        scores = jnp.concatenate([s_sel.reshape(h, Q_BLOCK, n_sel * MOBA_BLOCK), s_own], axis=-1)
        p = jax.nn.softmax(scores, axis=-1).astype(v.dtype)
        p_sel = p[..., :n_sel * MOBA_BLOCK].reshape(h, Q_BLOCK, n_sel, MOBA_BLOCK)
        p_own = p[..., n_sel * MOBA_BLOCK:]
        return (jnp.einsum('hqkc,hqkcd->hqd', p_sel, v_sel)
                + jnp.einsum('hqc,hcd->hqd', p_own, v_own))

    out = lax.map(one_block, (q_c, sel_c, b_id, c_id))
    return out.reshape(b, nqc, h, Q_BLOCK, dh).transpose(0, 1, 3, 2, 4).reshape(b, s, h * dh)


def causal_depthwise_conv(u, w, bias):
    out = lax.conv_general_dilated(u, w[:, None, :], window_strides=(1,),
                                   padding=[(SSD_CONV - 1, 0)],
                                   dimension_numbers=('NWC', 'WIO', 'NWC'),
                                   feature_group_count=u.shape[-1])
    return out + bias


def ssd_scan(xs, bm, cm, dt, a_log, d_skip):
    b, s = xs.shape[:2]
    nc = s // SSD_CHUNK
    hg = N_SSD_HEADS // SSD_GROUPS
    a = -jnp.exp(a_log.astype(jnp.float32)).reshape(SSD_GROUPS, hg)
    x = xs.reshape(b, nc, SSD_CHUNK, SSD_GROUPS, hg, SSD_HEAD_DIM)
    bc = bm.reshape(b, nc, SSD_CHUNK, SSD_GROUPS, SSD_STATE)
    cc = cm.reshape(b, nc, SSD_CHUNK, SSD_GROUPS, SSD_STATE)
    dtc = dt.reshape(b, nc, SSD_CHUNK, SSD_GROUPS, hg)
    xdt = x * dtc[..., None]
    a_cs = jnp.cumsum((dtc * a).transpose(0, 1, 3, 4, 2), axis=-1)
    causal = jnp.tril(jnp.ones((SSD_CHUNK, SSD_CHUNK), dtype=bool))
    decay = jnp.exp(jnp.where(causal, a_cs[..., :, None] - a_cs[..., None, :], -jnp.inf))
    cb = jnp.einsum('bclgn,bcsgn->bcgls', cc, bc)
    y_diag = jnp.einsum('bcgls,bcghls,bcsghp->bclghp', cb, decay, xdt)
    decay_states = jnp.exp(a_cs[..., -1:] - a_cs)
    states = jnp.einsum('bclgn,bcghl,bclghp->bcghpn', bc, decay_states, xdt)
    chunk_decay = jnp.exp(a_cs[..., -1])

    def step(carry, inp):
        st, dec = inp
        return carry * dec[..., None, None] + st, carry

    init = jnp.zeros((b, SSD_GROUPS, hg, SSD_HEAD_DIM, SSD_STATE), jnp.float32)
    _, prev = lax.scan(step, init, (jnp.moveaxis(states, 1, 0), jnp.moveaxis(chunk_decay, 1, 0)))
    prev = jnp.moveaxis(prev, 0, 1)
    y_off = jnp.einsum('bclgn,bcghpn,bcghl->bclghp', cc, prev, jnp.exp(a_cs))
    y = y_diag + y_off + x * d_skip.astype(jnp.float32).reshape(SSD_GROUPS, hg)[:, :, None]
    return y.reshape(b, s, N_SSD_HEADS * SSD_HEAD_DIM)


def hybrid_layer(x, ln_w, w_in, q_norm_w, k_norm_w, conv_w, conv_b, dt_bias, a_log, d_skip, ssd_norm_w, w_out):
    b, s, _ = x.shape
    h = rms_norm(x, ln_w)
    proj = h @ w_in
    cuts = [D_ATTN, 2 * D_ATTN, 3 * D_ATTN, 4 * D_ATTN, 4 * D_ATTN + D_SSD, 4 * D_ATTN + D_SSD + D_CONV_CH]
    q, k, v, g_attn, z, xbc, dt_raw = jnp.split(proj, cuts, axis=-1)
    q = rms_norm(q.reshape(b, s, N_ATTN_HEADS, ATTN_HEAD_DIM), q_norm_w)
    k = rms_norm(k.reshape(b, s, N_ATTN_HEADS, ATTN_HEAD_DIM), k_norm_w)
    v = v.reshape(b, s, N_ATTN_HEADS, ATTN_HEAD_DIM)
    attn = moba_attention(q, k, v) * jax.nn.silu(g_attn)
    xbc = jax.nn.silu(causal_depthwise_conv(xbc, conv_w, conv_b)).astype(jnp.float32)
    xs, bm, cm = jnp.split(xbc, [D_SSD, D_SSD + SSD_GROUPS * SSD_STATE], axis=-1)
    dt = jax.nn.softplus(dt_raw.astype(jnp.float32) + dt_bias.astype(jnp.float32))
    y = ssd_scan(xs.reshape(b, s, N_SSD_HEADS, SSD_HEAD_DIM),
                 bm.reshape(b, s, SSD_GROUPS, SSD_STATE),
                 cm.reshape(b, s, SSD_GROUPS, SSD_STATE), dt, a_log, d_skip)
    y = y * jax.nn.silu(z.astype(jnp.float32))
    y = rms_norm(y.reshape(b, s, SSD_GROUPS, D_SSD // SSD_GROUPS),
                 ssd_norm_w.reshape(SSD_GROUPS, D_SSD // SSD_GROUPS)).reshape(b, s, D_SSD).astype(x.dtype)
    mixed = jnp.concatenate([attn, y], axis=-1)
    return x + mixed @ w_out


def setup_inputs(seed: int = 0) -> dict:
    key = jax.random.key(seed)
    ks = jax.random.split(key, 13)
    f32 = jnp.float32
    x = jax.random.normal(ks[0], (BATCH, SEQ, D_MODEL), f32)
    ln_w = 1.0 + 0.01 * jax.random.normal(ks[1], (DEPTH, D_MODEL), f32)
    w_in = jax.random.normal(ks[2], (DEPTH, D_MODEL, D_IN_PROJ), f32) * D_MODEL ** -0.5
    q_norm_w = 1.0 + 0.01 * jax.random.normal(ks[3], (DEPTH, ATTN_HEAD_DIM), f32)
    k_norm_w = 1.0 + 0.01 * jax.random.normal(ks[4], (DEPTH, ATTN_HEAD_DIM), f32)
    conv_w = jax.random.normal(ks[5], (DEPTH, SSD_CONV, D_CONV_CH), f32) * SSD_CONV ** -0.5
    conv_b = 0.01 * jax.random.normal(ks[6], (DEPTH, D_CONV_CH), f32)
    dt0 = jnp.exp(jax.random.uniform(ks[7], (DEPTH, N_SSD_HEADS), f32,
                                     minval=float(np.log(1e-3)), maxval=float(np.log(1e-1))))
    dt_bias = dt0 + jnp.log(-jnp.expm1(-dt0))
    a_log = jnp.log(jax.random.uniform(ks[8], (DEPTH, N_SSD_HEADS), f32, minval=1.0, maxval=16.0))
    d_skip = 1.0 + 0.01 * jax.random.normal(ks[9], (DEPTH, N_SSD_HEADS), f32)
    ssd_norm_w = 1.0 + 0.01 * jax.random.normal(ks[10], (DEPTH, D_SSD), f32)
    w_out = jax.random.normal(ks[11], (DEPTH, D_MIX, D_MODEL), f32) * D_MIX ** -0.5
    return {"x": x, "ln_w": ln_w, "w_in": w_in, "q_norm_w": q_norm_w, "k_norm_w": k_norm_w,
            "conv_w": conv_w, "conv_b": conv_b, "dt_bias": dt_bias, "a_log": a_log,
            "d_skip": d_skip, "ssd_norm_w": ssd_norm_w, "w_out": w_out}


def reference(x, ln_w, w_in, q_norm_w, k_norm_w, conv_w, conv_b, dt_bias, a_log, d_skip, ssd_norm_w, w_out):
    s = x.shape[1]
    pad = (-s) % SEQ_ALIGN
    h = jnp.pad(x, ((0, 0), (0, pad), (0, 0)))
    for i in range(DEPTH):
        h = hybrid_layer(h, ln_w[i], w_in[i], q_norm_w[i], k_norm_w[i], conv_w[i], conv_b[i],
                         dt_bias[i], a_log[i], d_skip[i], ssd_norm_w[i], w_out[i])
    return h[:, :s]
```

```python
import contextlib
import numpy as np
import concourse.bass as bass
import concourse.mybir as mybir
from concourse.bass_utils import run_bass_kernel_spmd

F32 = mybir.dt.float32
BF16 = mybir.dt.bfloat16
AF = mybir.ActivationFunctionType
ALU = mybir.AluOpType
AX = mybir.AxisListType

ENGS = ("pe", "act", "dve", "pool", "sp")
SAME_ENGINE_SYNC = {"pe": False, "act": True, "dve": True, "pool": True, "sp": False}


class Ev:
    __slots__ = ("eng", "idx", "sem", "val", "needed")

    def __init__(self, eng, idx, sem=None, val=None):
        self.eng = eng
        self.idx = idx
        self.sem = sem
        self.val = val
        self.needed = False


def ev_is_dma(ev):
    return ev.sem is not None and not isinstance(ev.sem, tuple)


class Buf:
    __slots__ = ("name", "w", "rs")

    def __init__(self, name=""):
        self.name = name
        self.w = None
        self.rs = []


class Op:
    __slots__ = ("fn", "waits", "ev", "is_dma")

    def __init__(self, fn, waits, ev, is_dma):
        self.fn = fn
        self.waits = waits
        self.ev = ev
        self.is_dma = is_dma


class Sched:
    def __init__(self, nc):
        self.nc = nc
        self.ops = {e: [] for e in ENGS}
        self.handles = {"pe": nc.tensor, "act": nc.scalar, "dve": nc.vector, "pool": nc.gpsimd, "sp": nc.sync}
        self.esem = {}
        self.dma_sems = {}
        self.dma_cnt = {}
        self.all_dma_ev = {}

    def _deps(self, eng, reads, writes, xreads=()):
        out = []

        def add(ev, raw):
            if ev.eng == eng and not ev_is_dma(ev):
                if not (raw and SAME_ENGINE_SYNC[eng]):
                    return
            out.append(ev)

        for b in reads:
            if b.w is not None:
                add(b.w, True)
        for b in xreads:
            if b.w is not None:
                add(b.w, True)
            for r in b.rs:
                add(r, False)
        for b in writes:
            if b.w is not None:
                add(b.w, False)
            for r in b.rs:
                add(r, False)
        return out

    def _upd(self, ev, reads, writes):
        for b in reads:
            b.rs.append(ev)
        for b in writes:
            b.w = ev
            b.rs = []

    def op(self, eng, fn, reads=(), writes=(), xreads=()):
        waits = self._deps(eng, reads, writes, xreads)
        ev = Ev(eng, len(self.ops[eng]))
        self.ops[eng].append(Op(fn, waits, ev, False))
        self._upd(ev, list(reads) + list(xreads), writes)
        return ev

    def dma(self, eng, semname, out, in_, reads=(), writes=(), **kw):
        waits = self._deps(eng, reads, writes)
        self.dma_cnt[semname] += 16
        ev = Ev(eng, len(self.ops[eng]), sem=semname, val=self.dma_cnt[semname])
        h = self.handles[eng]
        fn = (lambda h=h, out=out, in_=in_, kw=kw: h.dma_start(out=out, in_=in_, **kw))
        self.ops[eng].append(Op(fn, waits, ev, True))
        self.all_dma_ev[semname] = ev
        self._upd(ev, reads, writes)
        return ev

    def barrier(self):
        lasts = []
        for e in ENGS:
            for o in reversed(self.ops[e]):
                if not o.is_dma and o.fn is not None:
                    lasts.append(o.ev)
                    break
        lasts.extend(self.all_dma_ev.values())
        for e in ENGS:
            waits = [ev for ev in lasts if not (ev.eng == e and not ev_is_dma(ev))]
            ev = Ev(e, len(self.ops[e]))
            self.ops[e].append(Op(None, waits, ev, False))

    def finalize(self):
        for e in ENGS:
            for o in self.ops[e]:
                for ev in o.waits:
                    ev.needed = True
        for e in ENGS:
            cnt = 0
            for o in self.ops[e]:
                if o.is_dma:
                    continue
                if o.ev.needed:
                    assert o.fn is not None
                    cnt += 1
                    o.ev.sem = ("E", e)
                    o.ev.val = cnt

    def _sem(self, key):
        if isinstance(key, tuple):
            return self.esem[key[1]]
        return self.dma_sems[key]

    def replay(self, eng):
        h = self.handles[eng]
        seen = {}
        for o in self.ops[eng]:
            need = {}
            for ev in o.waits:
                if need.get(ev.sem, 0) < ev.val:
                    need[ev.sem] = ev.val
            for k, v in need.items():
                if seen.get(k, 0) < v:
                    h.wait_ge(self._sem(k), v)
                    seen[k] = v
            if o.fn is None:
                continue
            ins = o.fn()
            if o.is_dma:
                ins.then_inc(self._sem(o.ev.sem), 16)
            elif o.ev.needed:
                ins.then_inc(self._sem(o.ev.sem), 1)


class Arena:
    def __init__(self, base, nwords):
        self.base = base
        self.cap = nwords * 4
        self.off = 0

    def mark(self):
        return self.off

    def release(self, m):
        self.off = m

    def alloc(self, shape, dt):
        n = int(np.prod(shape))
        esz = 4 if dt == F32 else 2
        nbytes = (n * esz + 63) // 64 * 64
        st = self.off
        self.off += nbytes
        assert self.off <= self.cap, f"SBUF arena overflow {self.off} > {self.cap}"
        w0 = st // 4
        if dt == F32:
            v = self.base[:, w0:w0 + n]
        else:
            v = self.base[:, w0:w0 + nbytes // 4].bitcast(BF16)[:, 0:n]
        if len(shape) == 2:
            v = v.rearrange("p (a b) -> p a b", a=shape[0], b=shape[1])
        elif len(shape) == 3:
            v = v.rearrange("p (a b c) -> p a b c", a=shape[0], b=shape[1], c=shape[2])
        return v


D = 1024
KC = 8
NPROJ = 6672
EPS = 1e-6
NEGBIG = -30000.0

C_IDENT = 0
C_U = 128
C_NEGU = 256
C_NEG = 384
C_ONES = 512
C_BH = 640
C_BIAS = C_BH + 2048
C_G = C_BIAS + 240
C_PAST = C_G + 16
C_E = C_PAST + 256
NCONST = C_E + 2048


def make_consts():
    c = np.zeros((128, NCONST), np.float32)
    p = np.arange(128)
    c[:, C_IDENT:C_IDENT + 128] = np.eye(128)
    U = (p[:, None] <= p[None, :]).astype(np.float32)
    c[:, C_U:C_U + 128] = U
    c[:, C_NEGU:C_NEGU + 128] = -U
    c[:, C_NEG:C_NEG + 128] = np.where(p[None, :] < p[:, None], NEGBIG, 0.0)
    c[:, C_ONES:C_ONES + 128] = 1.0
    slopes = 2.0 ** (-8.0 * np.arange(1, 9) / 8.0)
    qrel = np.arange(256)
    for h in range(8):
        d = qrel[None, :] - p[:, None]
        c[:, C_BH + h * 256:C_BH + (h + 1) * 256] = np.where(d >= 0, -slopes[h] * d, NEGBIG)
        for dd in range(1, 16):
            for kt in range(2):
                c[:, C_BIAS + h * 30 + (dd - 1) * 2 + kt] = -slopes[h] * (dd * 256 - kt * 128 - p)
        for hf in range(2):
            c[:, C_G + h * 2 + hf] = np.exp(-slopes[h] * (hf * 128 + p))
    for qb in range(16):
        for n in range(16):
            c[:, C_PAST + qb * 16 + n] = 0.0 if n < qb else -1e30
    for n in range(16):
        c[n, C_E + n * 128:C_E + (n + 1) * 128] = 1.0
    return c


def pipeline(n, stages):
    maxlag = max(l for l, _ in stages)
    for t in range(n + maxlag):
        for lag, fn in stages:
            i = t - lag
            if 0 <= i < n:
                fn(i)


def build(S, depth=2, dbg=False, stop_after=None):
    NT = S // 128
    NB = S // 256
    TG = S // 512
    assert S % 512 == 0 and NB <= 16
    nc = bass.Bass("TRN2", target_bir_lowering=False)
    V, A, P, T = nc.vector, nc.scalar, nc.gpsimd, nc.tensor

    def din(name, shape, dt=F32):
        return nc.dram_tensor(name, list(shape), dt, kind="ExternalInput").ap()

    def dscr(name, shape, dt):
        kind = "ExternalOutput" if dbg else "Internal"
        return nc.dram_tensor(name, list(shape), dt, kind=kind).ap()

    x_in = din("x", [S, D])
    w_in = din("w_in", [depth, D, NPROJ])
    w_out = din("w_out", [depth, 2 * D, D])
    lnwT_d = din("lnwT", [depth, 128, 8])
    cwT_d = din("cwT", [depth, 128, 12 * 4])
    cbT_d = din("cbT", [depth, 128, 12])
    qkw_d = din("qkw", [depth, 128, 2])
    snwT_d = din("snwT", [depth, 128, 8])
    hp_d = din("hp", [depth, 1, 48])
    consts_d = din("consts", [128, NCONST])
    out_d = nc.dram_tensor("out", [S, D], F32, kind="ExternalOutput").ap()

    qT_d = dscr("qT_s", [8, 128, S], BF16)
    kT_d = dscr("kT_s", [8, 128, S], BF16)
    V_d = dscr("V_s", [S, D], BF16)
    sg_d = dscr("sg_s", [S, D], F32)
    sz_d = dscr("sz_s", [S, D], F32)
    xsT_d = dscr("xsT_s", [D, S], F32)
    bcT_d = dscr("bcT_s", [512, S], BF16)
    mixT_d = dscr("mixT_s", [16, 128, S], BF16)
    xres_d = dscr("xres_s", [S, D], F32)
    dtraw_d = dscr("dtraw_s", [128, NT * 16], F32) if dbg else None
    hT_d = dscr("hT_s", [128, KC * S], BF16) if dbg else None

    ARENA_WORDS = 52224
    with contextlib.ExitStack() as st:
        arena_t = st.enter_context(nc.sbuf_tensor("arena", [128, ARENA_WORDS], F32))
        AR = Arena(arena_t[:, :], ARENA_WORDS)
        pbanks = [st.enter_context(nc.psum_tensor(f"pb{i}", [128, 512], F32)) for i in range(8)]
        pb = [t[:, :] for t in pbanks]
        Bpb = [Buf(f"pb{i}") for i in range(8)]
        S_ = Sched(nc)
        S_.esem = {e: st.enter_context(nc.semaphore("sem_" + e)) for e in ENGS}
        sem_pool = {}

        def dsem(name):
            if name not in sem_pool:
                sem_pool[name] = st.enter_context(nc.semaphore("d_" + name))
                S_.dma_sems[name] = sem_pool[name]
                S_.dma_cnt[name] = 0
            return name

        def eop(eng, h, fname, reads, writes, **kw):
            f = getattr(h, fname)
            xr = ()
            if eng != "pe":
                xr = [b for b in writes if b in Bpb]
                writes = [b for b in writes if b not in Bpb]
                reads = list(reads) + [b for b in writes if b not in reads]
            return S_.op(eng, (lambda f=f, kw=kw: f(**kw)), reads, writes, xr)

        def dve(fname, reads, writes, **kw):
            return eop("dve", V, fname, reads, writes, **kw)

        def act(fname, reads, writes, **kw):
            return eop("act", A, fname, reads, writes, **kw)

        def pool(fname, reads, writes, **kw):
            return eop("pool", P, fname, reads, writes, **kw)

        def pe(fname, reads, writes, **kw):
            return eop("pe", T, fname, reads, writes, **kw)

        def dma(semname, out, in_, reads=(), writes=(), eng="sp"):
            return S_.dma(eng, dsem(semname), out, in_, reads, writes)

        cst = AR.alloc((NCONST,), F32)
        Bcst = Buf("cst")
        ident_bf = AR.alloc((128,), BF16)
        bh_bf = AR.alloc((8, 256), BF16)
        e_bf = AR.alloc((16, 128), BF16)
        neg_bf = AR.alloc((128,), BF16)
        mhalf = AR.alloc((8,), F32)
        Bk = Buf("constbf")
        par = {}
        Bpar = Buf("par")
        for l in range(depth):
            par[l] = dict(
                lnwT=AR.alloc((8,), F32), cwT=AR.alloc((12, 4), F32), cbT=AR.alloc((12,), F32),
                qkw=AR.alloc((2,), F32), snwT=AR.alloc((8,), F32), hp=AR.alloc((48,), F32),
                qws=AR.alloc((1,), F32), aneg=AR.alloc((16,), F32))
        dtraw = AR.alloc((NT, 16), F32)
        Bdtraw = Buf("dtraw")

        identf = cst[:, C_IDENT:C_IDENT + 128]

        dma("cst", cst, consts_d[:, :], writes=[Bcst])
        for l in range(depth):
            pl = par[l]
            dma("par", pl["lnwT"], lnwT_d[l], writes=[Bpar])
            dma("par", pl["cwT"], cwT_d[l].rearrange("p (c k) -> p c k", k=4), writes=[Bpar])
            dma("par", pl["cbT"], cbT_d[l], writes=[Bpar])
            dma("par", pl["qkw"], qkw_d[l], writes=[Bpar])
            dma("par", pl["snwT"], snwT_d[l], writes=[Bpar])
            dma("par", pl["hp"], hp_d[l].partition_broadcast(128), writes=[Bpar])
        pool("tensor_copy", [Bcst], [Bk], out=ident_bf, in_=identf)
        pool("tensor_copy", [Bcst], [Bk], out=bh_bf, in_=cst[:, C_BH:C_BH + 2048].rearrange("p (h q) -> p h q", q=256))
        pool("tensor_copy", [Bcst], [Bk], out=e_bf, in_=cst[:, C_E:C_E + 2048].rearrange("p (n k) -> p n k", k=128))
        pool("tensor_copy", [Bcst], [Bk], out=neg_bf, in_=cst[:, C_NEG:C_NEG + 128])
        pool("memset", [], [Bk], ap=mhalf, constant=-0.5)
        for l in range(depth):
            pl = par[l]
            pool("tensor_scalar", [Bpar], [Bpar], out=pl["qws"], in0=pl["qkw"][:, 0:1], scalar1=float(128 ** -0.5), scalar2=None, op0=ALU.mult)
            act("activation", [Bpar], [Bpar], out=pl["aneg"], in_=pl["hp"][:, 16:32], func=AF.Exp)
            pool("tensor_scalar", [Bpar], [Bpar], out=pl["aneg"], in0=pl["aneg"], scalar1=-1.0, scalar2=None, op0=ALU.mult)
        S_.barrier()

        base_mark = AR.mark()
        p3_mark = base_mark

        for l in range(depth):
            pl = par[l]
            xsrc = x_in if l == 0 else xres_d
            xdst = xres_d if l < depth - 1 else out_d
            AR.release(base_mark)
            hT = AR.alloc((KC, S), BF16)
            BhT = Buf("hT")
            m1 = AR.mark()
            xt = [AR.alloc((D,), F32) for _ in range(2)]
            Bxt = [Buf("xt0"), Buf("xt1")]
            xn = [AR.alloc((D,), BF16) for _ in range(2)]
            Bxn = [Buf("xn0"), Buf("xn1")]
            junk = AR.alloc((D,), BF16)
            Bjunk = Buf("junk")
            ss1 = [AR.alloc((1,), F32) for _ in range(2)]
            Bss1 = [Buf("ss0"), Buf("ss1")]

            def p1_load(i):
                s = i % 2
                dma(f"xt{s}", xt[s], xsrc[i * 128:(i + 1) * 128, :], writes=[Bxt[s]])

            def p1_norm(i):
                s = i % 2
                act("activation", [Bxt[s]], [Bjunk, Bss1[s]], out=junk, in_=xt[s], func=AF.Square, accum_out=ss1[s])
                pool("tensor_scalar", [Bss1[s]], [Bss1[s]], out=ss1[s], in0=ss1[s], scalar1=1.0 / D, scalar2=EPS, op0=ALU.mult, op1=ALU.add)
                pool("tensor_tensor", [Bss1[s], Bk], [Bss1[s]], out=ss1[s], in0=ss1[s], in1=mhalf[:, 0:1], op=ALU.pow)
                dve("tensor_scalar", [Bxt[s], Bss1[s]], [Bxn[s]], out=xn[s], in0=xt[s], scalar1=ss1[s][:, 0:1], scalar2=None, op0=ALU.mult)

            def p1_tr(i):
                s = i % 2
                bk = 6 + (i % 2)
                pbt = pb[bk].bitcast(BF16)
                for c in range(KC):
                    pe("transpose", [Bxn[s], Bk], [Bpb[bk]], out=pbt[:, c * 128:(c + 1) * 128], in_=xn[s][:, c * 128:(c + 1) * 128], identity=ident_bf)
                dve("tensor_tensor", [Bpar], [Bpb[bk], BhT], out=hT[:, :, i * 128:(i + 1) * 128],
                    in0=pbt.rearrange("p (c t) -> p c t", t=128),
                    in1=pl["lnwT"].unsqueeze(2).to_broadcast([128, KC, 128]), op=ALU.mult)

            p1_load(0)
            for t in range(NT + 1):
                if t + 1 < NT:
                    p1_load(t + 1)
                if t < NT:
                    p1_norm(t)
                if t >= 1:
                    p1_tr(t - 1)
            if dbg and l == 0:
                dma("dbg", hT_d[:, :], hT.rearrange("p a b -> p (a b)"), reads=[BhT])
            if stop_after == "P1":
                break

            wst = [AR.alloc((KC, 512), F32) for _ in range(2)]
            Bwst = [Buf("wst0"), Buf("wst1")]
            wbf = [AR.alloc((KC, 512), BF16) for _ in range(2)]
            Bwbf = [Buf("wbf0"), Buf("wbf1")]
            NG = 14
            w_l = w_in[l]

            def gcols(g):
                c0 = g * 512
                return c0, min(512, NPROJ - c0)

            def wload(g):
                s = g % 2
                c0, ncl = gcols(g)
                dma(f"wst{s}", wst[s][:, :, 0:ncl], w_l[:, c0:c0 + ncl].rearrange("(k p) n -> p k n", p=128), writes=[Bwst[s]])

            def wconv(g):
                s = g % 2
                c0, ncl = gcols(g)
                pool("tensor_copy", [Bwst[s]], [Bwbf[s]], out=wbf[s][:, :, 0:ncl], in_=wst[s][:, :, 0:ncl])

            ss4 = [AR.alloc((4,), F32) for _ in range(2)]
            Bss4 = [Buf("ss4a"), Buf("ss4b")]
            sq = [AR.alloc((512,), F32) for _ in range(2)]
            Bsq = [Buf("sq0"), Buf("sq1")]
            qn = [AR.alloc((4, 128), BF16) for _ in range(2)]
            Bqn = [Buf("qn0"), Buf("qn1")]
            qst = [AR.alloc((4, 512), BF16) for _ in range(2)]
            Bqst = [Buf("qst0"), Buf("qst1")]
            vst = [AR.alloc((512,), BF16) for _ in range(2)]
            Bvst = [Buf("vst0"), Buf("vst1")]
            fst = [AR.alloc((512,), F32) for _ in range(2)]
            Bfst = [Buf("fst0"), Buf("fst1")]
            ubuf = [AR.alloc((515,), F32) for _ in range(2)]
            Bu = [Buf("u0"), Buf("u1")]
            cacc = [AR.alloc((512,), F32) for _ in range(2)]
            cacc2 = [AR.alloc((512,), F32) for _ in range(2)]
            cacc3 = [AR.alloc((512,), F32) for _ in range(2)]
            Bcacc2 = [Buf("cacc2a"), Buf("cacc2b")]
            Bcacc = [Buf("cacc0"), Buf("cacc1")]
            xo_f = [AR.alloc((512,), F32) for _ in range(2)]
            xo_b = [AR.alloc((512,), BF16) for _ in range(2)]
            Bxo = [Buf("xo0"), Buf("xo1")]
            accn = [0]

            def next_acc():
                b = accn[0] % 4
                accn[0] += 1
                return b

            def mm_tok(g, i, ncl, bk):
                s = g % 2
                for k in range(KC):
                    pe("matmul", [BhT, Bwbf[s]], [Bpb[bk]], out=pb[bk][:, 0:ncl], lhsT=hT[:, k, i * 128:(i + 1) * 128],
                       rhs=wbf[s][:, k, 0:ncl], start=(k == 0), stop=(k == KC - 1))

            def do_qk(g):
                which = 0 if g < 2 else 1
                h0 = (g % 2) * 4
                dst = qT_d if which == 0 else kT_d
                wsc = pl["qws"] if which == 0 else pl["qkw"][:, 1:2]
                banks = {}

                def s0(i):
                    bk = next_acc()
                    banks[i] = bk
                    mm_tok(g, i, 512, bk)
                    s = i % 2
                    act("activation", [], [Bpb[bk], Bsq[s]], out=sq[s], in_=pb[bk], func=AF.Square)
                    dve("tensor_reduce", [Bsq[s]], [Bss4[s]], out=ss4[s], in_=sq[s].rearrange("p (h d) -> p h d", d=128), axis=AX.X, op=ALU.add)
                    pool("tensor_scalar", [], [Bss4[s]], out=ss4[s], in0=ss4[s], scalar1=1.0 / 128, scalar2=EPS, op0=ALU.mult, op1=ALU.add)
                    pool("tensor_tensor", [Bk], [Bss4[s]], out=ss4[s], in0=ss4[s], in1=mhalf[:, 0:4], op=ALU.pow)
                    dve("tensor_tensor", [Bss4[s]], [Bpb[bk], Bqn[s]], out=qn[s], in0=pb[bk].rearrange("p (h d) -> p h d", d=128),
                        in1=ss4[s].unsqueeze(2).to_broadcast([128, 4, 128]), op=ALU.mult)

                def s2(i):
                    s = i % 2
                    bk = 4 + (i % 2)
                    pbt = pb[bk].bitcast(BF16)
                    for hh in range(4):
                        pe("transpose", [Bqn[s], Bk], [Bpb[bk]], out=pbt[:, hh * 128:(hh + 1) * 128], in_=qn[s][:, hh, :], identity=ident_bf)
                    ss_ = (i // 4) % 2
                    j = i % 4
                    act("activation", [Bpar], [Bpb[bk], Bqst[ss_]], out=qst[ss_][:, :, j * 128:(j + 1) * 128],
                        in_=pbt[:, 0:512].rearrange("p (h t) -> p h t", t=128), func=AF.Copy, scale=wsc)
                    if j == 3:
                        tg = i // 4
                        dma(f"qst{ss_}", dst[h0:h0 + 4, :, tg * 512:(tg + 1) * 512].rearrange("h p t -> p h t"), qst[ss_], reads=[Bqst[ss_]])

                pipeline(NT, [(2, s2), (0, s0)])

            def do_tok(g, kind):
                c0 = (g % 2) * 512

                def s0(i):
                    bk = next_acc()
                    mm_tok(g, i, 512, bk)
                    s = i % 2
                    if kind == "v":
                        act("activation", [], [Bpb[bk], Bvst[s]], out=vst[s], in_=pb[bk], func=AF.Copy)
                        dma(f"vst{s}", V_d[i * 128:(i + 1) * 128, c0:c0 + 512], vst[s], reads=[Bvst[s]])
                    else:
                        act("activation", [], [Bpb[bk], Bfst[s]], out=fst[s], in_=pb[bk], func=AF.Silu)
                        dd = sg_d if kind == "g" else sz_d
                        dma(f"fst{s}", dd[i * 128:(i + 1) * 128, c0:c0 + 512], fst[s], reads=[Bfst[s]])

                pipeline(NT, [(0, s0)])

            def do_feat(g):
                s_w = g % 2
                nloc = 4
                items = [(cl, tg) for cl in range(nloc) for tg in range(TG)]

                def s0(ix):
                    cl, tg = items[ix]
                    cc = (g - 10) * 4 + cl
                    bk = next_acc()
                    for k in range(KC):
                        pe("matmul", [BhT, Bwbf[s_w]], [Bpb[bk]], out=pb[bk], lhsT=wbf[s_w][:, k, cl * 128:(cl + 1) * 128],
                           rhs=hT[:, k, tg * 512:(tg + 1) * 512], start=(k == 0), stop=(k == KC - 1))
                    s = ix % 2
                    if tg == 0:
                        pool("memset", [], [Bu[s]], ap=ubuf[s][:, 0:3], constant=0.0)
                    else:
                        pool("tensor_copy", [Bu[1 - s]], [Bu[s]], out=ubuf[s][:, 0:3], in_=ubuf[1 - s][:, 512:515])
                    act("activation", [], [Bpb[bk], Bu[s]], out=ubuf[s][:, 3:515], in_=pb[bk], func=AF.Copy)
                    cw = pl["cwT"]
                    dve("tensor_scalar", [Bu[s]], [Bcacc[s]], out=cacc[s], in0=ubuf[s][:, 3:515], scalar1=cw[:, cc, 3:4], scalar2=None, op0=ALU.mult)
                    dve("scalar_tensor_tensor", [Bu[s]], [Bcacc[s]], out=cacc[s], in0=ubuf[s][:, 2:514], scalar=cw[:, cc, 2:3], in1=cacc[s], op0=ALU.mult, op1=ALU.add)
                    pool("tensor_scalar", [Bu[s]], [Bcacc2[s]], out=cacc2[s], in0=ubuf[s][:, 1:513], scalar1=cw[:, cc, 1:2], scalar2=None, op0=ALU.mult)
                    pool("tensor_scalar", [Bu[s]], [Bcacc2[s]], out=cacc3[s], in0=ubuf[s][:, 0:512], scalar1=cw[:, cc, 0:1], scalar2=None, op0=ALU.mult)
                    pool("tensor_tensor", [], [Bcacc2[s]], out=cacc2[s], in0=cacc2[s], in1=cacc3[s], op=ALU.add)
                    dve("tensor_tensor", [Bcacc2[s]], [Bcacc[s]], out=cacc[s], in0=cacc[s], in1=cacc2[s], op=ALU.add)
                    if cc < 8:
                        act("activation", [Bcacc[s], Bpar], [Bxo[s]], out=xo_f[s], in_=cacc[s], func=AF.Silu, bias=pl["cbT"][:, cc:cc + 1])
                        dma(f"xo{s}", xsT_d[cc * 128:(cc + 1) * 128, tg * 512:(tg + 1) * 512], xo_f[s], reads=[Bxo[s]])
                    else:
                        act("activation", [Bcacc[s], Bpar], [Bxo[s]], out=xo_b[s], in_=cacc[s], func=AF.Silu, bias=pl["cbT"][:, cc:cc + 1])
                        dma(f"xo{s}", bcT_d[(cc - 8) * 128:(cc - 7) * 128, tg * 512:(tg + 1) * 512], xo_b[s], reads=[Bxo[s]])

                pipeline(len(items), [(0, s0)])

            def do_dt(g):
                def s0(i):
                    bk = next_acc()
                    mm_tok(g, i, 16, bk)
                    dve("tensor_copy", [], [Bpb[bk], Bdtraw], out=dtraw[:, i, :], in_=pb[bk][:, 0:16])
                pipeline(NT, [(0, s0)])

            wload(0)
            wconv(0)
            wload(1)
            for g in range(NG):
                if g + 1 < NG:
                    wconv(g + 1)
                if g + 2 < NG:
                    wload(g + 2)
                if g < 4:
                    do_qk(g)
                elif g < 6:
                    do_tok(g, "v")
                elif g < 8:
                    do_tok(g, "g")
                elif g < 10:
                    do_tok(g, "z")
                elif g < 13:
                    do_feat(g)
                else:
                    do_dt(g)
            if dbg:
                dma("dbg", dtraw_d[:, :], dtraw.rearrange("p a b -> p (a b)"), reads=[Bdtraw])
            S_.barrier()
            if stop_after == "P2":
                break
            AR.release(p3_mark)
            qT_h = [AR.alloc((S,), BF16) for _ in range(2)]
            kT_h = [AR.alloc((S,), BF16) for _ in range(2)]
            V_h = [AR.alloc((NT, 129), BF16) for _ in range(2)]
            sg_h = [AR.alloc((NT, 128), F32) for _ in range(2)]
            Bq = [Buf("q0"), Buf("q1")]
            Bkk = [Buf("k0"), Buf("k1")]
            Bv = [Buf("v0"), Buf("v1")]
            Bsg = [Buf("sg0"), Buf("sg1")]
            attnT = [AR.alloc((S,), BF16) for _ in range(2)]
            Battn = [Buf("at0"), Buf("at1")]
            maskT = [AR.alloc((S,), BF16) for _ in range(2)]
            Bmask = [Buf("mk0"), Buf("mk1")]
            km = AR.alloc((16,), F32)
            kms = AR.alloc((16,), F32)
            kmh = [AR.alloc((16,), BF16) for _ in range(2)]
            kml = [AR.alloc((16,), BF16) for _ in range(2)]
            Bkm = Buf("km")
            Bkmhl = [Buf("kmhl0"), Buf("kmhl1")]
            gm = [AR.alloc((16,), F32) for _ in range(2)]
            top8 = [AR.alloc((8,), F32) for _ in range(2)]
            thr = [AR.alloc((1,), F32) for _ in range(2)]
            mk = [AR.alloc((16,), BF16) for _ in range(2)]
            Bgm = [Buf("gm0"), Buf("gm1")]
            Bmkk = [Buf("mkk0"), Buf("mkk1")]
            NPT = 6
            PT = [AR.alloc((512,), BF16) for _ in range(NPT)]
            BPT = [Buf(f"pt{i}") for i in range(NPT)]
            tsc = [AR.alloc((129,), F32) for _ in range(2)]
            num = [AR.alloc((129,), F32) for _ in range(2)]
            rden = [AR.alloc((1,), F32) for _ in range(2)]
            o_bf = [AR.alloc((128,), BF16) for _ in range(2)]
            Bcmb = [Buf("cmb0"), Buf("cmb1")]
            Bobf = [Buf("obf0"), Buf("obf1")]

            pool("memset", [], [Bkm], ap=km, constant=0.0)
            for s in range(2):
                pool("memset", [], [Bv[s]], ap=V_h[s][:, :, 128:129], constant=1.0)
                pool("memset", [], [Bmask[s]], ap=maskT[s], constant=0.0)

            def head_load(h):
                s = h % 2
                dma(f"hq{s}", qT_h[s], qT_d[h], writes=[Bq[s]])
                dma(f"hk{s}", kT_h[s], kT_d[h], writes=[Bkk[s]])
                dma(f"hv{s}", V_h[s][:, :, 0:128], V_d[:, h * 128:(h + 1) * 128].rearrange("(t p) d -> p t d", p=128), writes=[Bv[s]])
                dma(f"hg{s}", sg_h[s], sg_d[:, h * 128:(h + 1) * 128].rearrange("(t p) d -> p t d", p=128), writes=[Bsg[s]])

            def head_kmean(h):
                s = h % 2
                dve("tensor_reduce", [Bkk[s]], [Bkm], out=km[:, 0:NB], in_=kT_h[s].rearrange("p (n k) -> p n k", k=256), axis=AX.X, op=ALU.add)
                dve("tensor_scalar", [], [Bkm], out=kms, in0=km, scalar1=1.0 / 256, scalar2=None, op0=ALU.mult)
                dve("tensor_copy", [Bkm], [Bkmhl[s]], out=kmh[s], in_=kms)
                dve("tensor_tensor", [Bkm], [Bkmhl[s]], out=kml[s], in0=kms, in1=kmh[s], op=ALU.subtract)

            def gate_front(h, qb):
                s = h % 2
                for j in range(2):
                    i = 2 * qb + j
                    pe("matmul", [Bq[s], Bkmhl[s]], [Bpb[6]], out=pb[6][:, j * 16:(j + 1) * 16], lhsT=qT_h[s][:, i * 128:(i + 1) * 128], rhs=kmh[s], start=True, stop=False)
                    pe("matmul", [Bq[s], Bkmhl[s]], [Bpb[6]], out=pb[6][:, j * 16:(j + 1) * 16], lhsT=qT_h[s][:, i * 128:(i + 1) * 128], rhs=kml[s], start=False, stop=True)
                for j in range(2):
                    dve("tensor_tensor", [], [Bpb[6], Bgm[j]], out=gm[j], in0=pb[6][:, j * 16:(j + 1) * 16], in1=cst[:, C_PAST + qb * 16:C_PAST + (qb + 1) * 16], op=ALU.add)
                    dve("max", [], [Bgm[j]], out=top8[j], in_=gm[j])
                    dve("tensor_scalar", [], [Bgm[j]], out=thr[j], in0=top8[j][:, 2:3], scalar1=-1e29, scalar2=None, op0=ALU.max)
                    dve("tensor_scalar", [Bgm[j]], [Bmkk[j]], out=mk[j], in0=gm[j], scalar1=thr[j][:, 0:1], scalar2=NEGBIG, op0=ALU.is_lt, op1=ALU.mult)

            def gate_back(h, qb):
                s = h % 2
                pbt = pb[7].bitcast(BF16)
                for j in range(2):
                    pe("transpose", [Bmkk[j]], [Bpb[7]], out=pbt[0:16, j * 128:(j + 1) * 128], in_=mk[j], identity=ident_bf)
                dve("tensor_copy", [], [Bpb[7], Bmask[s]], out=maskT[s][0:16, qb * 256:(qb + 1) * 256], in_=pbt[0:16, 0:256])

            deferred = []

            def defer(n, fn):
                deferred.append([n, fn])

            def tick():
                for d_ in list(deferred):
                    d_[0] -= 1
                    if d_[0] <= 0:
                        deferred.remove(d_)
                        d_[1]()

            def flush():
                while deferred:
                    d_ = deferred.pop(0)
                    d_[1]()

            def attn_head(h, first, last_head):
                s = h % 2
                NP = S // 512
                items = []
                for p in range(NP):
                    for kt in range(4 * p):
                        items.append(("pc", p, kt))
                    items.append(("own0", 2 * p, 4 * p))
                    items.append(("own1", 2 * p, 4 * p + 1))
                    items.append(("po", p, 4 * p))
                    items.append(("po", p, 4 * p + 1))
                    items.append(("own0", 2 * p + 1, 4 * p + 2))
                    items.append(("own1", 2 * p + 1, 4 * p + 3))
                slots = {}
                seen_pair = set()
                seen_po = set()

                def exp_past(sb_, ps_, c0, qb, n, kt):
                    bcol = C_BIAS + h * 30 + (qb - n - 1) * 2 + (kt % 2)
                    act("activation", [], [Bpb[sb_], BPT[ps_]], out=PT[ps_][:, c0:c0 + 256], in_=pb[sb_][:, c0:c0 + 256], func=AF.Exp, bias=cst[:, bcol:bcol + 1])

                def stA(ix):
                    kind, a0, kt = items[ix]
                    sb_ = ix % 3
                    ps_ = ix % NPT
                    slots[ix] = ps_
                    if not last_head:
                        if kind in ("pc", "own0") and (a0 if kind == "pc" else a0 // 2) not in seen_pair and (kind == "pc" or a0 % 2 == 0):
                            p_ = a0 if kind == "pc" else a0 // 2
                            seen_pair.add(p_)
                            gate_front(h + 1, 2 * p_)
                            defer(4, (lambda qb=2 * p_: gate_back(h + 1, qb)))
                        if kind == "po" and a0 not in seen_po:
                            seen_po.add(a0)
                            gate_front(h + 1, 2 * a0 + 1)
                            defer(4, (lambda qb=2 * a0 + 1: gate_back(h + 1, qb)))
                    n = kt // 2
                    if kind == "pc":
                        p = a0
                        qpair = qT_h[s][:, p * 512:(p + 1) * 512]
                        pe("matmul", [Bq[s], Bkk[s]], [Bpb[sb_]], out=pb[sb_], lhsT=kT_h[s][:, kt * 128:(kt + 1) * 128], rhs=qpair, start=True, stop=False)
                        pe("matmul", [Bmask[s]], [Bpb[sb_]], out=pb[sb_], lhsT=e_bf[0:16, n, :], rhs=maskT[s][0:16, p * 512:(p + 1) * 512], start=False, stop=True)
                        exp_past(sb_, ps_, 0, 2 * p, n, kt)
                        exp_past(sb_, ps_, 256, 2 * p + 1, n, kt)
                    elif kind == "po":
                        qb = 2 * a0 + 1
                        qblk = qT_h[s][:, qb * 256:(qb + 1) * 256]
                        pe("matmul", [Bq[s], Bkk[s]], [Bpb[sb_]], out=pb[sb_][:, 0:256], lhsT=kT_h[s][:, kt * 128:(kt + 1) * 128], rhs=qblk, start=True, stop=False)
                        pe("matmul", [Bmask[s]], [Bpb[sb_]], out=pb[sb_][:, 0:256], lhsT=e_bf[0:16, n, :], rhs=maskT[s][0:16, qb * 256:(qb + 1) * 256], start=False, stop=True)
                        exp_past(sb_, ps_, 0, qb, n, kt)
                    elif kind == "own0":
                        qb = a0
                        qblk = qT_h[s][:, qb * 256:(qb + 1) * 256]
                        pe("matmul", [Bq[s], Bkk[s]], [Bpb[sb_]], out=pb[sb_][:, 0:256], lhsT=kT_h[s][:, kt * 128:(kt + 1) * 128], rhs=qblk, start=True, stop=False)
                        pe("matmul", [], [Bpb[sb_]], out=pb[sb_][:, 0:256], lhsT=ident_bf, rhs=bh_bf[:, h, :], start=False, stop=True)
                        act("activation", [], [Bpb[sb_], BPT[ps_]], out=PT[ps_][:, 0:256], in_=pb[sb_][:, 0:256], func=AF.Exp)
                    else:
                        qb = a0
                        qblk = qT_h[s][:, qb * 256:(qb + 1) * 256]
                        pe("matmul", [Bq[s], Bkk[s]], [Bpb[sb_]], out=pb[sb_][:, 0:128], lhsT=kT_h[s][:, kt * 128:(kt + 1) * 128], rhs=qblk[:, 128:256], start=True, stop=False)
                        pe("matmul", [], [Bpb[sb_]], out=pb[sb_][:, 0:128], lhsT=ident_bf, rhs=bh_bf[:, h, 0:128], start=False, stop=True)
                        act("activation", [], [Bpb[sb_], BPT[ps_]], out=PT[ps_][:, 0:128], in_=pb[sb_][:, 0:128], func=AF.Exp)
                    tick()

                def stB(ix):
                    kind, a0, kt = items[ix]
                    ps_ = slots[ix]
                    if kind == "pc":
                        p = a0
                        for j in range(4):
                            bk = 3 + j // 2
                            c0 = (j % 2) * 256
                            pe("matmul", [BPT[ps_], Bv[s]], [Bpb[bk]], out=pb[bk][:, c0:c0 + 129], lhsT=PT[ps_][:, j * 128:(j + 1) * 128], rhs=V_h[s][:, kt, :],
                               start=(kt == 0 and j % 2 == 0), stop=(j < 2 and kt == 4 * p - 1), skip_group_check=True)
                    elif kind == "po":
                        p = a0
                        for jj in range(2):
                            c0 = jj * 256
                            pe("matmul", [BPT[ps_], Bv[s]], [Bpb[4]], out=pb[4][:, c0:c0 + 129], lhsT=PT[ps_][:, jj * 128:(jj + 1) * 128], rhs=V_h[s][:, kt, :],
                               start=(kt == 0 and jj == 0), stop=(kt == 4 * p + 1), skip_group_check=True)
                    elif kind == "own0":
                        pe("matmul", [BPT[ps_], Bv[s]], [Bpb[5]], out=pb[5][:, 0:129], lhsT=PT[ps_][:, 0:128], rhs=V_h[s][:, kt, :], start=True, stop=True)
                        pe("matmul", [BPT[ps_], Bv[s]], [Bpb[5]], out=pb[5][:, 256:385], lhsT=PT[ps_][:, 128:256], rhs=V_h[s][:, kt, :], start=True, stop=False)
                    else:
                        qb = a0
                        pe("matmul", [BPT[ps_], Bv[s]], [Bpb[5]], out=pb[5][:, 256:385], lhsT=PT[ps_][:, 0:128], rhs=V_h[s][:, kt, :], start=False, stop=True)
                        obk = 3 + (qb % 2)
                        for j in range(2):
                            i = 2 * qb + j
                            if qb > 0:
                                gcol = C_G + h * 2 + j
                                act("activation", [], [Bpb[obk], Bcmb[j]], out=tsc[j], in_=pb[obk][:, j * 256:j * 256 + 129], func=AF.Copy, scale=cst[:, gcol:gcol + 1])
                                dve("tensor_tensor", [], [Bpb[5], Bcmb[j]], out=num[j], in0=tsc[j], in1=pb[5][:, j * 256:j * 256 + 129], op=ALU.add)
                            else:
                                dve("tensor_copy", [], [Bpb[5], Bcmb[j]], out=num[j], in_=pb[5][:, j * 256:j * 256 + 129])
                            dve("reciprocal", [], [Bcmb[j]], out=rden[j], in_=num[j][:, 128:129])
                            dve("scalar_tensor_tensor", [Bcmb[j], Bsg[s]], [Bobf[j]], out=o_bf[j], in0=num[j][:, 0:128], scalar=rden[j][:, 0:1], in1=sg_h[s][:, i, :],
                                op0=ALU.mult, op1=ALU.mult)

                        def back(qb=qb):
                            pbt = pb[7].bitcast(BF16)
                            for j in range(2):
                                pe("transpose", [Bobf[j]], [Bpb[7]], out=pbt[:, 512 + j * 128:512 + (j + 1) * 128], in_=o_bf[j], identity=ident_bf)
                            dve("tensor_copy", [], [Bpb[7], Battn[s]], out=attnT[s][:, qb * 256:(qb + 1) * 256], in_=pbt[:, 512:768])
                        defer(3, back)

                pipeline(len(items), [(2, stB), (0, stA)])
                flush()
                dma(f"at{s}", mixT_d[h], attnT[s], reads=[Battn[s]])

            def pe_warm(n):
                rhs_w = bh_bf[:, 0:2, :].rearrange("p a b -> p (a b)")
                for i_ in range(n):
                    bk_ = i_ % 3
                    pe("matmul", [], [Bpb[bk_]], out=pb[bk_], lhsT=ident_bf, rhs=rhs_w, start=True, stop=True)

            head_load(0)
            head_kmean(0)
            for qb in range(NB):
                gate_front(0, qb)
                gate_back(0, qb)
            for h in range(8):
                if h + 1 < 8:
                    head_load(h + 1)
                    head_kmean(h + 1)
                pe_warm(20)
                attn_head(h, h == 0, h == 7)
            S_.barrier()
            if stop_after == "P3":
                break
            AR.release(p3_mark)
            NCH = NT
            hp = pl["hp"]
            xsT_c = [AR.alloc((8, 128), F32) for _ in range(2)]
            bcT_c = [AR.alloc((4, 128), BF16) for _ in range(2)]
            sz_c = [AR.alloc((D,), F32) for _ in range(2)]
            Bxs = [Buf("xsc0"), Buf("xsc1")]
            Bbc = [Buf("bcc0"), Buf("bcc1")]
            Bsz = [Buf("szc0"), Buf("szc1")]
            x1a = AR.alloc((NT, 16), F32)
            axa = AR.alloc((NT, 16), F32)
            lga = AR.alloc((NT, 16), F32)
            dta = AR.alloc((NT, 16), F32)
            dAa = AR.alloc((NT, 16), F32)
            acs_a = AR.alloc((NT, 16), F32)
            aend_a = AR.alloc((NT, 16), F32)
            dse_a = AR.alloc((NT, 16), F32)
            nacs_a = AR.alloc((NT, 16), F32)
            ea_a = AR.alloc((NT, 16), F32)
            cd_a = AR.alloc((NT, 16), F32)
            eds_a = AR.alloc((NT, 16), F32)
            dtw_a = AR.alloc((NT, 16), F32)
            Bdt = Buf("dtc")
            Rm = AR.alloc((16, 128), F32)
            BR = Buf("R")
            negb4 = AR.alloc((4, 128), BF16)
            decay = AR.alloc((16, 128), BF16)
            Bdecay = Buf("decay")
            MT = [AR.alloc((16, 128), BF16) for _ in range(2)]
            BMT = [Buf("MT0"), Buf("MT1")]
            cb_sb = AR.alloc((2, 128), BF16)
            Bcb = Buf("cb")
            Btok = [AR.alloc((2, 128), BF16) for _ in range(2)]
            BBtok = [Buf("btok0"), Buf("btok1")]
            xs_sb = AR.alloc((16, 64), F32)
            Bxssb = Buf("xssb")
            xdt = [AR.alloc((16, 64), BF16) for _ in range(2)]
            xw = [AR.alloc((16, 64), BF16) for _ in range(2)]
            xD = [AR.alloc((16, 64), F32) for _ in range(2)]
            Bxdt = [Buf("xdt0"), Buf("xdt1")]
            Bxw = [Buf("xw0"), Buf("xw1")]
            BxD = [Buf("xD0"), Buf("xD1")]
            prev = AR.alloc((16, 64), F32)
            prevbf = AR.alloc((16, 64), BF16)
            Bprev = Buf("prev")
            Bprevbf = Buf("prevbf")
            t1 = AR.alloc((16, 64), F32)
            Bt1 = Buf("t1")
            yg = AR.alloc((16, 64), F32)
            Byg = Buf("yg")
            ss2 = AR.alloc((2,), F32)
            Bss2 = Buf("ss2")
            yn = AR.alloc((D,), BF16)
            Byn = Buf("yn")
            ystage = [AR.alloc((8, 512), BF16) for _ in range(2)]
            Byst = [Buf("yst0"), Buf("yst1")]
            junk4 = AR.alloc((512,), BF16)
            Bjunk4 = Buf("junk4")
            ones_f = cst[:, C_ONES:C_ONES + 128]
            U_f = cst[:, C_U:C_U + 128]

            pool("memset", [], [Bprev], ap=prev, constant=0.0)
            pool("memset", [], [Bprevbf], ap=prevbf, constant=0.0)
            pool("tensor_copy", [], [BR], out=negb4, in_=cst[:, C_NEG:C_NEG + 128].unsqueeze(1).to_broadcast([128, 4, 128]))

            NW = NT * 16
            fl = lambda t: t.rearrange("p a b -> p (a b)")
            pool("tensor_tensor", [Bdtraw], [Bdt], out=x1a, in0=dtraw, in1=hp[:, 0:16].unsqueeze(1).to_broadcast([128, NT, 16]), op=ALU.add)
            act("activation", [], [Bdt], out=axa, in_=x1a, func=AF.Abs)
            act("activation", [], [Bdt], out=axa, in_=axa, func=AF.Exp, scale=-1.0)
            act("activation", [], [Bdt], out=lga, in_=axa, func=AF.Ln, bias=1.0)
            dve("tensor_scalar", [], [Bdt], out=x1a, in0=x1a, scalar1=0.0, scalar2=None, op0=ALU.max)
            dve("tensor_tensor", [], [Bdt], out=dta, in0=x1a, in1=lga, op=ALU.add)
            dve("tensor_tensor", [], [Bdt], out=dAa, in0=dta, in1=pl["aneg"].unsqueeze(1).to_broadcast([128, NT, 16]), op=ALU.mult)
            pe("matmul", [Bdt], [Bpb[2]], out=pb[2][:, 0:NW], lhsT=U_f, rhs=fl(dAa), start=True, stop=True)
            pe("matmul", [Bdt], [Bpb[3]], out=pb[3][:, 0:NW], lhsT=ones_f, rhs=fl(dAa), start=True, stop=True)
            act("activation", [], [Bpb[2], Bdt], out=fl(acs_a), in_=pb[2][:, 0:NW], func=AF.Copy)
            act("activation", [], [Bpb[3], Bdt], out=fl(aend_a), in_=pb[3][:, 0:NW], func=AF.Copy)
            dve("tensor_tensor", [], [Bdt], out=dse_a, in0=aend_a, in1=acs_a, op=ALU.subtract)
            dve("tensor_scalar", [], [Bdt], out=nacs_a, in0=acs_a, scalar1=-1.0, scalar2=None, op0=ALU.mult)
            act("activation", [], [Bdt], out=ea_a, in_=acs_a, func=AF.Exp)
            act("activation", [], [Bdt], out=cd_a, in_=aend_a, func=AF.Exp)
            act("activation", [], [Bdt], out=eds_a, in_=dse_a, func=AF.Exp)
            dve("tensor_tensor", [], [Bdt], out=dtw_a, in0=dta, in1=eds_a, op=ALU.mult)

            def p4_load(c):
                s = c % 2
                dma(f"xsc{s}", xsT_c[s], xsT_d[:, c * 128:(c + 1) * 128].rearrange("(k p) t -> p k t", p=128), writes=[Bxs[s]])
                dma(f"bcc{s}", bcT_c[s], bcT_d[:, c * 128:(c + 1) * 128].rearrange("(k p) t -> p k t", p=128), writes=[Bbc[s]])
                dma(f"szc{s}", sz_c[s], sz_d[c * 128:(c + 1) * 128, :], writes=[Bsz[s]])

            def stA(c):
                s = c % 2
                pool("tensor_tensor", [Bdt], [BR], out=Rm, in0=U_f.unsqueeze(1).to_broadcast([128, 16, 128]),
                     in1=dAa[:, c, :].unsqueeze(2).to_broadcast([128, 16, 128]), op=ALU.mult)
                for g in range(2):
                    pe("matmul", [Bbc[s]], [Bpb[2]], out=pb[2][:, 64 + g * 128:64 + (g + 1) * 128], lhsT=bcT_c[s][:, g, :], rhs=bcT_c[s][:, 2 + g, :], start=True, stop=True)
                pbt2 = pb[2].bitcast(BF16)
                for g in range(2):
                    pe("transpose", [Bbc[s]], [Bpb[2]], out=pbt2[:, 768 + g * 128:768 + (g + 1) * 128], in_=bcT_c[s][:, g, :], identity=ident_bf)
                act("activation", [], [Bpb[2], Bcb], out=cb_sb, in_=pb[2][:, 64:320].rearrange("p (g l) -> p g l", l=128), func=AF.Copy)
                act("activation", [], [Bpb[2], BBtok[s]], out=Btok[s], in_=pbt2[:, 768:1024].rearrange("p (g l) -> p g l", l=128), func=AF.Copy)
                Rflat = Rm.rearrange("p h l -> p (h l)")
                for qd in range(4):
                    bk = qd % 2
                    pe("matmul", [BR], [Bpb[bk]], out=pb[bk], lhsT=ones_f, rhs=Rflat[:, qd * 512:(qd + 1) * 512], start=True, stop=False)
                    pe("matmul", [BR], [Bpb[bk]], out=pb[bk], lhsT=ident_bf, rhs=negb4.rearrange("p h l -> p (h l)"), start=False, stop=True)
                    for hh in range(4):
                        h = qd * 4 + hh
                        act("activation", [Bdt], [Bpb[bk], Bdecay], out=decay[:, h, :], in_=pb[bk][:, hh * 128:(hh + 1) * 128], func=AF.Exp, bias=nacs_a[:, c, h:h + 1])
                    g = qd // 2
                    dve("tensor_tensor", [Bdecay, Bcb], [BMT[s]], out=MT[s][:, qd * 4:(qd + 1) * 4, :], in0=decay[:, qd * 4:(qd + 1) * 4, :],
                        in1=cb_sb[:, g, :].unsqueeze(1).to_broadcast([128, 4, 128]), op=ALU.mult)
                for g in range(2):
                    for cc in range(4):
                        pe("transpose", [Bxs[s]], [Bpb[3]], out=pb[3][:, cc * 128:(cc + 1) * 128], in_=xsT_c[s][:, g * 4 + cc, :], identity=identf)
                    act("activation", [], [Bpb[3], Bxssb], out=xs_sb[:, g * 8:(g + 1) * 8, :], in_=pb[3].rearrange("p (h d) -> p h d", d=64), func=AF.Copy)
                    hs = slice(g * 8, (g + 1) * 8)
                    dve("tensor_tensor", [Bxssb, Bdt], [Bxdt[s]], out=xdt[s][:, hs, :], in0=xs_sb[:, hs, :],
                        in1=dta[:, c, hs].unsqueeze(2).to_broadcast([128, 8, 64]), op=ALU.mult)
                    pool("tensor_tensor", [Bxssb, Bdt], [Bxw[s]], out=xw[s][:, hs, :], in0=xs_sb[:, hs, :],
                         in1=dtw_a[:, c, hs].unsqueeze(2).to_broadcast([128, 8, 64]), op=ALU.mult)
                    pool("tensor_tensor", [Bxssb], [BxD[s]], out=xD[s][:, hs, :], in0=xs_sb[:, hs, :],
                         in1=hp[:, 32 + g * 8:32 + (g + 1) * 8].unsqueeze(2).to_broadcast([128, 8, 64]), op=ALU.mult)

            def stB(c):
                s = c % 2
                for g in range(2):
                    hs = slice(g * 8, (g + 1) * 8)
                    for hh in range(8):
                        h = g * 8 + hh
                        pe("matmul", [BMT[s], Bxdt[s]], [Bpb[4]], out=pb[4][:, hh * 64:(hh + 1) * 64], lhsT=MT[s][:, h, :], rhs=xdt[s][:, h, :], start=True, stop=True)
                    pe("matmul", [Bbc[s], Bprevbf], [Bpb[5]], out=pb[5], lhsT=bcT_c[s][:, 2 + g, :], rhs=prevbf[:, hs, :].rearrange("p h d -> p (h d)"), start=True, stop=True)
                    pe("matmul", [BBtok[s], Bxw[s]], [Bpb[6]], out=pb[6], lhsT=Btok[s][:, g, :], rhs=xw[s][:, hs, :].rearrange("p h d -> p (h d)"), start=True, stop=True)
                    dve("tensor_tensor", [Bdt], [Bpb[5], Bt1], out=t1[:, hs, :], in0=pb[5].rearrange("p (h d) -> p h d", d=64),
                        in1=ea_a[:, c, hs].unsqueeze(2).to_broadcast([128, 8, 64]), op=ALU.mult)
                    dve("tensor_tensor", [], [Bpb[4], Bt1], out=t1[:, hs, :], in0=t1[:, hs, :], in1=pb[4].rearrange("p (h d) -> p h d", d=64), op=ALU.add)
                    pool("tensor_tensor", [BxD[s]], [Bt1], out=t1[:, hs, :], in0=t1[:, hs, :], in1=xD[s][:, hs, :], op=ALU.add)
                    pool("tensor_tensor", [Bt1, Bsz[s]], [Byg], out=yg[:, hs, :], in0=t1[:, hs, :], in1=sz_c[s][:, g * 512:(g + 1) * 512].rearrange("p (h d) -> p h d", d=64), op=ALU.mult)
                    act("activation", [Byg], [Bjunk4, Bss2], out=junk4, in_=yg[:, hs, :].rearrange("p h d -> p (h d)"), func=AF.Square, accum_out=ss2[:, g:g + 1])
                    dve("tensor_tensor", [Bdt], [Bprev], out=prev[:, hs, :], in0=prev[:, hs, :],
                        in1=cd_a[:, c, hs].unsqueeze(2).to_broadcast([128, 8, 64]), op=ALU.mult)
                    dve("tensor_tensor", [], [Bpb[6], Bprev], out=prev[:, hs, :], in0=prev[:, hs, :], in1=pb[6].rearrange("p (h d) -> p h d", d=64), op=ALU.add)
                    act("activation", [Bprev], [Bprevbf], out=prevbf[:, hs, :], in_=prev[:, hs, :], func=AF.Copy)
                pool("tensor_scalar", [], [Bss2], out=ss2, in0=ss2, scalar1=1.0 / 512, scalar2=EPS, op0=ALU.mult, op1=ALU.add)
                pool("tensor_tensor", [], [Bss2], out=ss2, in0=ss2, in1=mhalf[:, 0:2], op=ALU.pow)
                for g in range(2):
                    act("activation", [Byg, Bss2], [Byn], out=yn[:, g * 512:(g + 1) * 512], in_=yg[:, g * 8:(g + 1) * 8, :].rearrange("p h d -> p (h d)"),
                        func=AF.Copy, scale=ss2[:, g:g + 1])
                pbt7 = pb[7].bitcast(BF16)
                for cc in range(8):
                    pe("transpose", [Byn], [Bpb[7]], out=pbt7[:, cc * 128:(cc + 1) * 128], in_=yn[:, cc * 128:(cc + 1) * 128], identity=ident_bf)
                ys = (c // 4) % 2
                j = c % 4
                dve("tensor_tensor", [], [Bpb[7], Byst[ys]], out=ystage[ys][:, :, j * 128:(j + 1) * 128], in0=pbt7.rearrange("p (c t) -> p c t", t=128),
                    in1=pl["snwT"].unsqueeze(2).to_broadcast([128, 8, 128]), op=ALU.mult)
                if j == 3:
                    tg = c // 4
                    dma(f"yst{ys}", mixT_d[8:16, :, tg * 512:(tg + 1) * 512].rearrange("h p t -> p h t"), ystage[ys], reads=[Byst[ys]])

            p4_load(0)
            for t in range(NCH + 1):
                if t >= 1:
                    stB(t - 1)
                if t + 1 < NCH:
                    p4_load(t + 1)
                if t < NCH:
                    stA(t)
            S_.barrier()
            if stop_after == "P4":
                break

            AR.release(p3_mark)
            wo_bf = AR.alloc((16, D), BF16)
            Bwo = Buf("wo")
            wos = [AR.alloc((4, D), F32) for _ in range(2)]
            Bwos = [Buf("wos0"), Buf("wos1")]
            mixs = [AR.alloc((16, 512), BF16) for _ in range(2)]
            Bmix = [Buf("mix0"), Buf("mix1")]
            xr = [AR.alloc((D,), F32) for _ in range(2)]
            Bxr = [Buf("xr0"), Buf("xr1")]
            ot = [AR.alloc((D,), F32) for _ in range(2)]
            Bot = [Buf("ot0"), Buf("ot1")]
            wo_l = w_out[l]
            for q4 in range(4):
                s = q4 % 2
                dma(f"wos{s}", wos[s], wo_l[q4 * 512:(q4 + 1) * 512, :].rearrange("(k p) n -> p k n", p=128), writes=[Bwos[s]])
                pool("tensor_copy", [Bwos[s]], [Bwo], out=wo_bf[:, q4 * 4:(q4 + 1) * 4, :], in_=wos[s])

            def p5_loadmix(tg):
                s = tg % 2
                dma(f"mix{s}", mixs[s], mixT_d[:, :, tg * 512:(tg + 1) * 512].rearrange("c p t -> p c t"), writes=[Bmix[s]])

            def p5_loadx(i):
                s = i % 2
                dma(f"xr{s}", xr[s], xsrc[i * 128:(i + 1) * 128, :], writes=[Bxr[s]])

            def p5_tile(i):
                s = i % 2
                tg = i // 4
                j = i % 4
                ms = tg % 2
                for half in range(2):
                    bk = (2 * i + half) % 4
                    for cch in range(16):
                        pe("matmul", [Bmix[ms], Bwo], [Bpb[bk]], out=pb[bk], lhsT=mixs[ms][:, cch, j * 128:(j + 1) * 128], rhs=wo_bf[:, cch, half * 512:(half + 1) * 512],
                           start=(cch == 0), stop=(cch == 15))
                    dve("tensor_tensor", [Bxr[s]], [Bpb[bk], Bot[s]], out=ot[s][:, half * 512:(half + 1) * 512], in0=pb[bk], in1=xr[s][:, half * 512:(half + 1) * 512], op=ALU.add)
                dma(f"ot{s}", xdst[i * 128:(i + 1) * 128, :], ot[s], reads=[Bot[s]])

            p5_loadmix(0)
            p5_loadx(0)
            for i in range(NT):
                if i % 4 == 0 and i // 4 + 1 < TG:
                    p5_loadmix(i // 4 + 1)
                if i + 1 < NT:
                    p5_loadx(i + 1)
                p5_tile(i)
            S_.barrier()
            if stop_after == "L0":
                break

        S_.barrier()
        S_.finalize()
        with nc.Block() as block:
            @block.sync
            def _(e):
                S_.replay("sp")

            @block.tensor
            def _(e):
                S_.replay("pe")

            @block.vector
            def _(e):
                S_.replay("dve")

            @block.scalar
            def _(e):
                S_.replay("act")

            @block.gpsimd
            def _(e):
                S_.replay("pool")
    return nc


def host_inputs(inputs, b, S, depth=2):
    f = np.float32
    d = {}
    d["x"] = np.ascontiguousarray(inputs["x"][b, :S]).astype(f)
    d["w_in"] = np.ascontiguousarray(inputs["w_in"]).astype(f)
    d["w_out"] = np.ascontiguousarray(inputs["w_out"]).astype(f)
    d["lnwT"] = np.ascontiguousarray(inputs["ln_w"].reshape(depth, 8, 128).transpose(0, 2, 1)).astype(f)
    cw = inputs["conv_w"].reshape(depth, 4, 12, 128).transpose(0, 3, 2, 1)
    d["cwT"] = np.ascontiguousarray(cw.reshape(depth, 128, 48)).astype(f)
    d["cbT"] = np.ascontiguousarray(inputs["conv_b"].reshape(depth, 12, 128).transpose(0, 2, 1)).astype(f)
    d["qkw"] = np.ascontiguousarray(np.stack([inputs["q_norm_w"], inputs["k_norm_w"]], axis=-1)).astype(f)
    d["snwT"] = np.ascontiguousarray(inputs["ssd_norm_w"].reshape(depth, 8, 128).transpose(0, 2, 1)).astype(f)
    d["hp"] = np.ascontiguousarray(np.concatenate([inputs["dt_bias"], inputs["a_log"], inputs["d_skip"]], axis=-1).reshape(depth, 1, 48)).astype(f)
    d["consts"] = make_consts()
    return d


BATCH = 8
SEQ = 4096


def kernel(**inputs):
    inputs = {k: np.asarray(v) for k, v in inputs.items()}
    nc = build(SEQ, depth=2, dbg=False)
    in_maps = [host_inputs(inputs, b, SEQ) for b in range(BATCH)]
    res = run_bass_kernel_spmd(nc, in_maps, core_ids=list(range(BATCH)))
    out = np.stack([np.asarray(r["out"], dtype=np.float32) for r in res.results], axis=0)
    return out
```

```python
import contextlib
import numpy as np
import concourse.bass as bass
import concourse.mybir as mybir
from concourse.bass_utils import run_bass_kernel_spmd

F32 = mybir.dt.float32
BF16 = mybir.dt.bfloat16
AF = mybir.ActivationFunctionType
ALU = mybir.AluOpType
AX = mybir.AxisListType

ENGS = ("pe", "act", "dve", "pool", "sp")
SAME_ENGINE_SYNC = {"pe": False, "act": True, "dve": True, "pool": True, "sp": False}


class Ev:
    __slots__ = ("eng", "idx", "sem", "val", "needed")

    def __init__(self, eng, idx, sem=None, val=None):
        self.eng = eng
        self.idx = idx
        self.sem = sem
        self.val = val
        self.needed = False


def ev_is_dma(ev):
    return ev.sem is not None and not isinstance(ev.sem, tuple)


class Buf:
    __slots__ = ("name", "w", "rs")

    def __init__(self, name=""):
        self.name = name
        self.w = None
        self.rs = []


class Op:
    __slots__ = ("fn", "waits", "ev", "is_dma")

    def __init__(self, fn, waits, ev, is_dma):
        self.fn = fn
        self.waits = waits
        self.ev = ev
        self.is_dma = is_dma


class Sched:
    def __init__(self, nc):
        self.nc = nc
        self.ops = {e: [] for e in ENGS}
        self.handles = {"pe": nc.tensor, "act": nc.scalar, "dve": nc.vector, "pool": nc.gpsimd, "sp": nc.sync}
        self.esem = {}
        self.dma_sems = {}
        self.dma_cnt = {}
        self.all_dma_ev = {}

    def _deps(self, eng, reads, writes, xreads=()):
        out = []

        def add(ev, raw):
            if ev.eng == eng and not ev_is_dma(ev):
                if not (raw and SAME_ENGINE_SYNC[eng]):
                    return
            out.append(ev)

        for b in reads:
            if b.w is not None:
                add(b.w, True)
        for b in xreads:
            if b.w is not None:
                add(b.w, True)
            for r in b.rs:
                add(r, False)
        for b in writes:
            if b.w is not None:
                add(b.w, False)
            for r in b.rs:
                add(r, False)
        return out

    def _upd(self, ev, reads, writes):
        for b in reads:
            b.rs.append(ev)
        for b in writes:
            b.w = ev
            b.rs = []

    def op(self, eng, fn, reads=(), writes=(), xreads=()):
        waits = self._deps(eng, reads, writes, xreads)
        ev = Ev(eng, len(self.ops[eng]))
        self.ops[eng].append(Op(fn, waits, ev, False))
        self._upd(ev, list(reads) + list(xreads), writes)
        return ev

    def dma(self, eng, semname, out, in_, reads=(), writes=(), **kw):
        waits = self._deps(eng, reads, writes)
        self.dma_cnt[semname] += 16
        ev = Ev(eng, len(self.ops[eng]), sem=semname, val=self.dma_cnt[semname])
        h = self.handles[eng]
        fn = (lambda h=h, out=out, in_=in_, kw=kw: h.dma_start(out=out, in_=in_, **kw))
        self.ops[eng].append(Op(fn, waits, ev, True))
        self.all_dma_ev[semname] = ev
        self._upd(ev, reads, writes)
        return ev

    def barrier(self):
        lasts = []
        for e in ENGS:
            for o in reversed(self.ops[e]):
                if not o.is_dma and o.fn is not None:
                    lasts.append(o.ev)
                    break
        lasts.extend(self.all_dma_ev.values())
        for e in ENGS:
            waits = [ev for ev in lasts if not (ev.eng == e and not ev_is_dma(ev))]
            ev = Ev(e, len(self.ops[e]))
            self.ops[e].append(Op(None, waits, ev, False))

    def finalize(self):
        for e in ENGS:
            for o in self.ops[e]:
                for ev in o.waits:
                    ev.needed = True
        for e in ENGS:
            cnt = 0
            for o in self.ops[e]:
                if o.is_dma:
                    continue
                if o.ev.needed:
                    assert o.fn is not None
                    cnt += 1
                    o.ev.sem = ("E", e)
                    o.ev.val = cnt

    def _sem(self, key):
        if isinstance(key, tuple):
            return self.esem[key[1]]
        return self.dma_sems[key]

    def replay(self, eng):
        h = self.handles[eng]
        seen = {}
        for o in self.ops[eng]:
            need = {}
            for ev in o.waits:
                if need.get(ev.sem, 0) < ev.val:
                    need[ev.sem] = ev.val
            for k, v in need.items():
                if seen.get(k, 0) < v:
                    h.wait_ge(self._sem(k), v)
                    seen[k] = v
            if o.fn is None:
                continue
            ins = o.fn()
            if o.is_dma:
                ins.then_inc(self._sem(o.ev.sem), 16)
            elif o.ev.needed:
                ins.then_inc(self._sem(o.ev.sem), 1)


class Arena:
    def __init__(self, base, nwords):
        self.base = base
        self.cap = nwords * 4
        self.off = 0

    def mark(self):
        return self.off

    def release(self, m):
        self.off = m

    def alloc(self, shape, dt):
        n = int(np.prod(shape))
        esz = 4 if dt == F32 else 2
        nbytes = (n * esz + 63) // 64 * 64
        st = self.off
        self.off += nbytes
        assert self.off <= self.cap, f"SBUF arena overflow {self.off} > {self.cap}"
        w0 = st // 4
        if dt == F32:
            v = self.base[:, w0:w0 + n]
        else:
            v = self.base[:, w0:w0 + nbytes // 4].bitcast(BF16)[:, 0:n]
        if len(shape) == 2:
            v = v.rearrange("p (a b) -> p a b", a=shape[0], b=shape[1])
        elif len(shape) == 3:
            v = v.rearrange("p (a b c) -> p a b c", a=shape[0], b=shape[1], c=shape[2])
        return v


D = 1024
KC = 8
NPROJ = 6672
EPS = 1e-6
NEGBIG = -30000.0

C_IDENT = 0
C_U = 128
C_NEGU = 256
C_NEG = 384
C_ONES = 512
C_BH = 640
C_BIAS = C_BH + 2048
C_G = C_BIAS + 240
C_PAST = C_G + 16
C_E = C_PAST + 256
NCONST = C_E + 2048


def make_consts():
    c = np.zeros((128, NCONST), np.float32)
    p = np.arange(128)
    c[:, C_IDENT:C_IDENT + 128] = np.eye(128)
    U = (p[:, None] <= p[None, :]).astype(np.float32)
    c[:, C_U:C_U + 128] = U
    c[:, C_NEGU:C_NEGU + 128] = -U
    c[:, C_NEG:C_NEG + 128] = np.where(p[None, :] < p[:, None], NEGBIG, 0.0)
    c[:, C_ONES:C_ONES + 128] = 1.0
    slopes = 2.0 ** (-8.0 * np.arange(1, 9) / 8.0)
    qrel = np.arange(256)
    for h in range(8):
        d = qrel[None, :] - p[:, None]
        c[:, C_BH + h * 256:C_BH + (h + 1) * 256] = np.where(d >= 0, -slopes[h] * d, NEGBIG)
        for dd in range(1, 16):
            for kt in range(2):
                c[:, C_BIAS + h * 30 + (dd - 1) * 2 + kt] = -slopes[h] * (dd * 256 - kt * 128 - p)
        for hf in range(2):
            c[:, C_G + h * 2 + hf] = np.exp(-slopes[h] * (hf * 128 + p))
    for qb in range(16):
        for n in range(16):
            c[:, C_PAST + qb * 16 + n] = 0.0 if n < qb else -1e30
    for n in range(16):
        c[n, C_E + n * 128:C_E + (n + 1) * 128] = 1.0
    return c


def pipeline(n, stages):
    maxlag = max(l for l, _ in stages)
    for t in range(n + maxlag):
        for lag, fn in stages:
            i = t - lag
            if 0 <= i < n:
                fn(i)


def build(S, depth=2, dbg=False, stop_after=None):
    NT = S // 128
    NB = S // 256
    TG = S // 512
    assert S % 512 == 0 and NB <= 16
    nc = bass.Bass("TRN2", target_bir_lowering=False)
    V, A, P, T = nc.vector, nc.scalar, nc.gpsimd, nc.tensor

    def din(name, shape, dt=F32):
        return nc.dram_tensor(name, list(shape), dt, kind="ExternalInput").ap()

    def dscr(name, shape, dt):
        kind = "ExternalOutput" if dbg else "Internal"
        return nc.dram_tensor(name, list(shape), dt, kind=kind).ap()

    x_in = din("x", [S, D])
    w_in = din("w_in", [depth, D, NPROJ])
    w_out = din("w_out", [depth, 2 * D, D])
    lnwT_d = din("lnwT", [depth, 128, 8])
    cwT_d = din("cwT", [depth, 128, 12 * 4])
    cbT_d = din("cbT", [depth, 128, 12])
    qkw_d = din("qkw", [depth, 128, 2])
    snwT_d = din("snwT", [depth, 128, 8])
    hp_d = din("hp", [depth, 1, 48])
    consts_d = din("consts", [128, NCONST])
    out_d = nc.dram_tensor("out", [S, D], F32, kind="ExternalOutput").ap()

    qT_d = dscr("qT_s", [8, 128, S], BF16)
    kT_d = dscr("kT_s", [8, 128, S], BF16)
    V_d = dscr("V_s", [S, D], BF16)
    sg_d = dscr("sg_s", [S, D], F32)
    sz_d = dscr("sz_s", [S, D], F32)
    xsT_d = dscr("xsT_s", [D, S], F32)
    bcT_d = dscr("bcT_s", [512, S], BF16)
    mixT_d = dscr("mixT_s", [16, 128, S], BF16)
    xres_d = dscr("xres_s", [S, D], F32)
    dtraw_d = dscr("dtraw_s", [128, NT * 16], F32) if dbg else None
    hT_d = dscr("hT_s", [128, KC * S], BF16) if dbg else None

    ARENA_WORDS = 51200
    with contextlib.ExitStack() as st:
        arena_t = st.enter_context(nc.sbuf_tensor("arena", [128, ARENA_WORDS], F32))
        AR = Arena(arena_t[:, :], ARENA_WORDS)
        pbanks = [st.enter_context(nc.psum_tensor(f"pb{i}", [128, 512], F32)) for i in range(8)]
        pb = [t[:, :] for t in pbanks]
        Bpb = [Buf(f"pb{i}") for i in range(8)]
        S_ = Sched(nc)
        S_.esem = {e: st.enter_context(nc.semaphore("sem_" + e)) for e in ENGS}
        sem_pool = {}

        def dsem(name):
            if name not in sem_pool:
                sem_pool[name] = st.enter_context(nc.semaphore("d_" + name))
                S_.dma_sems[name] = sem_pool[name]
                S_.dma_cnt[name] = 0
            return name

        def eop(eng, h, fname, reads, writes, **kw):
            f = getattr(h, fname)
            xr = ()
            if eng != "pe":
                xr = [b for b in writes if b in Bpb]
                writes = [b for b in writes if b not in Bpb]
                reads = list(reads) + [b for b in writes if b not in reads]
            return S_.op(eng, (lambda f=f, kw=kw: f(**kw)), reads, writes, xr)

        def dve(fname, reads, writes, **kw):
            return eop("dve", V, fname, reads, writes, **kw)

        def act(fname, reads, writes, **kw):
            return eop("act", A, fname, reads, writes, **kw)

        def pool(fname, reads, writes, **kw):
            return eop("pool", P, fname, reads, writes, **kw)

        def pe(fname, reads, writes, **kw):
            return eop("pe", T, fname, reads, writes, **kw)

        def dma(semname, out, in_, reads=(), writes=(), eng="sp"):
            return S_.dma(eng, dsem(semname), out, in_, reads, writes)

        cst = AR.alloc((NCONST,), F32)
        Bcst = Buf("cst")
        ident_bf = AR.alloc((128,), BF16)
        bh_bf = AR.alloc((8, 256), BF16)
        e_bf = AR.alloc((16, 128), BF16)
        neg_bf = AR.alloc((128,), BF16)
        mhalf = AR.alloc((8,), F32)
        Bk = Buf("constbf")
        par = {}
        Bpar = Buf("par")
        for l in range(depth):
            par[l] = dict(
                lnwT=AR.alloc((8,), F32), cwT=AR.alloc((12, 4), F32), cbT=AR.alloc((12,), F32),
                qkw=AR.alloc((2,), F32), snwT=AR.alloc((8,), F32), hp=AR.alloc((48,), F32),
                qws=AR.alloc((1,), F32), aneg=AR.alloc((16,), F32))
        dtraw = AR.alloc((NT, 16), F32)
        Bdtraw = Buf("dtraw")

        identf = cst[:, C_IDENT:C_IDENT + 128]

        dma("cst", cst, consts_d[:, :], writes=[Bcst])
        for l in range(depth):
            pl = par[l]
            dma("par", pl["lnwT"], lnwT_d[l], writes=[Bpar])
            dma("par", pl["cwT"], cwT_d[l].rearrange("p (c k) -> p c k", k=4), writes=[Bpar])
            dma("par", pl["cbT"], cbT_d[l], writes=[Bpar])
            dma("par", pl["qkw"], qkw_d[l], writes=[Bpar])
            dma("par", pl["snwT"], snwT_d[l], writes=[Bpar])
            dma("par", pl["hp"], hp_d[l].partition_broadcast(128), writes=[Bpar])
        pool("tensor_copy", [Bcst], [Bk], out=ident_bf, in_=identf)
        pool("tensor_copy", [Bcst], [Bk], out=bh_bf, in_=cst[:, C_BH:C_BH + 2048].rearrange("p (h q) -> p h q", q=256))
        pool("tensor_copy", [Bcst], [Bk], out=e_bf, in_=cst[:, C_E:C_E + 2048].rearrange("p (n k) -> p n k", k=128))
        pool("tensor_copy", [Bcst], [Bk], out=neg_bf, in_=cst[:, C_NEG:C_NEG + 128])
        pool("memset", [], [Bk], ap=mhalf, constant=-0.5)
        for l in range(depth):
            pl = par[l]
            pool("tensor_scalar", [Bpar], [Bpar], out=pl["qws"], in0=pl["qkw"][:, 0:1], scalar1=float(128 ** -0.5), scalar2=None, op0=ALU.mult)
            act("activation", [Bpar], [Bpar], out=pl["aneg"], in_=pl["hp"][:, 16:32], func=AF.Exp)
            pool("tensor_scalar", [Bpar], [Bpar], out=pl["aneg"], in0=pl["aneg"], scalar1=-1.0, scalar2=None, op0=ALU.mult)
        S_.barrier()

        base_mark = AR.mark()
        p3_mark = base_mark

        for l in range(depth):
            pl = par[l]
            xsrc = x_in if l == 0 else xres_d
            xdst = xres_d if l < depth - 1 else out_d
            AR.release(base_mark)
            hT = AR.alloc((KC, S), BF16)
            BhT = Buf("hT")
            m1 = AR.mark()
            xt = [AR.alloc((D,), F32) for _ in range(2)]
            Bxt = [Buf("xt0"), Buf("xt1")]
            xn = [AR.alloc((D,), BF16) for _ in range(2)]
            Bxn = [Buf("xn0"), Buf("xn1")]
            junk = AR.alloc((D,), BF16)
            Bjunk = Buf("junk")
            ss1 = [AR.alloc((1,), F32) for _ in range(2)]
            Bss1 = [Buf("ss0"), Buf("ss1")]

            def p1_load(i):
                s = i % 2
                dma(f"xt{s}", xt[s], xsrc[i * 128:(i + 1) * 128, :], writes=[Bxt[s]])

            def p1_norm(i):
                s = i % 2
                act("activation", [Bxt[s]], [Bjunk, Bss1[s]], out=junk, in_=xt[s], func=AF.Square, accum_out=ss1[s])
                pool("tensor_scalar", [Bss1[s]], [Bss1[s]], out=ss1[s], in0=ss1[s], scalar1=1.0 / D, scalar2=EPS, op0=ALU.mult, op1=ALU.add)
                pool("tensor_tensor", [Bss1[s], Bk], [Bss1[s]], out=ss1[s], in0=ss1[s], in1=mhalf[:, 0:1], op=ALU.pow)
                dve("tensor_scalar", [Bxt[s], Bss1[s]], [Bxn[s]], out=xn[s], in0=xt[s], scalar1=ss1[s][:, 0:1], scalar2=None, op0=ALU.mult)

            def p1_tr(i):
                s = i % 2
                bk = 6 + (i % 2)
                pbt = pb[bk].bitcast(BF16)
                for c in range(KC):
                    pe("transpose", [Bxn[s], Bk], [Bpb[bk]], out=pbt[:, c * 128:(c + 1) * 128], in_=xn[s][:, c * 128:(c + 1) * 128], identity=ident_bf)
                dve("tensor_tensor", [Bpar], [Bpb[bk], BhT], out=hT[:, :, i * 128:(i + 1) * 128],
                    in0=pbt.rearrange("p (c t) -> p c t", t=128),
                    in1=pl["lnwT"].unsqueeze(2).to_broadcast([128, KC, 128]), op=ALU.mult)

            p1_load(0)
            for t in range(NT + 1):
                if t + 1 < NT:
                    p1_load(t + 1)
                if t < NT:
                    p1_norm(t)
                if t >= 1:
                    p1_tr(t - 1)
            if dbg and l == 0:
                dma("dbg", hT_d[:, :], hT.rearrange("p a b -> p (a b)"), reads=[BhT])
            if stop_after == "P1":
                break

            wst = [AR.alloc((KC, 512), F32) for _ in range(2)]
            Bwst = [Buf("wst0"), Buf("wst1")]
            wbf = [AR.alloc((KC, 512), BF16) for _ in range(2)]
            Bwbf = [Buf("wbf0"), Buf("wbf1")]
            NG = 14
            w_l = w_in[l]

            def gcols(g):
                c0 = g * 512
                return c0, min(512, NPROJ - c0)

            def wload(g):
                s = g % 2
                c0, ncl = gcols(g)
                dma(f"wst{s}", wst[s][:, :, 0:ncl], w_l[:, c0:c0 + ncl].rearrange("(k p) n -> p k n", p=128), writes=[Bwst[s]])

            def wconv(g):
                s = g % 2
                c0, ncl = gcols(g)
                pool("tensor_copy", [Bwst[s]], [Bwbf[s]], out=wbf[s][:, :, 0:ncl], in_=wst[s][:, :, 0:ncl])

            ss4 = [AR.alloc((4,), F32) for _ in range(2)]
            Bss4 = [Buf("ss4a"), Buf("ss4b")]
            sq = [AR.alloc((512,), F32) for _ in range(2)]
            Bsq = [Buf("sq0"), Buf("sq1")]
            qn = [AR.alloc((4, 128), BF16) for _ in range(2)]
            Bqn = [Buf("qn0"), Buf("qn1")]
            qst = [AR.alloc((4, 512), BF16) for _ in range(2)]
            Bqst = [Buf("qst0"), Buf("qst1")]
            vst = [AR.alloc((512,), BF16) for _ in range(2)]
            Bvst = [Buf("vst0"), Buf("vst1")]
            fst = [AR.alloc((512,), F32) for _ in range(2)]
            Bfst = [Buf("fst0"), Buf("fst1")]
            ubuf = [AR.alloc((515,), F32) for _ in range(2)]
            Bu = [Buf("u0"), Buf("u1")]
            cacc = [AR.alloc((512,), F32) for _ in range(2)]
            Bcacc = [Buf("cacc0"), Buf("cacc1")]
            xo_f = [AR.alloc((512,), F32) for _ in range(2)]
            xo_b = [AR.alloc((512,), BF16) for _ in range(2)]
            Bxo = [Buf("xo0"), Buf("xo1")]
            accn = [0]

            def next_acc():
                b = accn[0] % 4
                accn[0] += 1
                return b

            def mm_tok(g, i, ncl, bk):
                s = g % 2
                for k in range(KC):
                    pe("matmul", [BhT, Bwbf[s]], [Bpb[bk]], out=pb[bk][:, 0:ncl], lhsT=hT[:, k, i * 128:(i + 1) * 128],
                       rhs=wbf[s][:, k, 0:ncl], start=(k == 0), stop=(k == KC - 1))

            def do_qk(g):
                which = 0 if g < 2 else 1
                h0 = (g % 2) * 4
                dst = qT_d if which == 0 else kT_d
                wsc = pl["qws"] if which == 0 else pl["qkw"][:, 1:2]
                banks = {}

                def s0(i):
                    bk = next_acc()
                    banks[i] = bk
                    mm_tok(g, i, 512, bk)
                    s = i % 2
                    act("activation", [], [Bpb[bk], Bsq[s]], out=sq[s], in_=pb[bk], func=AF.Square)
                    dve("tensor_reduce", [Bsq[s]], [Bss4[s]], out=ss4[s], in_=sq[s].rearrange("p (h d) -> p h d", d=128), axis=AX.X, op=ALU.add)
                    pool("tensor_scalar", [], [Bss4[s]], out=ss4[s], in0=ss4[s], scalar1=1.0 / 128, scalar2=EPS, op0=ALU.mult, op1=ALU.add)
                    pool("tensor_tensor", [Bk], [Bss4[s]], out=ss4[s], in0=ss4[s], in1=mhalf[:, 0:4], op=ALU.pow)
                    dve("tensor_tensor", [Bss4[s]], [Bpb[bk], Bqn[s]], out=qn[s], in0=pb[bk].rearrange("p (h d) -> p h d", d=128),
                        in1=ss4[s].unsqueeze(2).to_broadcast([128, 4, 128]), op=ALU.mult)

                def s2(i):
                    s = i % 2
                    bk = 4 + (i % 2)
                    pbt = pb[bk].bitcast(BF16)
                    for hh in range(4):
                        pe("transpose", [Bqn[s], Bk], [Bpb[bk]], out=pbt[:, hh * 128:(hh + 1) * 128], in_=qn[s][:, hh, :], identity=ident_bf)
                    ss_ = (i // 4) % 2
                    j = i % 4
                    act("activation", [Bpar], [Bpb[bk], Bqst[ss_]], out=qst[ss_][:, :, j * 128:(j + 1) * 128],
                        in_=pbt[:, 0:512].rearrange("p (h t) -> p h t", t=128), func=AF.Copy, scale=wsc)
                    if j == 3:
                        tg = i // 4
                        dma(f"qst{ss_}", dst[h0:h0 + 4, :, tg * 512:(tg + 1) * 512].rearrange("h p t -> p h t"), qst[ss_], reads=[Bqst[ss_]])

                pipeline(NT, [(2, s2), (0, s0)])

            def do_tok(g, kind):
                c0 = (g % 2) * 512

                def s0(i):
                    bk = next_acc()
                    mm_tok(g, i, 512, bk)
                    s = i % 2
                    if kind == "v":
                        act("activation", [], [Bpb[bk], Bvst[s]], out=vst[s], in_=pb[bk], func=AF.Copy)
                        dma(f"vst{s}", V_d[i * 128:(i + 1) * 128, c0:c0 + 512], vst[s], reads=[Bvst[s]])
                    else:
                        act("activation", [], [Bpb[bk], Bfst[s]], out=fst[s], in_=pb[bk], func=AF.Silu)
                        dd = sg_d if kind == "g" else sz_d
                        dma(f"fst{s}", dd[i * 128:(i + 1) * 128, c0:c0 + 512], fst[s], reads=[Bfst[s]])

                pipeline(NT, [(0, s0)])

            def do_feat(g):
                s_w = g % 2
                nloc = 4
                items = [(cl, tg) for cl in range(nloc) for tg in range(TG)]

                def s0(ix):
                    cl, tg = items[ix]
                    cc = (g - 10) * 4 + cl
                    bk = next_acc()
                    for k in range(KC):
                        pe("matmul", [BhT, Bwbf[s_w]], [Bpb[bk]], out=pb[bk], lhsT=wbf[s_w][:, k, cl * 128:(cl + 1) * 128],
                           rhs=hT[:, k, tg * 512:(tg + 1) * 512], start=(k == 0), stop=(k == KC - 1))
                    s = ix % 2
                    if tg == 0:
                        pool("memset", [], [Bu[s]], ap=ubuf[s][:, 0:3], constant=0.0)
                    else:
                        pool("tensor_copy", [Bu[1 - s]], [Bu[s]], out=ubuf[s][:, 0:3], in_=ubuf[1 - s][:, 512:515])
                    act("activation", [], [Bpb[bk], Bu[s]], out=ubuf[s][:, 3:515], in_=pb[bk], func=AF.Copy)
                    cw = pl["cwT"]
                    dve("tensor_scalar", [Bu[s], Bpar], [Bcacc[s]], out=cacc[s], in0=ubuf[s][:, 3:515], scalar1=cw[:, cc, 3:4], scalar2=None, op0=ALU.mult)
                    for kk in (2, 1, 0):
                        dve("scalar_tensor_tensor", [Bu[s], Bpar], [Bcacc[s]], out=cacc[s], in0=ubuf[s][:, kk:kk + 512], scalar=cw[:, cc, kk:kk + 1],
                            in1=cacc[s], op0=ALU.mult, op1=ALU.add)
                    if cc < 8:
                        act("activation", [Bcacc[s], Bpar], [Bxo[s]], out=xo_f[s], in_=cacc[s], func=AF.Silu, bias=pl["cbT"][:, cc:cc + 1])
                        dma(f"xo{s}", xsT_d[cc * 128:(cc + 1) * 128, tg * 512:(tg + 1) * 512], xo_f[s], reads=[Bxo[s]])
                    else:
                        act("activation", [Bcacc[s], Bpar], [Bxo[s]], out=xo_b[s], in_=cacc[s], func=AF.Silu, bias=pl["cbT"][:, cc:cc + 1])
                        dma(f"xo{s}", bcT_d[(cc - 8) * 128:(cc - 7) * 128, tg * 512:(tg + 1) * 512], xo_b[s], reads=[Bxo[s]])

                pipeline(len(items), [(0, s0)])

            def do_dt(g):
                def s0(i):
                    bk = next_acc()
                    mm_tok(g, i, 16, bk)
                    dve("tensor_copy", [], [Bpb[bk], Bdtraw], out=dtraw[:, i, :], in_=pb[bk][:, 0:16])
                pipeline(NT, [(0, s0)])

            wload(0)
            wconv(0)
            wload(1)
            for g in range(NG):
                if g + 1 < NG:
                    wconv(g + 1)
                if g + 2 < NG:
                    wload(g + 2)
                if g < 4:
                    do_qk(g)
                elif g < 6:
                    do_tok(g, "v")
                elif g < 8:
                    do_tok(g, "g")
                elif g < 10:
                    do_tok(g, "z")
                elif g < 13:
                    do_feat(g)
                else:
                    do_dt(g)
            if dbg:
                dma("dbg", dtraw_d[:, :], dtraw.rearrange("p a b -> p (a b)"), reads=[Bdtraw])
            S_.barrier()
            if stop_after == "P2":
                break
            AR.release(p3_mark)
            qT_h = [AR.alloc((S,), BF16) for _ in range(2)]
            kT_h = [AR.alloc((S,), BF16) for _ in range(2)]
            V_h = [AR.alloc((NT, 129), BF16) for _ in range(2)]
            sg_h = [AR.alloc((NT, 128), F32) for _ in range(2)]
            Bq = [Buf("q0"), Buf("q1")]
            Bkk = [Buf("k0"), Buf("k1")]
            Bv = [Buf("v0"), Buf("v1")]
            Bsg = [Buf("sg0"), Buf("sg1")]
            attnT = [AR.alloc((S,), BF16) for _ in range(2)]
            Battn = [Buf("at0"), Buf("at1")]
            maskT = [AR.alloc((S,), BF16) for _ in range(2)]
            Bmask = [Buf("mk0"), Buf("mk1")]
            km = AR.alloc((16,), F32)
            kms = AR.alloc((16,), F32)
            kmh = [AR.alloc((16,), BF16) for _ in range(2)]
            kml = [AR.alloc((16,), BF16) for _ in range(2)]
            Bkm = Buf("km")
            Bkmhl = [Buf("kmhl0"), Buf("kmhl1")]
            gm = [AR.alloc((16,), F32) for _ in range(2)]
            top8 = [AR.alloc((8,), F32) for _ in range(2)]
            thr = [AR.alloc((1,), F32) for _ in range(2)]
            mk = [AR.alloc((16,), BF16) for _ in range(2)]
            Bgm = [Buf("gm0"), Buf("gm1")]
            Bmkk = [Buf("mkk0"), Buf("mkk1")]
            NPT = 6
            PT = [AR.alloc((512,), BF16) for _ in range(NPT)]
            BPT = [Buf(f"pt{i}") for i in range(NPT)]
            tsc = [AR.alloc((129,), F32) for _ in range(2)]
            num = [AR.alloc((129,), F32) for _ in range(2)]
            rden = [AR.alloc((1,), F32) for _ in range(2)]
            o_bf = [AR.alloc((128,), BF16) for _ in range(2)]
            Bcmb = [Buf("cmb0"), Buf("cmb1")]
            Bobf = [Buf("obf0"), Buf("obf1")]

            pool("memset", [], [Bkm], ap=km, constant=0.0)
            for s in range(2):
                pool("memset", [], [Bv[s]], ap=V_h[s][:, :, 128:129], constant=1.0)
                pool("memset", [], [Bmask[s]], ap=maskT[s], constant=0.0)

            def head_load(h):
                s = h % 2
                dma(f"hq{s}", qT_h[s], qT_d[h], writes=[Bq[s]])
                dma(f"hk{s}", kT_h[s], kT_d[h], writes=[Bkk[s]])
                dma(f"hv{s}", V_h[s][:, :, 0:128], V_d[:, h * 128:(h + 1) * 128].rearrange("(t p) d -> p t d", p=128), writes=[Bv[s]])
                dma(f"hg{s}", sg_h[s], sg_d[:, h * 128:(h + 1) * 128].rearrange("(t p) d -> p t d", p=128), writes=[Bsg[s]])

            def head_kmean(h):
                s = h % 2
                dve("tensor_reduce", [Bkk[s]], [Bkm], out=km[:, 0:NB], in_=kT_h[s].rearrange("p (n k) -> p n k", k=256), axis=AX.X, op=ALU.add)
                dve("tensor_scalar", [], [Bkm], out=kms, in0=km, scalar1=1.0 / 256, scalar2=None, op0=ALU.mult)
                dve("tensor_copy", [Bkm], [Bkmhl[s]], out=kmh[s], in_=kms)
                dve("tensor_tensor", [Bkm], [Bkmhl[s]], out=kml[s], in0=kms, in1=kmh[s], op=ALU.subtract)

            def gate_front(h, qb):
                s = h % 2
                for j in range(2):
                    i = 2 * qb + j
                    pe("matmul", [Bq[s], Bkmhl[s]], [Bpb[6]], out=pb[6][:, j * 16:(j + 1) * 16], lhsT=qT_h[s][:, i * 128:(i + 1) * 128], rhs=kmh[s], start=True, stop=False)
                    pe("matmul", [Bq[s], Bkmhl[s]], [Bpb[6]], out=pb[6][:, j * 16:(j + 1) * 16], lhsT=qT_h[s][:, i * 128:(i + 1) * 128], rhs=kml[s], start=False, stop=True)
                for j in range(2):
                    dve("tensor_tensor", [], [Bpb[6], Bgm[j]], out=gm[j], in0=pb[6][:, j * 16:(j + 1) * 16], in1=cst[:, C_PAST + qb * 16:C_PAST + (qb + 1) * 16], op=ALU.add)
                    dve("max", [], [Bgm[j]], out=top8[j], in_=gm[j])
                    dve("tensor_scalar", [], [Bgm[j]], out=thr[j], in0=top8[j][:, 2:3], scalar1=-1e29, scalar2=None, op0=ALU.max)
                    dve("tensor_scalar", [Bgm[j]], [Bmkk[j]], out=mk[j], in0=gm[j], scalar1=thr[j][:, 0:1], scalar2=NEGBIG, op0=ALU.is_lt, op1=ALU.mult)

            def gate_back(h, qb):
                s = h % 2
                pbt = pb[7].bitcast(BF16)
                for j in range(2):
                    pe("transpose", [Bmkk[j]], [Bpb[7]], out=pbt[0:16, j * 128:(j + 1) * 128], in_=mk[j], identity=ident_bf)
                dve("tensor_copy", [], [Bpb[7], Bmask[s]], out=maskT[s][0:16, qb * 256:(qb + 1) * 256], in_=pbt[0:16, 0:256])

            deferred = []

            def defer(n, fn):
                deferred.append([n, fn])

            def tick():
                for d_ in list(deferred):
                    d_[0] -= 1
                    if d_[0] <= 0:
                        deferred.remove(d_)
                        d_[1]()

            def flush():
                while deferred:
                    d_ = deferred.pop(0)
                    d_[1]()

            def attn_head(h, first, last_head):
                s = h % 2
                NP = S // 512
                items = []
                for p in range(NP):
                    for kt in range(4 * p):
                        items.append(("pc", p, kt))
                    items.append(("own0", 2 * p, 4 * p))
                    items.append(("own1", 2 * p, 4 * p + 1))
                    items.append(("po", p, 4 * p))
                    items.append(("po", p, 4 * p + 1))
                    items.append(("own0", 2 * p + 1, 4 * p + 2))
                    items.append(("own1", 2 * p + 1, 4 * p + 3))
                slots = {}
                seen_pair = set()
                seen_po = set()

                def exp_past(sb_, ps_, c0, qb, n, kt):
                    bcol = C_BIAS + h * 30 + (qb - n - 1) * 2 + (kt % 2)
                    act("activation", [], [Bpb[sb_], BPT[ps_]], out=PT[ps_][:, c0:c0 + 256], in_=pb[sb_][:, c0:c0 + 256], func=AF.Exp, bias=cst[:, bcol:bcol + 1])

                def stA(ix):
                    kind, a0, kt = items[ix]
                    sb_ = ix % 3
                    ps_ = ix % NPT
                    slots[ix] = ps_
                    if not last_head:
                        if kind in ("pc", "own0") and (a0 if kind == "pc" else a0 // 2) not in seen_pair and (kind == "pc" or a0 % 2 == 0):
                            p_ = a0 if kind == "pc" else a0 // 2
                            seen_pair.add(p_)
                            gate_front(h + 1, 2 * p_)
                            defer(4, (lambda qb=2 * p_: gate_back(h + 1, qb)))
                        if kind == "po" and a0 not in seen_po:
                            seen_po.add(a0)
                            gate_front(h + 1, 2 * a0 + 1)
                            defer(4, (lambda qb=2 * a0 + 1: gate_back(h + 1, qb)))
                    n = kt // 2
                    if kind == "pc":
                        p = a0
                        qpair = qT_h[s][:, p * 512:(p + 1) * 512]
                        pe("matmul", [Bq[s], Bkk[s]], [Bpb[sb_]], out=pb[sb_], lhsT=kT_h[s][:, kt * 128:(kt + 1) * 128], rhs=qpair, start=True, stop=False)
                        pe("matmul", [Bmask[s]], [Bpb[sb_]], out=pb[sb_], lhsT=e_bf[0:16, n, :], rhs=maskT[s][0:16, p * 512:(p + 1) * 512], start=False, stop=True)
                        exp_past(sb_, ps_, 0, 2 * p, n, kt)
                        exp_past(sb_, ps_, 256, 2 * p + 1, n, kt)
                    elif kind == "po":
                        qb = 2 * a0 + 1
                        qblk = qT_h[s][:, qb * 256:(qb + 1) * 256]
                        pe("matmul", [Bq[s], Bkk[s]], [Bpb[sb_]], out=pb[sb_][:, 0:256], lhsT=kT_h[s][:, kt * 128:(kt + 1) * 128], rhs=qblk, start=True, stop=False)
                        pe("matmul", [Bmask[s]], [Bpb[sb_]], out=pb[sb_][:, 0:256], lhsT=e_bf[0:16, n, :], rhs=maskT[s][0:16, qb * 256:(qb + 1) * 256], start=False, stop=True)
                        exp_past(sb_, ps_, 0, qb, n, kt)
                    elif kind == "own0":
                        qb = a0
                        qblk = qT_h[s][:, qb * 256:(qb + 1) * 256]
                        pe("matmul", [Bq[s], Bkk[s]], [Bpb[sb_]], out=pb[sb_][:, 0:256], lhsT=kT_h[s][:, kt * 128:(kt + 1) * 128], rhs=qblk, start=True, stop=False)
                        pe("matmul", [], [Bpb[sb_]], out=pb[sb_][:, 0:256], lhsT=ident_bf, rhs=bh_bf[:, h, :], start=False, stop=True)
                        act("activation", [], [Bpb[sb_], BPT[ps_]], out=PT[ps_][:, 0:256], in_=pb[sb_][:, 0:256], func=AF.Exp)
                    else:
                        qb = a0
                        qblk = qT_h[s][:, qb * 256:(qb + 1) * 256]
                        pe("matmul", [Bq[s], Bkk[s]], [Bpb[sb_]], out=pb[sb_][:, 0:128], lhsT=kT_h[s][:, kt * 128:(kt + 1) * 128], rhs=qblk[:, 128:256], start=True, stop=False)
                        pe("matmul", [], [Bpb[sb_]], out=pb[sb_][:, 0:128], lhsT=ident_bf, rhs=bh_bf[:, h, 0:128], start=False, stop=True)
                        act("activation", [], [Bpb[sb_], BPT[ps_]], out=PT[ps_][:, 0:128], in_=pb[sb_][:, 0:128], func=AF.Exp)
                    tick()

                def stB(ix):
                    kind, a0, kt = items[ix]
                    ps_ = slots[ix]
                    if kind == "pc":
                        p = a0
                        for j in range(4):
                            bk = 3 + j // 2
                            c0 = (j % 2) * 256
                            pe("matmul", [BPT[ps_], Bv[s]], [Bpb[bk]], out=pb[bk][:, c0:c0 + 129], lhsT=PT[ps_][:, j * 128:(j + 1) * 128], rhs=V_h[s][:, kt, :],
                               start=(kt == 0 and j % 2 == 0), stop=(j < 2 and kt == 4 * p - 1), skip_group_check=True)
                    elif kind == "po":
                        p = a0
                        for jj in range(2):
                            c0 = jj * 256
                            pe("matmul", [BPT[ps_], Bv[s]], [Bpb[4]], out=pb[4][:, c0:c0 + 129], lhsT=PT[ps_][:, jj * 128:(jj + 1) * 128], rhs=V_h[s][:, kt, :],
                               start=(kt == 0 and jj == 0), stop=(kt == 4 * p + 1), skip_group_check=True)
                    elif kind == "own0":
                        pe("matmul", [BPT[ps_], Bv[s]], [Bpb[5]], out=pb[5][:, 0:129], lhsT=PT[ps_][:, 0:128], rhs=V_h[s][:, kt, :], start=True, stop=True)
                        pe("matmul", [BPT[ps_], Bv[s]], [Bpb[5]], out=pb[5][:, 256:385], lhsT=PT[ps_][:, 128:256], rhs=V_h[s][:, kt, :], start=True, stop=False)
                    else:
                        qb = a0
                        pe("matmul", [BPT[ps_], Bv[s]], [Bpb[5]], out=pb[5][:, 256:385], lhsT=PT[ps_][:, 0:128], rhs=V_h[s][:, kt, :], start=False, stop=True)
                        obk = 3 + (qb % 2)
                        for j in range(2):
                            i = 2 * qb + j
                            if qb > 0:
                                gcol = C_G + h * 2 + j
                                act("activation", [], [Bpb[obk], Bcmb[j]], out=tsc[j], in_=pb[obk][:, j * 256:j * 256 + 129], func=AF.Copy, scale=cst[:, gcol:gcol + 1])
                                dve("tensor_tensor", [], [Bpb[5], Bcmb[j]], out=num[j], in0=tsc[j], in1=pb[5][:, j * 256:j * 256 + 129], op=ALU.add)
                            else:
                                dve("tensor_copy", [], [Bpb[5], Bcmb[j]], out=num[j], in_=pb[5][:, j * 256:j * 256 + 129])
                            dve("reciprocal", [], [Bcmb[j]], out=rden[j], in_=num[j][:, 128:129])
                            dve("scalar_tensor_tensor", [Bcmb[j], Bsg[s]], [Bobf[j]], out=o_bf[j], in0=num[j][:, 0:128], scalar=rden[j][:, 0:1], in1=sg_h[s][:, i, :],
                                op0=ALU.mult, op1=ALU.mult)

                        def back(qb=qb):
                            pbt = pb[7].bitcast(BF16)
                            for j in range(2):
                                pe("transpose", [Bobf[j]], [Bpb[7]], out=pbt[:, 512 + j * 128:512 + (j + 1) * 128], in_=o_bf[j], identity=ident_bf)
                            dve("tensor_copy", [], [Bpb[7], Battn[s]], out=attnT[s][:, qb * 256:(qb + 1) * 256], in_=pbt[:, 512:768])
                        defer(3, back)

                pipeline(len(items), [(2, stB), (0, stA)])
                flush()
                dma(f"at{s}", mixT_d[h], attnT[s], reads=[Battn[s]])

            head_load(0)
            head_kmean(0)
            for qb in range(NB):
                gate_front(0, qb)
                gate_back(0, qb)
            for h in range(8):
                if h + 1 < 8:
                    head_load(h + 1)
                    head_kmean(h + 1)
                attn_head(h, h == 0, h == 7)
            S_.barrier()
            if stop_after == "P3":
                break
            AR.release(p3_mark)
            NCH = NT
            hp = pl["hp"]
            xsT_c = [AR.alloc((8, 128), F32) for _ in range(3)]
            bcT_c = [AR.alloc((4, 128), BF16) for _ in range(3)]
            sz_c = [AR.alloc((D,), F32) for _ in range(3)]
            Bxs = [Buf("xsc0"), Buf("xsc1"), Buf("xsc2")]
            Bbc = [Buf("bcc0"), Buf("bcc1"), Buf("bcc2")]
            Bsz = [Buf("szc0"), Buf("szc1"), Buf("szc2")]
            x1a = AR.alloc((NT, 16), F32)
            axa = AR.alloc((NT, 16), F32)
            lga = AR.alloc((NT, 16), F32)
            dta = AR.alloc((NT, 16), F32)
            dAa = AR.alloc((NT, 16), F32)
            acs_a = AR.alloc((NT, 16), F32)
            aend_a = AR.alloc((NT, 16), F32)
            dse_a = AR.alloc((NT, 16), F32)
            nacs_a = AR.alloc((NT, 16), F32)
            ea_a = AR.alloc((NT, 16), F32)
            cd_a = AR.alloc((NT, 16), F32)
            eds_a = AR.alloc((NT, 16), F32)
            dtw_a = AR.alloc((NT, 16), F32)
            Bdt = Buf("dtc")
            Rm = AR.alloc((16, 128), F32)
            BR = Buf("R")
            negb4 = AR.alloc((4, 128), BF16)
            decay = AR.alloc((16, 128), BF16)
            Bdecay = Buf("decay")
            MT = [AR.alloc((16, 128), BF16) for _ in range(2)]
            BMT = [Buf("MT0"), Buf("MT1")]
            cb_sb = AR.alloc((2, 128), BF16)
            Bcb = Buf("cb")
            Btok = [AR.alloc((2, 128), BF16) for _ in range(2)]
            BBtok = [Buf("btok0"), Buf("btok1")]
            xs_sb = AR.alloc((16, 64), F32)
            Bxssb = Buf("xssb")
            xdt = [AR.alloc((16, 64), BF16) for _ in range(2)]
            xw = [AR.alloc((16, 64), BF16) for _ in range(2)]
            xD = [AR.alloc((16, 64), F32) for _ in range(2)]
            Bxdt = [Buf("xdt0"), Buf("xdt1")]
            Bxw = [Buf("xw0"), Buf("xw1")]
            BxD = [Buf("xD0"), Buf("xD1")]
            prev = AR.alloc((16, 64), F32)
            prevbf = AR.alloc((16, 64), BF16)
            Bprev = Buf("prev")
            Bprevbf = Buf("prevbf")
            t1 = AR.alloc((16, 64), F32)
            Bt1 = Buf("t1")
            yg = AR.alloc((16, 64), F32)
            Byg = Buf("yg")
            ss2 = AR.alloc((2,), F32)
            Bss2 = Buf("ss2")
            yn2 = [AR.alloc((D,), BF16) for _ in range(2)]
            Byn2 = [Buf("yn0"), Buf("yn1")]
            ystage = [AR.alloc((8, 512), BF16) for _ in range(2)]
            Byst = [Buf("yst0"), Buf("yst1")]
            junk4 = AR.alloc((512,), BF16)
            Bjunk4 = Buf("junk4")
            ones_f = cst[:, C_ONES:C_ONES + 128]
            U_f = cst[:, C_U:C_U + 128]

            pool("memset", [], [Bprev], ap=prev, constant=0.0)
            pool("memset", [], [Bprevbf], ap=prevbf, constant=0.0)
            pool("tensor_copy", [], [BR], out=negb4, in_=cst[:, C_NEG:C_NEG + 128].unsqueeze(1).to_broadcast([128, 4, 128]))

            NW = NT * 16
            fl = lambda t: t.rearrange("p a b -> p (a b)")
            pool("tensor_tensor", [Bdtraw], [Bdt], out=x1a, in0=dtraw, in1=hp[:, 0:16].unsqueeze(1).to_broadcast([128, NT, 16]), op=ALU.add)
            act("activation", [], [Bdt], out=axa, in_=x1a, func=AF.Abs)
            act("activation", [], [Bdt], out=axa, in_=axa, func=AF.Exp, scale=-1.0)
            act("activation", [], [Bdt], out=lga, in_=axa, func=AF.Ln, bias=1.0)
            dve("tensor_scalar", [], [Bdt], out=x1a, in0=x1a, scalar1=0.0, scalar2=None, op0=ALU.max)
            dve("tensor_tensor", [], [Bdt], out=dta, in0=x1a, in1=lga, op=ALU.add)
            dve("tensor_tensor", [], [Bdt], out=dAa, in0=dta, in1=pl["aneg"].unsqueeze(1).to_broadcast([128, NT, 16]), op=ALU.mult)
            pe("matmul", [Bdt], [Bpb[2]], out=pb[2][:, 0:NW], lhsT=U_f, rhs=fl(dAa), start=True, stop=True)
            pe("matmul", [Bdt], [Bpb[3]], out=pb[3][:, 0:NW], lhsT=ones_f, rhs=fl(dAa), start=True, stop=True)
            act("activation", [], [Bpb[2], Bdt], out=fl(acs_a), in_=pb[2][:, 0:NW], func=AF.Copy)
            act("activation", [], [Bpb[3], Bdt], out=fl(aend_a), in_=pb[3][:, 0:NW], func=AF.Copy)
            dve("tensor_tensor", [], [Bdt], out=dse_a, in0=aend_a, in1=acs_a, op=ALU.subtract)
            dve("tensor_scalar", [], [Bdt], out=nacs_a, in0=acs_a, scalar1=-1.0, scalar2=None, op0=ALU.mult)
            act("activation", [], [Bdt], out=ea_a, in_=acs_a, func=AF.Exp)
            act("activation", [], [Bdt], out=cd_a, in_=aend_a, func=AF.Exp)
            act("activation", [], [Bdt], out=eds_a, in_=dse_a, func=AF.Exp)
            dve("tensor_tensor", [], [Bdt], out=dtw_a, in0=dta, in1=eds_a, op=ALU.mult)

            def p4_load(c):
                s = c % 3
                dma(f"xsc{s}", xsT_c[s], xsT_d[:, c * 128:(c + 1) * 128].rearrange("(k p) t -> p k t", p=128), writes=[Bxs[s]])
                dma(f"bcc{s}", bcT_c[s], bcT_d[:, c * 128:(c + 1) * 128].rearrange("(k p) t -> p k t", p=128), writes=[Bbc[s]])
                dma(f"szc{s}", sz_c[s], sz_d[c * 128:(c + 1) * 128, :], writes=[Bsz[s]])

            def stA(c):
                s = c % 2
                s3 = c % 3
                pool("tensor_tensor", [Bdt], [BR], out=Rm, in0=U_f.unsqueeze(1).to_broadcast([128, 16, 128]),
                     in1=dAa[:, c, :].unsqueeze(2).to_broadcast([128, 16, 128]), op=ALU.mult)
                for g in range(2):
                    pe("matmul", [Bbc[s3]], [Bpb[2]], out=pb[2][:, 64 + g * 128:64 + (g + 1) * 128], lhsT=bcT_c[s3][:, g, :], rhs=bcT_c[s3][:, 2 + g, :], start=True, stop=True)
                pbt2 = pb[2].bitcast(BF16)
                for g in range(2):
                    pe("transpose", [Bbc[s3]], [Bpb[2]], out=pbt2[:, 768 + g * 128:768 + (g + 1) * 128], in_=bcT_c[s3][:, g, :], identity=ident_bf)
                act("activation", [], [Bpb[2], Bcb], out=cb_sb, in_=pb[2][:, 64:320].rearrange("p (g l) -> p g l", l=128), func=AF.Copy)
                act("activation", [], [Bpb[2], BBtok[s]], out=Btok[s], in_=pbt2[:, 768:1024].rearrange("p (g l) -> p g l", l=128), func=AF.Copy)
                Rflat = Rm.rearrange("p h l -> p (h l)")
                for qd in range(4):
                    bk = qd % 2
                    pe("matmul", [BR], [Bpb[bk]], out=pb[bk], lhsT=ones_f, rhs=Rflat[:, qd * 512:(qd + 1) * 512], start=True, stop=False)
                    pe("matmul", [BR], [Bpb[bk]], out=pb[bk], lhsT=ident_bf, rhs=negb4.rearrange("p h l -> p (h l)"), start=False, stop=True)
                    for hh in range(4):
                        h = qd * 4 + hh
                        act("activation", [Bdt], [Bpb[bk], Bdecay], out=decay[:, h, :], in_=pb[bk][:, hh * 128:(hh + 1) * 128], func=AF.Exp, bias=nacs_a[:, c, h:h + 1])
                    g = qd // 2
                    dve("tensor_tensor", [Bdecay, Bcb], [BMT[s]], out=MT[s][:, qd * 4:(qd + 1) * 4, :], in0=decay[:, qd * 4:(qd + 1) * 4, :],
                        in1=cb_sb[:, g, :].unsqueeze(1).to_broadcast([128, 4, 128]), op=ALU.mult)
                for g in range(2):
                    for cc in range(4):
                        pe("transpose", [Bxs[s3]], [Bpb[3]], out=pb[3][:, cc * 128:(cc + 1) * 128], in_=xsT_c[s3][:, g * 4 + cc, :], identity=identf)
                    act("activation", [], [Bpb[3], Bxssb], out=xs_sb[:, g * 8:(g + 1) * 8, :], in_=pb[3].rearrange("p (h d) -> p h d", d=64), func=AF.Copy)
                    hs = slice(g * 8, (g + 1) * 8)
                    dve("tensor_tensor", [Bxssb, Bdt], [Bxdt[s]], out=xdt[s][:, hs, :], in0=xs_sb[:, hs, :],
                        in1=dta[:, c, hs].unsqueeze(2).to_broadcast([128, 8, 64]), op=ALU.mult)
                    pool("tensor_tensor", [Bxssb, Bdt], [Bxw[s]], out=xw[s][:, hs, :], in0=xs_sb[:, hs, :],
                         in1=dtw_a[:, c, hs].unsqueeze(2).to_broadcast([128, 8, 64]), op=ALU.mult)
                    pool("tensor_tensor", [Bxssb], [BxD[s]], out=xD[s][:, hs, :], in0=xs_sb[:, hs, :],
                         in1=hp[:, 32 + g * 8:32 + (g + 1) * 8].unsqueeze(2).to_broadcast([128, 8, 64]), op=ALU.mult)

            def stB1(c):
                s = c % 2
                s3 = c % 3
                for g in range(2):
                    hs = slice(g * 8, (g + 1) * 8)
                    for hh in range(8):
                        h = g * 8 + hh
                        pe("matmul", [BMT[s], Bxdt[s]], [Bpb[4]], out=pb[4][:, hh * 64:(hh + 1) * 64], lhsT=MT[s][:, h, :], rhs=xdt[s][:, h, :], start=True, stop=True)
                    pe("matmul", [Bbc[s3], Bprevbf], [Bpb[5]], out=pb[5], lhsT=bcT_c[s3][:, 2 + g, :], rhs=prevbf[:, hs, :].rearrange("p h d -> p (h d)"), start=True, stop=True)
                    pe("matmul", [BBtok[s], Bxw[s]], [Bpb[6]], out=pb[6], lhsT=Btok[s][:, g, :], rhs=xw[s][:, hs, :].rearrange("p h d -> p (h d)"), start=True, stop=True)
                    dve("tensor_tensor", [Bdt], [Bpb[5], Bt1], out=t1[:, hs, :], in0=pb[5].rearrange("p (h d) -> p h d", d=64),
                        in1=ea_a[:, c, hs].unsqueeze(2).to_broadcast([128, 8, 64]), op=ALU.mult)
                    dve("tensor_tensor", [], [Bpb[4], Bt1], out=t1[:, hs, :], in0=t1[:, hs, :], in1=pb[4].rearrange("p (h d) -> p h d", d=64), op=ALU.add)
                    pool("tensor_tensor", [BxD[s]], [Bt1], out=t1[:, hs, :], in0=t1[:, hs, :], in1=xD[s][:, hs, :], op=ALU.add)
                    pool("tensor_tensor", [Bt1, Bsz[s3]], [Byg], out=yg[:, hs, :], in0=t1[:, hs, :], in1=sz_c[s3][:, g * 512:(g + 1) * 512].rearrange("p (h d) -> p h d", d=64), op=ALU.mult)
                    act("activation", [Byg], [Bjunk4, Bss2], out=junk4, in_=yg[:, hs, :].rearrange("p h d -> p (h d)"), func=AF.Square, accum_out=ss2[:, g:g + 1])
                    dve("tensor_tensor", [Bdt], [Bprev], out=prev[:, hs, :], in0=prev[:, hs, :],
                        in1=cd_a[:, c, hs].unsqueeze(2).to_broadcast([128, 8, 64]), op=ALU.mult)
                    dve("tensor_tensor", [], [Bpb[6], Bprev], out=prev[:, hs, :], in0=prev[:, hs, :], in1=pb[6].rearrange("p (h d) -> p h d", d=64), op=ALU.add)
                    act("activation", [Bprev], [Bprevbf], out=prevbf[:, hs, :], in_=prev[:, hs, :], func=AF.Copy)
                pool("tensor_scalar", [], [Bss2], out=ss2, in0=ss2, scalar1=1.0 / 512, scalar2=EPS, op0=ALU.mult, op1=ALU.add)
                pool("tensor_tensor", [], [Bss2], out=ss2, in0=ss2, in1=mhalf[:, 0:2], op=ALU.pow)
                for g in range(2):
                    act("activation", [Byg, Bss2], [Byn2[s]], out=yn2[s][:, g * 512:(g + 1) * 512], in_=yg[:, g * 8:(g + 1) * 8, :].rearrange("p h d -> p (h d)"),
                        func=AF.Copy, scale=ss2[:, g:g + 1])

            def stB2(c):
                s = c % 2
                pbt7 = pb[7].bitcast(BF16)
                for cc in range(8):
                    pe("transpose", [Byn2[s]], [Bpb[7]], out=pbt7[:, cc * 128:(cc + 1) * 128], in_=yn2[s][:, cc * 128:(cc + 1) * 128], identity=ident_bf)
                ys = (c // 4) % 2
                j = c % 4
                dve("tensor_tensor", [], [Bpb[7], Byst[ys]], out=ystage[ys][:, :, j * 128:(j + 1) * 128], in0=pbt7.rearrange("p (c t) -> p c t", t=128),
                    in1=pl["snwT"].unsqueeze(2).to_broadcast([128, 8, 128]), op=ALU.mult)
                if j == 3:
                    tg = c // 4
                    dma(f"yst{ys}", mixT_d[8:16, :, tg * 512:(tg + 1) * 512].rearrange("h p t -> p h t"), ystage[ys], reads=[Byst[ys]])

            p4_load(0)
            if NCH > 1:
                p4_load(1)
            for t in range(NCH + 2):
                if t < NCH:
                    stA(t)
                if 1 <= t <= NCH:
                    stB1(t - 1)
                if t + 2 < NCH:
                    p4_load(t + 2)
                if t >= 2:
                    stB2(t - 2)
            S_.barrier()
            if stop_after == "P4":
                break

            AR.release(p3_mark)
            wo_bf = AR.alloc((16, D), BF16)
            Bwo = Buf("wo")
            wos = [AR.alloc((4, D), F32) for _ in range(2)]
            Bwos = [Buf("wos0"), Buf("wos1")]
            mixs = [AR.alloc((16, 512), BF16) for _ in range(2)]
            Bmix = [Buf("mix0"), Buf("mix1")]
            xr = [AR.alloc((D,), F32) for _ in range(2)]
            Bxr = [Buf("xr0"), Buf("xr1")]
            ot = [AR.alloc((D,), F32) for _ in range(2)]
            Bot = [Buf("ot0"), Buf("ot1")]
            wo_l = w_out[l]
            for q4 in range(4):
                s = q4 % 2
                dma(f"wos{s}", wos[s], wo_l[q4 * 512:(q4 + 1) * 512, :].rearrange("(k p) n -> p k n", p=128), writes=[Bwos[s]])
                pool("tensor_copy", [Bwos[s]], [Bwo], out=wo_bf[:, q4 * 4:(q4 + 1) * 4, :], in_=wos[s])

            def p5_loadmix(tg):
                s = tg % 2
                dma(f"mix{s}", mixs[s], mixT_d[:, :, tg * 512:(tg + 1) * 512].rearrange("c p t -> p c t"), writes=[Bmix[s]])

            def p5_loadx(i):
                s = i % 2
                dma(f"xr{s}", xr[s], xsrc[i * 128:(i + 1) * 128, :], writes=[Bxr[s]])

            def p5_tile(i):
                s = i % 2
                tg = i // 4
                j = i % 4
                ms = tg % 2
                for half in range(2):
                    bk = (2 * i + half) % 4
                    for cch in range(16):
                        pe("matmul", [Bmix[ms], Bwo], [Bpb[bk]], out=pb[bk], lhsT=mixs[ms][:, cch, j * 128:(j + 1) * 128], rhs=wo_bf[:, cch, half * 512:(half + 1) * 512],
                           start=(cch == 0), stop=(cch == 15))
                    dve("tensor_tensor", [Bxr[s]], [Bpb[bk], Bot[s]], out=ot[s][:, half * 512:(half + 1) * 512], in0=pb[bk], in1=xr[s][:, half * 512:(half + 1) * 512], op=ALU.add)
                dma(f"ot{s}", xdst[i * 128:(i + 1) * 128, :], ot[s], reads=[Bot[s]])

            p5_loadmix(0)
            p5_loadx(0)
            for i in range(NT):
                if i % 4 == 0 and i // 4 + 1 < TG:
                    p5_loadmix(i // 4 + 1)
                if i + 1 < NT:
                    p5_loadx(i + 1)
                p5_tile(i)
            S_.barrier()
            if stop_after == "L0":
                break

        S_.barrier()
        S_.finalize()
        with nc.Block() as block:
            @block.sync
            def _(e):
                S_.replay("sp")

            @block.tensor
            def _(e):
                S_.replay("pe")

            @block.vector
            def _(e):
                S_.replay("dve")

            @block.scalar
            def _(e):
                S_.replay("act")

            @block.gpsimd
            def _(e):
                S_.replay("pool")
    return nc


def host_inputs(inputs, b, S, depth=2):
    f = np.float32
    d = {}
    d["x"] = np.ascontiguousarray(inputs["x"][b, :S]).astype(f)
    d["w_in"] = np.ascontiguousarray(inputs["w_in"]).astype(f)
    d["w_out"] = np.ascontiguousarray(inputs["w_out"]).astype(f)
    d["lnwT"] = np.ascontiguousarray(inputs["ln_w"].reshape(depth, 8, 128).transpose(0, 2, 1)).astype(f)
    cw = inputs["conv_w"].reshape(depth, 4, 12, 128).transpose(0, 3, 2, 1)
    d["cwT"] = np.ascontiguousarray(cw.reshape(depth, 128, 48)).astype(f)
    d["cbT"] = np.ascontiguousarray(inputs["conv_b"].reshape(depth, 12, 128).transpose(0, 2, 1)).astype(f)
    d["qkw"] = np.ascontiguousarray(np.stack([inputs["q_norm_w"], inputs["k_norm_w"]], axis=-1)).astype(f)
    d["snwT"] = np.ascontiguousarray(inputs["ssd_norm_w"].reshape(depth, 8, 128).transpose(0, 2, 1)).astype(f)
    d["hp"] = np.ascontiguousarray(np.concatenate([inputs["dt_bias"], inputs["a_log"], inputs["d_skip"]], axis=-1).reshape(depth, 1, 48)).astype(f)
    d["consts"] = make_consts()
    return d


BATCH = 8
SEQ = 4096


def kernel(**inputs):
    inputs = {k: np.asarray(v) for k, v in inputs.items()}
    nc = build(SEQ, depth=2, dbg=False)
    in_maps = [host_inputs(inputs, b, SEQ) for b in range(BATCH)]
    res = run_bass_kernel_spmd(nc, in_maps, core_ids=list(range(BATCH)))
    out = np.stack([np.asarray(r["out"], dtype=np.float32) for r in res.results], axis=0)
    return out
```

```python
import contextlib
import numpy as np
import concourse.bass as bass
import concourse.mybir as mybir
from concourse.bass_utils import run_bass_kernel_spmd

F32 = mybir.dt.float32
BF16 = mybir.dt.bfloat16
AF = mybir.ActivationFunctionType
ALU = mybir.AluOpType
AX = mybir.AxisListType

ENGS = ("pe", "act", "dve", "pool", "sp")
SAME_ENGINE_SYNC = {"pe": False, "act": True, "dve": True, "pool": True, "sp": False}


class Ev:
    __slots__ = ("eng", "idx", "sem", "val", "needed")

    def __init__(self, eng, idx, sem=None, val=None):
        self.eng = eng
        self.idx = idx
        self.sem = sem
        self.val = val
        self.needed = False


def ev_is_dma(ev):
    return ev.sem is not None and not isinstance(ev.sem, tuple)


class Buf:
    __slots__ = ("name", "w", "rs")

    def __init__(self, name=""):
        self.name = name
        self.w = None
        self.rs = []


class Op:
    __slots__ = ("fn", "waits", "ev", "is_dma")

    def __init__(self, fn, waits, ev, is_dma):
        self.fn = fn
        self.waits = waits
        self.ev = ev
        self.is_dma = is_dma


class Sched:
    def __init__(self, nc):
        self.nc = nc
        self.ops = {e: [] for e in ENGS}
        self.handles = {"pe": nc.tensor, "act": nc.scalar, "dve": nc.vector, "pool": nc.gpsimd, "sp": nc.sync}
        self.esem = {}
        self.dma_sems = {}
        self.dma_cnt = {}
        self.all_dma_ev = {}

    def _deps(self, eng, reads, writes, xreads=()):
        out = []

        def add(ev, raw):
            if ev.eng == eng and not ev_is_dma(ev):
                if not (raw and SAME_ENGINE_SYNC[eng]):
                    return
            out.append(ev)

        for b in reads:
            if b.w is not None:
                add(b.w, True)
        for b in xreads:
            if b.w is not None:
                add(b.w, True)
            for r in b.rs:
                add(r, False)
        for b in writes:
            if b.w is not None:
                add(b.w, False)
            for r in b.rs:
                add(r, False)
        return out

    def _upd(self, ev, reads, writes):
        for b in reads:
            b.rs.append(ev)
        for b in writes:
            b.w = ev
            b.rs = []

    def op(self, eng, fn, reads=(), writes=(), xreads=()):
        waits = self._deps(eng, reads, writes, xreads)
        ev = Ev(eng, len(self.ops[eng]))
        self.ops[eng].append(Op(fn, waits, ev, False))
        self._upd(ev, list(reads) + list(xreads), writes)
        return ev

    def dma(self, eng, semname, out, in_, reads=(), writes=(), **kw):
        waits = self._deps(eng, reads, writes)
        self.dma_cnt[semname] += 16
        ev = Ev(eng, len(self.ops[eng]), sem=semname, val=self.dma_cnt[semname])
        h = self.handles[eng]
        fn = (lambda h=h, out=out, in_=in_, kw=kw: h.dma_start(out=out, in_=in_, **kw))
        self.ops[eng].append(Op(fn, waits, ev, True))
        self.all_dma_ev[semname] = ev
        self._upd(ev, reads, writes)
        return ev

    def barrier(self):
        lasts = []
        for e in ENGS:
            for o in reversed(self.ops[e]):
                if not o.is_dma and o.fn is not None:
                    lasts.append(o.ev)
                    break
        lasts.extend(self.all_dma_ev.values())
        for e in ENGS:
            waits = [ev for ev in lasts if not (ev.eng == e and not ev_is_dma(ev))]
            ev = Ev(e, len(self.ops[e]))
            self.ops[e].append(Op(None, waits, ev, False))

    def finalize(self):
        for e in ENGS:
            for o in self.ops[e]:
                for ev in o.waits:
                    ev.needed = True
        for e in ENGS:
            cnt = 0
            for o in self.ops[e]:
                if o.is_dma:
                    continue
                if o.ev.needed:
                    assert o.fn is not None
                    cnt += 1
                    o.ev.sem = ("E", e)
                    o.ev.val = cnt

    def _sem(self, key):
        if isinstance(key, tuple):
            return self.esem[key[1]]
        return self.dma_sems[key]

    def replay(self, eng):
        h = self.handles[eng]
        seen = {}
        for o in self.ops[eng]:
            need = {}
            for ev in o.waits:
                if need.get(ev.sem, 0) < ev.val:
                    need[ev.sem] = ev.val
            for k, v in need.items():
                if seen.get(k, 0) < v:
                    h.wait_ge(self._sem(k), v)
                    seen[k] = v
            if o.fn is None:
                continue
            ins = o.fn()
            if o.is_dma:
                ins.then_inc(self._sem(o.ev.sem), 16)
            elif o.ev.needed:
                ins.then_inc(self._sem(o.ev.sem), 1)


class Arena:
    def __init__(self, base, nwords):
        self.base = base
        self.cap = nwords * 4
        self.off = 0

    def mark(self):
        return self.off

    def release(self, m):
        self.off = m

    def alloc(self, shape, dt):
        n = int(np.prod(shape))
        esz = 4 if dt == F32 else 2
        nbytes = (n * esz + 63) // 64 * 64
        st = self.off
        self.off += nbytes
        assert self.off <= self.cap, f"SBUF arena overflow {self.off} > {self.cap}"
        w0 = st // 4
        if dt == F32:
            v = self.base[:, w0:w0 + n]
        else:
            v = self.base[:, w0:w0 + nbytes // 4].bitcast(BF16)[:, 0:n]
        if len(shape) == 2:
            v = v.rearrange("p (a b) -> p a b", a=shape[0], b=shape[1])
        elif len(shape) == 3:
            v = v.rearrange("p (a b c) -> p a b c", a=shape[0], b=shape[1], c=shape[2])
        return v


D = 1024
KC = 8
NPROJ = 6672
EPS = 1e-6
NEGBIG = -30000.0

C_IDENT = 0
C_U = 128
C_NEGU = 256
C_NEG = 384
C_ONES = 512
C_BH = 640
C_BIAS = C_BH + 2048
C_G = C_BIAS + 240
C_PAST = C_G + 16
C_E = C_PAST + 256
NCONST = C_E + 2048


def make_consts():
    c = np.zeros((128, NCONST), np.float32)
    p = np.arange(128)
    c[:, C_IDENT:C_IDENT + 128] = np.eye(128)
    U = (p[:, None] <= p[None, :]).astype(np.float32)
    c[:, C_U:C_U + 128] = U
    c[:, C_NEGU:C_NEGU + 128] = -U
    c[:, C_NEG:C_NEG + 128] = np.where(p[None, :] < p[:, None], NEGBIG, 0.0)
    c[:, C_ONES:C_ONES + 128] = 1.0
    slopes = 2.0 ** (-8.0 * np.arange(1, 9) / 8.0)
    qrel = np.arange(256)
    for h in range(8):
        d = qrel[None, :] - p[:, None]
        c[:, C_BH + h * 256:C_BH + (h + 1) * 256] = np.where(d >= 0, -slopes[h] * d, NEGBIG)
        for dd in range(1, 16):
            for kt in range(2):
                c[:, C_BIAS + h * 30 + (dd - 1) * 2 + kt] = -slopes[h] * (dd * 256 - kt * 128 - p)
        for hf in range(2):
            c[:, C_G + h * 2 + hf] = np.exp(-slopes[h] * (hf * 128 + p))
    for qb in range(16):
        for n in range(16):
            c[:, C_PAST + qb * 16 + n] = 0.0 if n < qb else -1e30
    for n in range(16):
        c[n, C_E + n * 128:C_E + (n + 1) * 128] = 1.0
    return c


def pipeline(n, stages):
    maxlag = max(l for l, _ in stages)
    for t in range(n + maxlag):
        for lag, fn in stages:
            i = t - lag
            if 0 <= i < n:
                fn(i)


def build(S, depth=2, dbg=False, stop_after=None):
    NT = S // 128
    NB = S // 256
    TG = S // 512
    assert S % 512 == 0 and NB <= 16
    nc = bass.Bass("TRN2", target_bir_lowering=False)
    V, A, P, T = nc.vector, nc.scalar, nc.gpsimd, nc.tensor

    def din(name, shape, dt=F32):
        return nc.dram_tensor(name, list(shape), dt, kind="ExternalInput").ap()

    def dscr(name, shape, dt):
        kind = "ExternalOutput" if dbg else "Internal"
        return nc.dram_tensor(name, list(shape), dt, kind=kind).ap()

    x_in = din("x", [S, D])
    w_in = din("w_in", [depth, D, NPROJ])
    w_out = din("w_out", [depth, 2 * D, D])
    lnwT_d = din("lnwT", [depth, 128, 8])
    cwT_d = din("cwT", [depth, 128, 12 * 4])
    cbT_d = din("cbT", [depth, 128, 12])
    qkw_d = din("qkw", [depth, 128, 2])
    snwT_d = din("snwT", [depth, 128, 8])
    hp_d = din("hp", [depth, 1, 48])
    consts_d = din("consts", [128, NCONST])
    out_d = nc.dram_tensor("out", [S, D], F32, kind="ExternalOutput").ap()

    qT_d = dscr("qT_s", [8, 128, S], BF16)
    kT_d = dscr("kT_s", [8, 128, S], BF16)
    V_d = dscr("V_s", [S, D], BF16)
    sg_d = dscr("sg_s", [S, D], F32)
    sz_d = dscr("sz_s", [S, D], F32)
    xsT_d = dscr("xsT_s", [D, S], F32)
    bcT_d = dscr("bcT_s", [512, S], BF16)
    mixT_d = dscr("mixT_s", [16, 128, S], BF16)
    xres_d = dscr("xres_s", [S, D], F32)
    dtraw_d = dscr("dtraw_s", [128, NT * 16], F32) if dbg else None
    hT_d = dscr("hT_s", [128, KC * S], BF16) if dbg else None

    ARENA_WORDS = 52736
    with contextlib.ExitStack() as st:
        arena_t = st.enter_context(nc.sbuf_tensor("arena", [128, ARENA_WORDS], F32))
        AR = Arena(arena_t[:, :], ARENA_WORDS)
        pbanks = [st.enter_context(nc.psum_tensor(f"pb{i}", [128, 512], F32)) for i in range(8)]
        pb = [t[:, :] for t in pbanks]
        Bpb = [Buf(f"pb{i}") for i in range(8)]
        S_ = Sched(nc)
        S_.esem = {e: st.enter_context(nc.semaphore("sem_" + e)) for e in ENGS}
        sem_pool = {}

        def dsem(name):
            if name not in sem_pool:
                sem_pool[name] = st.enter_context(nc.semaphore("d_" + name))
                S_.dma_sems[name] = sem_pool[name]
                S_.dma_cnt[name] = 0
            return name

        def eop(eng, h, fname, reads, writes, **kw):
            f = getattr(h, fname)
            xr = ()
            if eng != "pe":
                xr = [b for b in writes if b in Bpb]
                writes = [b for b in writes if b not in Bpb]
                reads = list(reads) + [b for b in writes if b not in reads]
            return S_.op(eng, (lambda f=f, kw=kw: f(**kw)), reads, writes, xr)

        def dve(fname, reads, writes, **kw):
            return eop("dve", V, fname, reads, writes, **kw)

        def act(fname, reads, writes, **kw):
            return eop("act", A, fname, reads, writes, **kw)

        def pool(fname, reads, writes, **kw):
            return eop("pool", P, fname, reads, writes, **kw)

        def pe(fname, reads, writes, **kw):
            return eop("pe", T, fname, reads, writes, **kw)

        def dma(semname, out, in_, reads=(), writes=(), eng="sp"):
            return S_.dma(eng, dsem(semname), out, in_, reads, writes)

        cst = AR.alloc((NCONST,), F32)
        Bcst = Buf("cst")
        ident_bf = AR.alloc((128,), BF16)
        bh_bf = AR.alloc((8, 256), BF16)
        e_bf = AR.alloc((16, 128), BF16)
        neg_bf = AR.alloc((128,), BF16)
        mhalf = AR.alloc((8,), F32)
        Bk = Buf("constbf")
        par = {}
        Bpar = Buf("par")
        for l in range(depth):
            par[l] = dict(
                lnwT=AR.alloc((8,), F32), cwT=AR.alloc((12, 4), F32), cbT=AR.alloc((12,), F32),
                qkw=AR.alloc((2,), F32), snwT=AR.alloc((8,), F32), hp=AR.alloc((48,), F32),
                qws=AR.alloc((1,), F32), aneg=AR.alloc((16,), F32))
        dtraw = AR.alloc((NT, 16), F32)
        Bdtraw = Buf("dtraw")

        identf = cst[:, C_IDENT:C_IDENT + 128]

        dma("cst", cst, consts_d[:, :], writes=[Bcst])
        for l in range(depth):
            pl = par[l]
            dma("par", pl["lnwT"], lnwT_d[l], writes=[Bpar])
            dma("par", pl["cwT"], cwT_d[l].rearrange("p (c k) -> p c k", k=4), writes=[Bpar])
            dma("par", pl["cbT"], cbT_d[l], writes=[Bpar])
            dma("par", pl["qkw"], qkw_d[l], writes=[Bpar])
            dma("par", pl["snwT"], snwT_d[l], writes=[Bpar])
            dma("par", pl["hp"], hp_d[l].partition_broadcast(128), writes=[Bpar])
        pool("tensor_copy", [Bcst], [Bk], out=ident_bf, in_=identf)
        pool("tensor_copy", [Bcst], [Bk], out=bh_bf, in_=cst[:, C_BH:C_BH + 2048].rearrange("p (h q) -> p h q", q=256))
        pool("tensor_copy", [Bcst], [Bk], out=e_bf, in_=cst[:, C_E:C_E + 2048].rearrange("p (n k) -> p n k", k=128))
        pool("tensor_copy", [Bcst], [Bk], out=neg_bf, in_=cst[:, C_NEG:C_NEG + 128])
        pool("memset", [], [Bk], ap=mhalf, constant=-0.5)
        for l in range(depth):
            pl = par[l]
            pool("tensor_scalar", [Bpar], [Bpar], out=pl["qws"], in0=pl["qkw"][:, 0:1], scalar1=float(128 ** -0.5), scalar2=None, op0=ALU.mult)
            act("activation", [Bpar], [Bpar], out=pl["aneg"], in_=pl["hp"][:, 16:32], func=AF.Exp)
            pool("tensor_scalar", [Bpar], [Bpar], out=pl["aneg"], in0=pl["aneg"], scalar1=-1.0, scalar2=None, op0=ALU.mult)
        S_.barrier()

        base_mark = AR.mark()
        p3_mark = base_mark

        for l in range(depth):
            pl = par[l]
            xsrc = x_in if l == 0 else xres_d
            xdst = xres_d if l < depth - 1 else out_d
            AR.release(base_mark)
            hT = AR.alloc((KC, S), BF16)
            BhT = Buf("hT")
            m1 = AR.mark()
            xt = [AR.alloc((D,), F32) for _ in range(2)]
            Bxt = [Buf("xt0"), Buf("xt1")]
            xn = [AR.alloc((D,), BF16) for _ in range(2)]
            Bxn = [Buf("xn0"), Buf("xn1")]
            junk = AR.alloc((D,), BF16)
            Bjunk = Buf("junk")
            ss1 = [AR.alloc((1,), F32) for _ in range(2)]
            Bss1 = [Buf("ss0"), Buf("ss1")]

            def p1_load(i):
                s = i % 2
                dma(f"xt{s}", xt[s], xsrc[i * 128:(i + 1) * 128, :], writes=[Bxt[s]])

            def p1_norm(i):
                s = i % 2
                act("activation", [Bxt[s]], [Bjunk, Bss1[s]], out=junk, in_=xt[s], func=AF.Square, accum_out=ss1[s])
                pool("tensor_scalar", [Bss1[s]], [Bss1[s]], out=ss1[s], in0=ss1[s], scalar1=1.0 / D, scalar2=EPS, op0=ALU.mult, op1=ALU.add)
                pool("tensor_tensor", [Bss1[s], Bk], [Bss1[s]], out=ss1[s], in0=ss1[s], in1=mhalf[:, 0:1], op=ALU.pow)
                dve("tensor_scalar", [Bxt[s], Bss1[s]], [Bxn[s]], out=xn[s], in0=xt[s], scalar1=ss1[s][:, 0:1], scalar2=None, op0=ALU.mult)

            def p1_tr(i):
                s = i % 2
                bk = 6 + (i % 2)
                pbt = pb[bk].bitcast(BF16)
                for c in range(KC):
                    pe("transpose", [Bxn[s], Bk], [Bpb[bk]], out=pbt[:, c * 128:(c + 1) * 128], in_=xn[s][:, c * 128:(c + 1) * 128], identity=ident_bf)
                dve("tensor_tensor", [Bpar], [Bpb[bk], BhT], out=hT[:, :, i * 128:(i + 1) * 128],
                    in0=pbt.rearrange("p (c t) -> p c t", t=128),
                    in1=pl["lnwT"].unsqueeze(2).to_broadcast([128, KC, 128]), op=ALU.mult)

            p1_load(0)
            for t in range(NT + 1):
                if t + 1 < NT:
                    p1_load(t + 1)
                if t < NT:
                    p1_norm(t)
                if t >= 1:
                    p1_tr(t - 1)
            if dbg and l == 0:
                dma("dbg", hT_d[:, :], hT.rearrange("p a b -> p (a b)"), reads=[BhT])
            if stop_after == "P1":
                break

            wst = [AR.alloc((KC, 512), F32) for _ in range(2)]
            Bwst = [Buf("wst0"), Buf("wst1")]
            wbf = [AR.alloc((KC, 512), BF16) for _ in range(2)]
            Bwbf = [Buf("wbf0"), Buf("wbf1")]
            NG = 14
            w_l = w_in[l]

            def gcols(g):
                c0 = g * 512
                return c0, min(512, NPROJ - c0)

            def wload(g):
                s = g % 2
                c0, ncl = gcols(g)
                dma(f"wst{s}", wst[s][:, :, 0:ncl], w_l[:, c0:c0 + ncl].rearrange("(k p) n -> p k n", p=128), writes=[Bwst[s]])

            def wconv(g):
                s = g % 2
                c0, ncl = gcols(g)
                pool("tensor_copy", [Bwst[s]], [Bwbf[s]], out=wbf[s][:, :, 0:ncl], in_=wst[s][:, :, 0:ncl])

            ss4 = [AR.alloc((4,), F32) for _ in range(2)]
            Bss4 = [Buf("ss4a"), Buf("ss4b")]
            sq = [AR.alloc((512,), F32) for _ in range(2)]
            Bsq = [Buf("sq0"), Buf("sq1")]
            qn = [AR.alloc((4, 128), BF16) for _ in range(2)]
            Bqn = [Buf("qn0"), Buf("qn1")]
            qst = [AR.alloc((4, 512), BF16) for _ in range(2)]
            Bqst = [Buf("qst0"), Buf("qst1")]
            vst = [AR.alloc((512,), BF16) for _ in range(2)]
            Bvst = [Buf("vst0"), Buf("vst1")]
            fst = [AR.alloc((512,), F32) for _ in range(2)]
            Bfst = [Buf("fst0"), Buf("fst1")]
            ubuf = [AR.alloc((515,), F32) for _ in range(2)]
            Bu = [Buf("u0"), Buf("u1")]
            cacc = [AR.alloc((512,), F32) for _ in range(2)]
            Bcacc = [Buf("cacc0"), Buf("cacc1")]
            xo_f = [AR.alloc((512,), F32) for _ in range(2)]
            xo_b = [AR.alloc((512,), BF16) for _ in range(2)]
            Bxo = [Buf("xo0"), Buf("xo1")]
            accn = [0]

            def next_acc():
                b = accn[0] % 4
                accn[0] += 1
                return b

            def mm_tok(g, i, ncl, bk):
                s = g % 2
                for k in range(KC):
                    pe("matmul", [BhT, Bwbf[s]], [Bpb[bk]], out=pb[bk][:, 0:ncl], lhsT=hT[:, k, i * 128:(i + 1) * 128],
                       rhs=wbf[s][:, k, 0:ncl], start=(k == 0), stop=(k == KC - 1))

            def do_qk(g):
                which = 0 if g < 2 else 1
                h0 = (g % 2) * 4
                dst = qT_d if which == 0 else kT_d
                wsc = pl["qws"] if which == 0 else pl["qkw"][:, 1:2]
                banks = {}

                def s0(i):
                    bk = next_acc()
                    banks[i] = bk
                    mm_tok(g, i, 512, bk)
                    s = i % 2
                    act("activation", [], [Bpb[bk], Bsq[s]], out=sq[s], in_=pb[bk], func=AF.Square)
                    dve("tensor_reduce", [Bsq[s]], [Bss4[s]], out=ss4[s], in_=sq[s].rearrange("p (h d) -> p h d", d=128), axis=AX.X, op=ALU.add)
                    pool("tensor_scalar", [], [Bss4[s]], out=ss4[s], in0=ss4[s], scalar1=1.0 / 128, scalar2=EPS, op0=ALU.mult, op1=ALU.add)
                    pool("tensor_tensor", [Bk], [Bss4[s]], out=ss4[s], in0=ss4[s], in1=mhalf[:, 0:4], op=ALU.pow)
                    dve("tensor_tensor", [Bss4[s]], [Bpb[bk], Bqn[s]], out=qn[s], in0=pb[bk].rearrange("p (h d) -> p h d", d=128),
                        in1=ss4[s].unsqueeze(2).to_broadcast([128, 4, 128]), op=ALU.mult)

                def s2(i):
                    s = i % 2
                    bk = 4 + (i % 2)
                    pbt = pb[bk].bitcast(BF16)
                    for hh in range(4):
                        pe("transpose", [Bqn[s], Bk], [Bpb[bk]], out=pbt[:, hh * 128:(hh + 1) * 128], in_=qn[s][:, hh, :], identity=ident_bf)
                    ss_ = (i // 4) % 2
                    j = i % 4
                    act("activation", [Bpar], [Bpb[bk], Bqst[ss_]], out=qst[ss_][:, :, j * 128:(j + 1) * 128],
                        in_=pbt[:, 0:512].rearrange("p (h t) -> p h t", t=128), func=AF.Copy, scale=wsc)
                    if j == 3:
                        tg = i // 4
                        dma(f"qst{ss_}", dst[h0:h0 + 4, :, tg * 512:(tg + 1) * 512].rearrange("h p t -> p h t"), qst[ss_], reads=[Bqst[ss_]])

                pipeline(NT, [(2, s2), (0, s0)])

            def do_tok(g, kind):
                c0 = (g % 2) * 512

                def s0(i):
                    bk = next_acc()
                    mm_tok(g, i, 512, bk)
                    s = i % 2
                    if kind == "v":
                        act("activation", [], [Bpb[bk], Bvst[s]], out=vst[s], in_=pb[bk], func=AF.Copy)
                        dma(f"vst{s}", V_d[i * 128:(i + 1) * 128, c0:c0 + 512], vst[s], reads=[Bvst[s]])
                    else:
                        act("activation", [], [Bpb[bk], Bfst[s]], out=fst[s], in_=pb[bk], func=AF.Silu)
                        dd = sg_d if kind == "g" else sz_d
                        dma(f"fst{s}", dd[i * 128:(i + 1) * 128, c0:c0 + 512], fst[s], reads=[Bfst[s]])

                pipeline(NT, [(0, s0)])

            def do_feat(g):
                s_w = g % 2
                nloc = 4
                items = [(cl, tg) for cl in range(nloc) for tg in range(TG)]

                def s0(ix):
                    cl, tg = items[ix]
                    cc = (g - 10) * 4 + cl
                    bk = next_acc()
                    for k in range(KC):
                        pe("matmul", [BhT, Bwbf[s_w]], [Bpb[bk]], out=pb[bk], lhsT=wbf[s_w][:, k, cl * 128:(cl + 1) * 128],
                           rhs=hT[:, k, tg * 512:(tg + 1) * 512], start=(k == 0), stop=(k == KC - 1))
                    s = ix % 2
                    if tg == 0:
                        pool("memset", [], [Bu[s]], ap=ubuf[s][:, 0:3], constant=0.0)
                    else:
                        pool("tensor_copy", [Bu[1 - s]], [Bu[s]], out=ubuf[s][:, 0:3], in_=ubuf[1 - s][:, 512:515])
                    act("activation", [], [Bpb[bk], Bu[s]], out=ubuf[s][:, 3:515], in_=pb[bk], func=AF.Copy)
                    cw = pl["cwT"]
                    act("activation", [Bu[s]], [Bcacc[s]], out=cacc[s], in_=ubuf[s][:, 3:515], func=AF.Copy, scale=cw[:, cc, 3:4])
                    for kk in (2, 1, 0):
                        dve("scalar_tensor_tensor", [Bu[s], Bpar], [Bcacc[s]], out=cacc[s], in0=ubuf[s][:, kk:kk + 512], scalar=cw[:, cc, kk:kk + 1],
                            in1=cacc[s], op0=ALU.mult, op1=ALU.add)
                    if cc < 8:
                        act("activation", [Bcacc[s], Bpar], [Bxo[s]], out=xo_f[s], in_=cacc[s], func=AF.Silu, bias=pl["cbT"][:, cc:cc + 1])
                        dma(f"xo{s}", xsT_d[cc * 128:(cc + 1) * 128, tg * 512:(tg + 1) * 512], xo_f[s], reads=[Bxo[s]])
                    else:
                        act("activation", [Bcacc[s], Bpar], [Bxo[s]], out=xo_b[s], in_=cacc[s], func=AF.Silu, bias=pl["cbT"][:, cc:cc + 1])
                        dma(f"xo{s}", bcT_d[(cc - 8) * 128:(cc - 7) * 128, tg * 512:(tg + 1) * 512], xo_b[s], reads=[Bxo[s]])

                pipeline(len(items), [(0, s0)])

            def do_dt(g):
                def s0(i):
                    bk = next_acc()
                    mm_tok(g, i, 16, bk)
                    dve("tensor_copy", [], [Bpb[bk], Bdtraw], out=dtraw[:, i, :], in_=pb[bk][:, 0:16])
                pipeline(NT, [(0, s0)])

            wload(0)
            wconv(0)
            wload(1)
            for g in range(NG):
                if g + 1 < NG:
                    wconv(g + 1)
                if g + 2 < NG:
                    wload(g + 2)
                if g < 4:
                    do_qk(g)
                elif g < 6:
                    do_tok(g, "v")
                elif g < 8:
                    do_tok(g, "g")
                elif g < 10:
                    do_tok(g, "z")
                elif g < 13:
                    do_feat(g)
                else:
                    do_dt(g)
            if dbg:
                dma("dbg", dtraw_d[:, :], dtraw.rearrange("p a b -> p (a b)"), reads=[Bdtraw])
            S_.barrier()
            if stop_after == "P2":
                break
            AR.release(p3_mark)
            qT_h = [AR.alloc((S,), BF16) for _ in range(2)]
            kT_h = [AR.alloc((S,), BF16) for _ in range(2)]
            V_h = [AR.alloc((NT, 129), BF16) for _ in range(2)]
            sg_h = [AR.alloc((NT, 128), F32) for _ in range(2)]
            Bq = [Buf("q0"), Buf("q1")]
            Bkk = [Buf("k0"), Buf("k1")]
            Bv = [Buf("v0"), Buf("v1")]
            Bsg = [Buf("sg0"), Buf("sg1")]
            attnT = [AR.alloc((S,), BF16) for _ in range(2)]
            Battn = [Buf("at0"), Buf("at1")]
            maskT = [AR.alloc((S,), BF16) for _ in range(2)]
            Bmask = [Buf("mk0"), Buf("mk1")]
            km = AR.alloc((16,), F32)
            kms = AR.alloc((16,), F32)
            kmh = [AR.alloc((16,), BF16) for _ in range(2)]
            kml = [AR.alloc((16,), BF16) for _ in range(2)]
            Bkm = Buf("km")
            Bkmhl = [Buf("kmhl0"), Buf("kmhl1")]
            gm = [AR.alloc((16,), F32) for _ in range(2)]
            top8 = [AR.alloc((8,), F32) for _ in range(2)]
            thr = [AR.alloc((1,), F32) for _ in range(2)]
            mk = [AR.alloc((16,), BF16) for _ in range(2)]
            Bgm = [Buf("gm0"), Buf("gm1")]
            Bmkk = [Buf("mkk0"), Buf("mkk1")]
            NPT = 6
            PT = [AR.alloc((512,), BF16) for _ in range(NPT)]
            BPT = [Buf(f"pt{i}") for i in range(NPT)]
            tsc = [AR.alloc((129,), F32) for _ in range(2)]
            num = [AR.alloc((129,), F32) for _ in range(2)]
            rden = [AR.alloc((1,), F32) for _ in range(2)]
            o_bf = [AR.alloc((128,), BF16) for _ in range(2)]
            Bcmb = [Buf("cmb0"), Buf("cmb1")]
            Bobf = [Buf("obf0"), Buf("obf1")]

            pool("memset", [], [Bkm], ap=km, constant=0.0)
            for s in range(2):
                pool("memset", [], [Bv[s]], ap=V_h[s][:, :, 128:129], constant=1.0)
                pool("memset", [], [Bmask[s]], ap=maskT[s], constant=0.0)

            def head_load(h):
                s = h % 2
                dma(f"hq{s}", qT_h[s], qT_d[h], writes=[Bq[s]])
                dma(f"hk{s}", kT_h[s], kT_d[h], writes=[Bkk[s]])
                dma(f"hv{s}", V_h[s][:, :, 0:128], V_d[:, h * 128:(h + 1) * 128].rearrange("(t p) d -> p t d", p=128), writes=[Bv[s]])
                dma(f"hg{s}", sg_h[s], sg_d[:, h * 128:(h + 1) * 128].rearrange("(t p) d -> p t d", p=128), writes=[Bsg[s]])

            def head_kmean(h):
                s = h % 2
                dve("tensor_reduce", [Bkk[s]], [Bkm], out=km[:, 0:NB], in_=kT_h[s].rearrange("p (n k) -> p n k", k=256), axis=AX.X, op=ALU.add)
                dve("tensor_scalar", [], [Bkm], out=kms, in0=km, scalar1=1.0 / 256, scalar2=None, op0=ALU.mult)
                dve("tensor_copy", [Bkm], [Bkmhl[s]], out=kmh[s], in_=kms)
                dve("tensor_tensor", [Bkm], [Bkmhl[s]], out=kml[s], in0=kms, in1=kmh[s], op=ALU.subtract)

            def gate_front(h, qb):
                s = h % 2
                for j in range(2):
                    i = 2 * qb + j
                    pe("matmul", [Bq[s], Bkmhl[s]], [Bpb[6]], out=pb[6][:, j * 16:(j + 1) * 16], lhsT=qT_h[s][:, i * 128:(i + 1) * 128], rhs=kmh[s], start=True, stop=False)
                    pe("matmul", [Bq[s], Bkmhl[s]], [Bpb[6]], out=pb[6][:, j * 16:(j + 1) * 16], lhsT=qT_h[s][:, i * 128:(i + 1) * 128], rhs=kml[s], start=False, stop=True)
                for j in range(2):
                    dve("tensor_tensor", [], [Bpb[6], Bgm[j]], out=gm[j], in0=pb[6][:, j * 16:(j + 1) * 16], in1=cst[:, C_PAST + qb * 16:C_PAST + (qb + 1) * 16], op=ALU.add)
                    dve("max", [], [Bgm[j]], out=top8[j], in_=gm[j])
                    dve("tensor_scalar", [], [Bgm[j]], out=thr[j], in0=top8[j][:, 2:3], scalar1=-1e29, scalar2=None, op0=ALU.max)
                    dve("tensor_scalar", [Bgm[j]], [Bmkk[j]], out=mk[j], in0=gm[j], scalar1=thr[j][:, 0:1], scalar2=NEGBIG, op0=ALU.is_lt, op1=ALU.mult)

            def gate_back(h, qb):
                s = h % 2
                pbt = pb[7].bitcast(BF16)
                for j in range(2):
                    pe("transpose", [Bmkk[j]], [Bpb[7]], out=pbt[0:16, j * 128:(j + 1) * 128], in_=mk[j], identity=ident_bf)
                dve("tensor_copy", [], [Bpb[7], Bmask[s]], out=maskT[s][0:16, qb * 256:(qb + 1) * 256], in_=pbt[0:16, 0:256])

            deferred = []

            def defer(n, fn):
                deferred.append([n, fn])

            def tick():
                for d_ in list(deferred):
                    d_[0] -= 1
                    if d_[0] <= 0:
                        deferred.remove(d_)
                        d_[1]()

            def flush():
                while deferred:
                    d_ = deferred.pop(0)
                    d_[1]()

            def attn_head(h, first, last_head):
                s = h % 2
                NP = S // 512
                items = []
                for p in range(NP):
                    for kt in range(4 * p):
                        items.append(("pc", p, kt))
                    items.append(("own0", 2 * p, 4 * p))
                    items.append(("own1", 2 * p, 4 * p + 1))
                    items.append(("po", p, 4 * p))
                    items.append(("po", p, 4 * p + 1))
                    items.append(("own0", 2 * p + 1, 4 * p + 2))
                    items.append(("own1", 2 * p + 1, 4 * p + 3))
                slots = {}
                seen_pair = set()
                seen_po = set()

                def exp_past(sb_, ps_, c0, qb, n, kt):
                    bcol = C_BIAS + h * 30 + (qb - n - 1) * 2 + (kt % 2)
                    act("activation", [], [Bpb[sb_], BPT[ps_]], out=PT[ps_][:, c0:c0 + 256], in_=pb[sb_][:, c0:c0 + 256], func=AF.Exp, bias=cst[:, bcol:bcol + 1])

                def stA(ix):
                    kind, a0, kt = items[ix]
                    sb_ = ix % 3
                    ps_ = ix % NPT
                    slots[ix] = ps_
                    if not last_head:
                        if kind in ("pc", "own0") and (a0 if kind == "pc" else a0 // 2) not in seen_pair and (kind == "pc" or a0 % 2 == 0):
                            p_ = a0 if kind == "pc" else a0 // 2
                            seen_pair.add(p_)
                            gate_front(h + 1, 2 * p_)
                            defer(4, (lambda qb=2 * p_: gate_back(h + 1, qb)))
                        if kind == "po" and a0 not in seen_po:
                            seen_po.add(a0)
                            gate_front(h + 1, 2 * a0 + 1)
                            defer(4, (lambda qb=2 * a0 + 1: gate_back(h + 1, qb)))
                    n = kt // 2
                    if kind == "pc":
                        p = a0
                        qpair = qT_h[s][:, p * 512:(p + 1) * 512]
                        pe("matmul", [Bq[s], Bkk[s]], [Bpb[sb_]], out=pb[sb_], lhsT=kT_h[s][:, kt * 128:(kt + 1) * 128], rhs=qpair, start=True, stop=False)
                        pe("matmul", [Bmask[s]], [Bpb[sb_]], out=pb[sb_], lhsT=e_bf[0:16, n, :], rhs=maskT[s][0:16, p * 512:(p + 1) * 512], start=False, stop=True)
                        exp_past(sb_, ps_, 0, 2 * p, n, kt)
                        exp_past(sb_, ps_, 256, 2 * p + 1, n, kt)
                    elif kind == "po":
                        qb = 2 * a0 + 1
                        qblk = qT_h[s][:, qb * 256:(qb + 1) * 256]
                        pe("matmul", [Bq[s], Bkk[s]], [Bpb[sb_]], out=pb[sb_][:, 0:256], lhsT=kT_h[s][:, kt * 128:(kt + 1) * 128], rhs=qblk, start=True, stop=False)
                        pe("matmul", [Bmask[s]], [Bpb[sb_]], out=pb[sb_][:, 0:256], lhsT=e_bf[0:16, n, :], rhs=maskT[s][0:16, qb * 256:(qb + 1) * 256], start=False, stop=True)
                        exp_past(sb_, ps_, 0, qb, n, kt)
                    elif kind == "own0":
                        qb = a0
                        qblk = qT_h[s][:, qb * 256:(qb + 1) * 256]
                        pe("matmul", [Bq[s], Bkk[s]], [Bpb[sb_]], out=pb[sb_][:, 0:256], lhsT=kT_h[s][:, kt * 128:(kt + 1) * 128], rhs=qblk, start=True, stop=False)
                        pe("matmul", [], [Bpb[sb_]], out=pb[sb_][:, 0:256], lhsT=ident_bf, rhs=bh_bf[:, h, :], start=False, stop=True)
                        act("activation", [], [Bpb[sb_], BPT[ps_]], out=PT[ps_][:, 0:256], in_=pb[sb_][:, 0:256], func=AF.Exp)
                    else:
                        qb = a0
                        qblk = qT_h[s][:, qb * 256:(qb + 1) * 256]
                        pe("matmul", [Bq[s], Bkk[s]], [Bpb[sb_]], out=pb[sb_][:, 0:128], lhsT=kT_h[s][:, kt * 128:(kt + 1) * 128], rhs=qblk[:, 128:256], start=True, stop=False)
                        pe("matmul", [], [Bpb[sb_]], out=pb[sb_][:, 0:128], lhsT=ident_bf, rhs=bh_bf[:, h, 0:128], start=False, stop=True)
                        act("activation", [], [Bpb[sb_], BPT[ps_]], out=PT[ps_][:, 0:128], in_=pb[sb_][:, 0:128], func=AF.Exp)
                    tick()

                def stB(ix):
                    kind, a0, kt = items[ix]
                    ps_ = slots[ix]
                    if kind == "pc":
                        p = a0
                        for j in range(4):
                            bk = 3 + j // 2
                            c0 = (j % 2) * 256
                            pe("matmul", [BPT[ps_], Bv[s]], [Bpb[bk]], out=pb[bk][:, c0:c0 + 129], lhsT=PT[ps_][:, j * 128:(j + 1) * 128], rhs=V_h[s][:, kt, :],
                               start=(kt == 0 and j % 2 == 0), stop=(j < 2 and kt == 4 * p - 1), skip_group_check=True)
                    elif kind == "po":
                        p = a0
                        for jj in range(2):
                            c0 = jj * 256
                            pe("matmul", [BPT[ps_], Bv[s]], [Bpb[4]], out=pb[4][:, c0:c0 + 129], lhsT=PT[ps_][:, jj * 128:(jj + 1) * 128], rhs=V_h[s][:, kt, :],
                               start=(kt == 0 and jj == 0), stop=(kt == 4 * p + 1), skip_group_check=True)
                    elif kind == "own0":
                        pe("matmul", [BPT[ps_], Bv[s]], [Bpb[5]], out=pb[5][:, 0:129], lhsT=PT[ps_][:, 0:128], rhs=V_h[s][:, kt, :], start=True, stop=True)
                        pe("matmul", [BPT[ps_], Bv[s]], [Bpb[5]], out=pb[5][:, 256:385], lhsT=PT[ps_][:, 128:256], rhs=V_h[s][:, kt, :], start=True, stop=False)
                    else:
                        qb = a0
                        pe("matmul", [BPT[ps_], Bv[s]], [Bpb[5]], out=pb[5][:, 256:385], lhsT=PT[ps_][:, 0:128], rhs=V_h[s][:, kt, :], start=False, stop=True)
                        obk = 3 + (qb % 2)
                        for j in range(2):
                            i = 2 * qb + j
                            if qb > 0:
                                gcol = C_G + h * 2 + j
                                act("activation", [], [Bpb[obk], Bcmb[j]], out=tsc[j], in_=pb[obk][:, j * 256:j * 256 + 129], func=AF.Copy, scale=cst[:, gcol:gcol + 1])
                                dve("tensor_tensor", [], [Bpb[5], Bcmb[j]], out=num[j], in0=tsc[j], in1=pb[5][:, j * 256:j * 256 + 129], op=ALU.add)
                            else:
                                dve("tensor_copy", [], [Bpb[5], Bcmb[j]], out=num[j], in_=pb[5][:, j * 256:j * 256 + 129])
                            dve("reciprocal", [], [Bcmb[j]], out=rden[j], in_=num[j][:, 128:129])
                            dve("scalar_tensor_tensor", [Bcmb[j], Bsg[s]], [Bobf[j]], out=o_bf[j], in0=num[j][:, 0:128], scalar=rden[j][:, 0:1], in1=sg_h[s][:, i, :],
                                op0=ALU.mult, op1=ALU.mult)

                        def back(qb=qb):
                            pbt = pb[7].bitcast(BF16)
                            for j in range(2):
                                pe("transpose", [Bobf[j]], [Bpb[7]], out=pbt[:, 512 + j * 128:512 + (j + 1) * 128], in_=o_bf[j], identity=ident_bf)
                            dve("tensor_copy", [], [Bpb[7], Battn[s]], out=attnT[s][:, qb * 256:(qb + 1) * 256], in_=pbt[:, 512:768])
                        defer(3, back)

                pipeline(len(items), [(2, stB), (0, stA)])
                flush()
                dma(f"at{s}", mixT_d[h], attnT[s], reads=[Battn[s]])

            head_load(0)
            head_kmean(0)
            for qb in range(NB):
                gate_front(0, qb)
                gate_back(0, qb)
            for h in range(8):
                if h + 1 < 8:
                    head_load(h + 1)
                    head_kmean(h + 1)
                attn_head(h, h == 0, h == 7)
            S_.barrier()
            if stop_after == "P3":
                break
            AR.release(p3_mark)
            wo_bf = AR.alloc((16, D), BF16)
            Bwo = Buf("wo")
            wos = [AR.alloc((2, D), F32) for _ in range(2)]
            Bwos = [Buf("wos0"), Buf("wos1")]
            wo_l = w_out[l]
            p5_mark = AR.mark()
            for q8 in range(8):
                s = q8 % 2
                dma(f"wos{s}", wos[s], wo_l[q8 * 256:(q8 + 1) * 256, :].rearrange("(k p) n -> p k n", p=128), writes=[Bwos[s]])
                pool("tensor_copy", [Bwos[s]], [Bwo], out=wo_bf[:, q8 * 2:(q8 + 1) * 2, :], in_=wos[s])
            NCH = NT
            hp = pl["hp"]
            xsT_c = [AR.alloc((8, 128), F32) for _ in range(2)]
            bcT_c = [AR.alloc((4, 128), BF16) for _ in range(3)]
            sz_c = [AR.alloc((D,), F32) for _ in range(3)]
            Bxs = [Buf("xsc0"), Buf("xsc1"), Buf("xsc2")]
            Bbc = [Buf("bcc0"), Buf("bcc1"), Buf("bcc2")]
            Bsz = [Buf("szc0"), Buf("szc1"), Buf("szc2")]
            x1a = AR.alloc((NT, 16), F32)
            axa = AR.alloc((NT, 16), F32)
            lga = AR.alloc((NT, 16), F32)
            dta = AR.alloc((NT, 16), F32)
            dAa = AR.alloc((NT, 16), F32)
            acs_a = AR.alloc((NT, 16), F32)
            aend_a = AR.alloc((NT, 16), F32)
            dse_a = AR.alloc((NT, 16), F32)
            nacs_a = AR.alloc((NT, 16), F32)
            ea_a = AR.alloc((NT, 16), F32)
            cd_a = AR.alloc((NT, 16), F32)
            eds_a = AR.alloc((NT, 16), F32)
            dtw_a = AR.alloc((NT, 16), F32)
            Bdt = Buf("dtc")
            Rm = AR.alloc((16, 128), F32)
            BR = Buf("R")
            negb4 = AR.alloc((4, 128), BF16)
            decay = AR.alloc((16, 128), BF16)
            Bdecay = Buf("decay")
            MT = [AR.alloc((16, 128), BF16) for _ in range(2)]
            BMT = [Buf("MT0"), Buf("MT1")]
            cb_sb = AR.alloc((2, 128), BF16)
            Bcb = Buf("cb")
            Btok = [AR.alloc((2, 128), BF16) for _ in range(2)]
            BBtok = [Buf("btok0"), Buf("btok1")]
            xs_sb = AR.alloc((16, 64), F32)
            Bxssb = Buf("xssb")
            xdt = [AR.alloc((16, 64), BF16) for _ in range(2)]
            xw = [AR.alloc((16, 64), BF16) for _ in range(2)]
            xD = [AR.alloc((16, 64), F32) for _ in range(2)]
            Bxdt = [Buf("xdt0"), Buf("xdt1")]
            Bxw = [Buf("xw0"), Buf("xw1")]
            BxD = [Buf("xD0"), Buf("xD1")]
            prev = AR.alloc((16, 64), F32)
            prevbf = AR.alloc((16, 64), BF16)
            Bprev = Buf("prev")
            Bprevbf = Buf("prevbf")
            t1 = AR.alloc((16, 64), F32)
            Bt1 = Buf("t1")
            yg = t1
            Byg = Bt1
            ss2 = AR.alloc((2,), F32)
            Bss2 = Buf("ss2")
            yn2 = [AR.alloc((D,), BF16) for _ in range(2)]
            Byn2 = [Buf("yn0"), Buf("yn1")]
            ystage = [AR.alloc((8, 512), BF16) for _ in range(2)]
            Byst = [Buf("yst0"), Buf("yst1")]
            junk4 = AR.alloc((512,), BF16)
            Bjunk4 = Buf("junk4")
            ones_f = cst[:, C_ONES:C_ONES + 128]
            U_f = cst[:, C_U:C_U + 128]

            pool("memset", [], [Bprev], ap=prev, constant=0.0)
            pool("memset", [], [Bprevbf], ap=prevbf, constant=0.0)
            pool("tensor_copy", [], [BR], out=negb4, in_=cst[:, C_NEG:C_NEG + 128].unsqueeze(1).to_broadcast([128, 4, 128]))

            NW = NT * 16
            fl = lambda t: t.rearrange("p a b -> p (a b)")
            pool("tensor_tensor", [Bdtraw], [Bdt], out=x1a, in0=dtraw, in1=hp[:, 0:16].unsqueeze(1).to_broadcast([128, NT, 16]), op=ALU.add)
            act("activation", [], [Bdt], out=axa, in_=x1a, func=AF.Abs)
            act("activation", [], [Bdt], out=axa, in_=axa, func=AF.Exp, scale=-1.0)
            act("activation", [], [Bdt], out=lga, in_=axa, func=AF.Ln, bias=1.0)
            dve("tensor_scalar", [], [Bdt], out=x1a, in0=x1a, scalar1=0.0, scalar2=None, op0=ALU.max)
            dve("tensor_tensor", [], [Bdt], out=dta, in0=x1a, in1=lga, op=ALU.add)
            dve("tensor_tensor", [], [Bdt], out=dAa, in0=dta, in1=pl["aneg"].unsqueeze(1).to_broadcast([128, NT, 16]), op=ALU.mult)
            pe("matmul", [Bdt], [Bpb[2]], out=pb[2][:, 0:NW], lhsT=U_f, rhs=fl(dAa), start=True, stop=True)
            pe("matmul", [Bdt], [Bpb[3]], out=pb[3][:, 0:NW], lhsT=ones_f, rhs=fl(dAa), start=True, stop=True)
            act("activation", [], [Bpb[2], Bdt], out=fl(acs_a), in_=pb[2][:, 0:NW], func=AF.Copy)
            act("activation", [], [Bpb[3], Bdt], out=fl(aend_a), in_=pb[3][:, 0:NW], func=AF.Copy)
            dve("tensor_tensor", [], [Bdt], out=dse_a, in0=aend_a, in1=acs_a, op=ALU.subtract)
            dve("tensor_scalar", [], [Bdt], out=nacs_a, in0=acs_a, scalar1=-1.0, scalar2=None, op0=ALU.mult)
            act("activation", [], [Bdt], out=ea_a, in_=acs_a, func=AF.Exp)
            act("activation", [], [Bdt], out=cd_a, in_=aend_a, func=AF.Exp)
            act("activation", [], [Bdt], out=eds_a, in_=dse_a, func=AF.Exp)
            dve("tensor_tensor", [], [Bdt], out=dtw_a, in0=dta, in1=eds_a, op=ALU.mult)

            def p4_load(c):
                s = c % 3
                dma(f"xsc{c % 2}", xsT_c[c % 2], xsT_d[:, c * 128:(c + 1) * 128].rearrange("(k p) t -> p k t", p=128), writes=[Bxs[c % 2]])
                dma(f"bcc{s}", bcT_c[s], bcT_d[:, c * 128:(c + 1) * 128].rearrange("(k p) t -> p k t", p=128), writes=[Bbc[s]])
                dma(f"szc{s}", sz_c[s], sz_d[c * 128:(c + 1) * 128, :], writes=[Bsz[s]])

            def stA(c):
                s = c % 2
                s3 = c % 3
                pool("tensor_tensor", [Bdt], [BR], out=Rm, in0=U_f.unsqueeze(1).to_broadcast([128, 16, 128]),
                     in1=dAa[:, c, :].unsqueeze(2).to_broadcast([128, 16, 128]), op=ALU.mult)
                for g in range(2):
                    pe("matmul", [Bbc[s3]], [Bpb[2]], out=pb[2][:, 64 + g * 128:64 + (g + 1) * 128], lhsT=bcT_c[s3][:, g, :], rhs=bcT_c[s3][:, 2 + g, :], start=True, stop=True)
                pbt2 = pb[2].bitcast(BF16)
                for g in range(2):
                    pe("transpose", [Bbc[s3]], [Bpb[2]], out=pbt2[:, 768 + g * 128:768 + (g + 1) * 128], in_=bcT_c[s3][:, g, :], identity=ident_bf)
                act("activation", [], [Bpb[2], Bcb], out=cb_sb, in_=pb[2][:, 64:320].rearrange("p (g l) -> p g l", l=128), func=AF.Copy)
                act("activation", [], [Bpb[2], BBtok[s]], out=Btok[s], in_=pbt2[:, 768:1024].rearrange("p (g l) -> p g l", l=128), func=AF.Copy)
                Rflat = Rm.rearrange("p h l -> p (h l)")
                for qd in range(4):
                    bk = qd % 2
                    pe("matmul", [BR], [Bpb[bk]], out=pb[bk], lhsT=ones_f, rhs=Rflat[:, qd * 512:(qd + 1) * 512], start=True, stop=False)
                    pe("matmul", [BR], [Bpb[bk]], out=pb[bk], lhsT=ident_bf, rhs=negb4.rearrange("p h l -> p (h l)"), start=False, stop=True)
                    for hh in range(4):
                        h = qd * 4 + hh
                        act("activation", [Bdt], [Bpb[bk], Bdecay], out=decay[:, h, :], in_=pb[bk][:, hh * 128:(hh + 1) * 128], func=AF.Exp, bias=nacs_a[:, c, h:h + 1])
                    g = qd // 2
                    dve("tensor_tensor", [Bdecay, Bcb], [BMT[s]], out=MT[s][:, qd * 4:(qd + 1) * 4, :], in0=decay[:, qd * 4:(qd + 1) * 4, :],
                        in1=cb_sb[:, g, :].unsqueeze(1).to_broadcast([128, 4, 128]), op=ALU.mult)
                for g in range(2):
                    for cc in range(4):
                        pe("transpose", [Bxs[s]], [Bpb[3]], out=pb[3][:, cc * 128:(cc + 1) * 128], in_=xsT_c[s][:, g * 4 + cc, :], identity=identf)
                    act("activation", [], [Bpb[3], Bxssb], out=xs_sb[:, g * 8:(g + 1) * 8, :], in_=pb[3].rearrange("p (h d) -> p h d", d=64), func=AF.Copy)
                    hs = slice(g * 8, (g + 1) * 8)
                    dve("tensor_tensor", [Bxssb, Bdt], [Bxdt[s]], out=xdt[s][:, hs, :], in0=xs_sb[:, hs, :],
                        in1=dta[:, c, hs].unsqueeze(2).to_broadcast([128, 8, 64]), op=ALU.mult)
                    pool("tensor_tensor", [Bxssb, Bdt], [Bxw[s]], out=xw[s][:, hs, :], in0=xs_sb[:, hs, :],
                         in1=dtw_a[:, c, hs].unsqueeze(2).to_broadcast([128, 8, 64]), op=ALU.mult)
                    pool("tensor_tensor", [Bxssb], [BxD[s]], out=xD[s][:, hs, :], in0=xs_sb[:, hs, :],
                         in1=hp[:, 32 + g * 8:32 + (g + 1) * 8].unsqueeze(2).to_broadcast([128, 8, 64]), op=ALU.mult)

            def stB1(c):
                s = c % 2
                s3 = c % 3
                for g in range(2):
                    hs = slice(g * 8, (g + 1) * 8)
                    for hh in range(8):
                        h = g * 8 + hh
                        pe("matmul", [BMT[s], Bxdt[s]], [Bpb[4]], out=pb[4][:, hh * 64:(hh + 1) * 64], lhsT=MT[s][:, h, :], rhs=xdt[s][:, h, :], start=True, stop=True)
                    pe("matmul", [Bbc[s3], Bprevbf], [Bpb[5]], out=pb[5], lhsT=bcT_c[s3][:, 2 + g, :], rhs=prevbf[:, hs, :].rearrange("p h d -> p (h d)"), start=True, stop=True)
                    pe("matmul", [BBtok[s], Bxw[s]], [Bpb[6]], out=pb[6], lhsT=Btok[s][:, g, :], rhs=xw[s][:, hs, :].rearrange("p h d -> p (h d)"), start=True, stop=True)
                    dve("tensor_tensor", [Bdt], [Bpb[5], Bt1], out=t1[:, hs, :], in0=pb[5].rearrange("p (h d) -> p h d", d=64),
                        in1=ea_a[:, c, hs].unsqueeze(2).to_broadcast([128, 8, 64]), op=ALU.mult)
                    dve("tensor_tensor", [], [Bpb[4], Bt1], out=t1[:, hs, :], in0=t1[:, hs, :], in1=pb[4].rearrange("p (h d) -> p h d", d=64), op=ALU.add)
                    dve("tensor_tensor", [BxD[s]], [Bt1], out=t1[:, hs, :], in0=t1[:, hs, :], in1=xD[s][:, hs, :], op=ALU.add)
                    pool("tensor_tensor", [Bt1, Bsz[s3]], [Byg], out=yg[:, hs, :], in0=t1[:, hs, :], in1=sz_c[s3][:, g * 512:(g + 1) * 512].rearrange("p (h d) -> p h d", d=64), op=ALU.mult)
                    act("activation", [Byg], [Bjunk4, Bss2], out=junk4, in_=yg[:, hs, :].rearrange("p h d -> p (h d)"), func=AF.Square, accum_out=ss2[:, g:g + 1])
                    dve("tensor_tensor", [Bdt], [Bprev], out=prev[:, hs, :], in0=prev[:, hs, :],
                        in1=cd_a[:, c, hs].unsqueeze(2).to_broadcast([128, 8, 64]), op=ALU.mult)
                    dve("tensor_tensor", [], [Bpb[6], Bprev], out=prev[:, hs, :], in0=prev[:, hs, :], in1=pb[6].rearrange("p (h d) -> p h d", d=64), op=ALU.add)
                    act("activation", [Bprev], [Bprevbf], out=prevbf[:, hs, :], in_=prev[:, hs, :], func=AF.Copy)
                pool("tensor_scalar", [], [Bss2], out=ss2, in0=ss2, scalar1=1.0 / 512, scalar2=EPS, op0=ALU.mult, op1=ALU.add)
                pool("tensor_tensor", [], [Bss2], out=ss2, in0=ss2, in1=mhalf[:, 0:2], op=ALU.pow)
                for g in range(2):
                    act("activation", [Byg, Bss2], [Byn2[s]], out=yn2[s][:, g * 512:(g + 1) * 512], in_=yg[:, g * 8:(g + 1) * 8, :].rearrange("p h d -> p (h d)"),
                        func=AF.Copy, scale=ss2[:, g:g + 1])

            def stB2(c):
                s = c % 2
                pbt7 = pb[7].bitcast(BF16)
                for cc in range(8):
                    pe("transpose", [Byn2[s]], [Bpb[7]], out=pbt7[:, cc * 128:(cc + 1) * 128], in_=yn2[s][:, cc * 128:(cc + 1) * 128], identity=ident_bf)
                ys = (c // 4) % 2
                j = c % 4
                dve("tensor_tensor", [], [Bpb[7], Byst[ys]], out=ystage[ys][:, :, j * 128:(j + 1) * 128], in0=pbt7.rearrange("p (c t) -> p c t", t=128),
                    in1=pl["snwT"].unsqueeze(2).to_broadcast([128, 8, 128]), op=ALU.mult)
                if j == 3:
                    tg = c // 4
                    dma(f"yst{ys}", mixT_d[8:16, :, tg * 512:(tg + 1) * 512].rearrange("h p t -> p h t"), ystage[ys], reads=[Byst[ys]])

            p4_load(0)
            if NCH > 1:
                p4_load(1)
            for t in range(NCH + 2):
                if t < NCH:
                    stA(t)
                if 1 <= t <= NCH:
                    stB1(t - 1)
                if t + 2 < NCH:
                    p4_load(t + 2)
                if t >= 2:
                    stB2(t - 2)
            S_.barrier()
            if stop_after == "P4":
                break

            AR.release(p5_mark)
            mixs = [AR.alloc((16, 512), BF16) for _ in range(2)]
            Bmix = [Buf("mix0"), Buf("mix1")]
            xr = [AR.alloc((D,), F32) for _ in range(2)]
            Bxr = [Buf("xr0"), Buf("xr1")]
            ot = [AR.alloc((D,), F32) for _ in range(2)]
            Bot = [Buf("ot0"), Buf("ot1")]

            def p5_loadmix(tg):
                s = tg % 2
                dma(f"mix{s}", mixs[s], mixT_d[:, :, tg * 512:(tg + 1) * 512].rearrange("c p t -> p c t"), writes=[Bmix[s]])

            def p5_loadx(i):
                s = i % 2
                dma(f"xr{s}", xr[s], xsrc[i * 128:(i + 1) * 128, :], writes=[Bxr[s]])

            def p5_tile(i):
                s = i % 2
                tg = i // 4
                j = i % 4
                ms = tg % 2
                for half in range(2):
                    bk = (2 * i + half) % 4
                    for cch in range(16):
                        pe("matmul", [Bmix[ms], Bwo], [Bpb[bk]], out=pb[bk], lhsT=mixs[ms][:, cch, j * 128:(j + 1) * 128], rhs=wo_bf[:, cch, half * 512:(half + 1) * 512],
                           start=(cch == 0), stop=(cch == 15))
                    dve("tensor_tensor", [Bxr[s]], [Bpb[bk], Bot[s]], out=ot[s][:, half * 512:(half + 1) * 512], in0=pb[bk], in1=xr[s][:, half * 512:(half + 1) * 512], op=ALU.add)
                dma(f"ot{s}", xdst[i * 128:(i + 1) * 128, :], ot[s], reads=[Bot[s]])

            p5_loadmix(0)
            p5_loadx(0)
            for i in range(NT):
                if i % 4 == 0 and i // 4 + 1 < TG:
                    p5_loadmix(i // 4 + 1)
                if i + 1 < NT:
                    p5_loadx(i + 1)
                p5_tile(i)
            S_.barrier()
            if stop_after == "L0":
                break

        S_.barrier()
        S_.finalize()
        with nc.Block() as block:
            @block.sync
            def _(e):
                S_.replay("sp")

            @block.tensor
            def _(e):
                S_.replay("pe")

            @block.vector
            def _(e):
                S_.replay("dve")

            @block.scalar
            def _(e):
                S_.replay("act")

            @block.gpsimd
            def _(e):
                S_.replay("pool")
    return nc


def host_inputs(inputs, b, S, depth=2):
    f = np.float32
    d = {}
    d["x"] = np.ascontiguousarray(inputs["x"][b, :S]).astype(f)
    d["w_in"] = np.ascontiguousarray(inputs["w_in"]).astype(f)
    d["w_out"] = np.ascontiguousarray(inputs["w_out"]).astype(f)
    d["lnwT"] = np.ascontiguousarray(inputs["ln_w"].reshape(depth, 8, 128).transpose(0, 2, 1)).astype(f)
    cw = inputs["conv_w"].reshape(depth, 4, 12, 128).transpose(0, 3, 2, 1)
    d["cwT"] = np.ascontiguousarray(cw.reshape(depth, 128, 48)).astype(f)
    d["cbT"] = np.ascontiguousarray(inputs["conv_b"].reshape(depth, 12, 128).transpose(0, 2, 1)).astype(f)
    d["qkw"] = np.ascontiguousarray(np.stack([inputs["q_norm_w"], inputs["k_norm_w"]], axis=-1)).astype(f)
    d["snwT"] = np.ascontiguousarray(inputs["ssd_norm_w"].reshape(depth, 8, 128).transpose(0, 2, 1)).astype(f)
    d["hp"] = np.ascontiguousarray(np.concatenate([inputs["dt_bias"], inputs["a_log"], inputs["d_skip"]], axis=-1).reshape(depth, 1, 48)).astype(f)
    d["consts"] = make_consts()
    return d


BATCH = 8
SEQ = 4096


def kernel(**inputs):
    inputs = {k: np.asarray(v) for k, v in inputs.items()}
    nc = build(SEQ, depth=2, dbg=False)
    in_maps = [host_inputs(inputs, b, SEQ) for b in range(BATCH)]
    res = run_bass_kernel_spmd(nc, in_maps, core_ids=list(range(BATCH)))
    out = np.stack([np.asarray(r["out"], dtype=np.float32) for r in res.results], axis=0)
    return out
```

```python
import contextlib
import numpy as np
import concourse.bass as bass
import concourse.mybir as mybir
from concourse.bass_utils import run_bass_kernel_spmd

F32 = mybir.dt.float32
BF16 = mybir.dt.bfloat16
AF = mybir.ActivationFunctionType
ALU = mybir.AluOpType
AX = mybir.AxisListType

ENGS = ("pe", "act", "dve", "pool", "sp")
SAME_ENGINE_SYNC = {"pe": False, "act": True, "dve": True, "pool": True, "sp": False}


class Ev:
    __slots__ = ("eng", "idx", "sem", "val", "needed")

    def __init__(self, eng, idx, sem=None, val=None):
        self.eng = eng
        self.idx = idx
        self.sem = sem
        self.val = val
        self.needed = False


def ev_is_dma(ev):
    return ev.sem is not None and not isinstance(ev.sem, tuple)


class Buf:
    __slots__ = ("name", "w", "rs")

    def __init__(self, name=""):
        self.name = name
        self.w = None
        self.rs = []


class Op:
    __slots__ = ("fn", "waits", "ev", "is_dma")

    def __init__(self, fn, waits, ev, is_dma):
        self.fn = fn
        self.waits = waits
        self.ev = ev
        self.is_dma = is_dma


class Sched:
    def __init__(self, nc):
        self.nc = nc
        self.ops = {e: [] for e in ENGS}
        self.handles = {"pe": nc.tensor, "act": nc.scalar, "dve": nc.vector, "pool": nc.gpsimd, "sp": nc.sync}
        self.esem = {}
        self.dma_sems = {}
        self.dma_cnt = {}
        self.all_dma_ev = {}

    def _deps(self, eng, reads, writes, xreads=()):
        out = []

        def add(ev, raw):
            if ev.eng == eng and not ev_is_dma(ev):
                if not (raw and SAME_ENGINE_SYNC[eng]):
                    return
            out.append(ev)

        for b in reads:
            if b.w is not None:
                add(b.w, True)
        for b in xreads:
            if b.w is not None:
                add(b.w, True)
            for r in b.rs:
                add(r, False)
        for b in writes:
            if b.w is not None:
                add(b.w, False)
            for r in b.rs:
                add(r, False)
        return out

    def _upd(self, ev, reads, writes):
        for b in reads:
            b.rs.append(ev)
        for b in writes:
            b.w = ev
            b.rs = []

    def op(self, eng, fn, reads=(), writes=(), xreads=()):
        waits = self._deps(eng, reads, writes, xreads)
        ev = Ev(eng, len(self.ops[eng]))
        self.ops[eng].append(Op(fn, waits, ev, False))
        self._upd(ev, list(reads) + list(xreads), writes)
        return ev

    def dma(self, eng, semname, out, in_, reads=(), writes=(), **kw):
        waits = self._deps(eng, reads, writes)
        self.dma_cnt[semname] += 16
        ev = Ev(eng, len(self.ops[eng]), sem=semname, val=self.dma_cnt[semname])
        h = self.handles[eng]
        fn = (lambda h=h, out=out, in_=in_, kw=kw: h.dma_start(out=out, in_=in_, **kw))
        self.ops[eng].append(Op(fn, waits, ev, True))
        self.all_dma_ev[semname] = ev
        self._upd(ev, reads, writes)
        return ev

    def barrier(self):
        lasts = []
        for e in ENGS:
            for o in reversed(self.ops[e]):
                if not o.is_dma and o.fn is not None:
                    lasts.append(o.ev)
                    break
        lasts.extend(self.all_dma_ev.values())
        for e in ENGS:
            waits = [ev for ev in lasts if not (ev.eng == e and not ev_is_dma(ev))]
            ev = Ev(e, len(self.ops[e]))
            self.ops[e].append(Op(None, waits, ev, False))

    def finalize(self):
        for e in ENGS:
            for o in self.ops[e]:
                for ev in o.waits:
                    ev.needed = True
        for e in ENGS:
            cnt = 0
            for o in self.ops[e]:
                if o.is_dma:
                    continue
                if o.ev.needed:
                    assert o.fn is not None
                    cnt += 1
                    o.ev.sem = ("E", e)
                    o.ev.val = cnt

    def _sem(self, key):
        if isinstance(key, tuple):
            return self.esem[key[1]]
        return self.dma_sems[key]

    def replay(self, eng):
        h = self.handles[eng]
        seen = {}
        for o in self.ops[eng]:
            need = {}
            for ev in o.waits:
                if need.get(ev.sem, 0) < ev.val:
                    need[ev.sem] = ev.val
            for k, v in need.items():
                if seen.get(k, 0) < v:
                    h.wait_ge(self._sem(k), v)
                    seen[k] = v
            if o.fn is None:
                continue
            ins = o.fn()
            if o.is_dma:
                ins.then_inc(self._sem(o.ev.sem), 16)
            elif o.ev.needed:
                ins.then_inc(self._sem(o.ev.sem), 1)


class Arena:
    def __init__(self, base, nwords):
        self.base = base
        self.cap = nwords * 4
        self.off = 0

    def mark(self):
        return self.off

    def release(self, m):
        self.off = m

    def alloc(self, shape, dt):
        n = int(np.prod(shape))
        esz = 4 if dt == F32 else 2
        nbytes = (n * esz + 63) // 64 * 64
        st = self.off
        self.off += nbytes
        assert self.off <= self.cap, f"SBUF arena overflow {self.off} > {self.cap}"
        w0 = st // 4
        if dt == F32:
            v = self.base[:, w0:w0 + n]
        else:
            v = self.base[:, w0:w0 + nbytes // 4].bitcast(BF16)[:, 0:n]
        if len(shape) == 2:
            v = v.rearrange("p (a b) -> p a b", a=shape[0], b=shape[1])
        elif len(shape) == 3:
            v = v.rearrange("p (a b c) -> p a b c", a=shape[0], b=shape[1], c=shape[2])
        return v


D = 1024
KC = 8
NPROJ = 6672
EPS = 1e-6
NEGBIG = -30000.0

C_IDENT = 0
C_U = 128
C_NEGU = 256
C_NEG = 384
C_ONES = 512
C_BH = 640
C_BIAS = C_BH + 2048
C_G = C_BIAS + 240
C_PAST = C_G + 16
C_E = C_PAST + 256
NCONST = C_E + 2048


def make_consts():
    c = np.zeros((128, NCONST), np.float32)
    p = np.arange(128)
    c[:, C_IDENT:C_IDENT + 128] = np.eye(128)
    U = (p[:, None] <= p[None, :]).astype(np.float32)
    c[:, C_U:C_U + 128] = U
    c[:, C_NEGU:C_NEGU + 128] = -U
    c[:, C_NEG:C_NEG + 128] = np.where(p[None, :] < p[:, None], NEGBIG, 0.0)
    c[:, C_ONES:C_ONES + 128] = 1.0
    slopes = 2.0 ** (-8.0 * np.arange(1, 9) / 8.0)
    qrel = np.arange(256)
    for h in range(8):
        d = qrel[None, :] - p[:, None]
        c[:, C_BH + h * 256:C_BH + (h + 1) * 256] = np.where(d >= 0, -slopes[h] * d, NEGBIG)
        for dd in range(1, 16):
            for kt in range(2):
                c[:, C_BIAS + h * 30 + (dd - 1) * 2 + kt] = -slopes[h] * (dd * 256 - kt * 128 - p)
        for hf in range(2):
            c[:, C_G + h * 2 + hf] = np.exp(-slopes[h] * (hf * 128 + p))
    for qb in range(16):
        for n in range(16):
            c[:, C_PAST + qb * 16 + n] = 0.0 if n < qb else -1e30
    for n in range(16):
        c[n, C_E + n * 128:C_E + (n + 1) * 128] = 1.0
    return c


def pipeline(n, stages):
    maxlag = max(l for l, _ in stages)
    for t in range(n + maxlag):
        for lag, fn in stages:
            i = t - lag
            if 0 <= i < n:
                fn(i)


def build(S, depth=2, dbg=False, stop_after=None):
    NT = S // 128
    NB = S // 256
    TG = S // 512
    assert S % 512 == 0 and NB <= 16
    nc = bass.Bass("TRN2", target_bir_lowering=False)
    V, A, P, T = nc.vector, nc.scalar, nc.gpsimd, nc.tensor

    def din(name, shape, dt=F32):
        return nc.dram_tensor(name, list(shape), dt, kind="ExternalInput").ap()

    def dscr(name, shape, dt):
        kind = "ExternalOutput" if dbg else "Internal"
        return nc.dram_tensor(name, list(shape), dt, kind=kind).ap()

    x_in = din("x", [S, D])
    w_in = din("w_in", [depth, D, NPROJ])
    w_out = din("w_out", [depth, 2 * D, D])
    lnwT_d = din("lnwT", [depth, 128, 8])
    cwT_d = din("cwT", [depth, 128, 12 * 4])
    cbT_d = din("cbT", [depth, 128, 12])
    qkw_d = din("qkw", [depth, 128, 2])
    snwT_d = din("snwT", [depth, 128, 8])
    hp_d = din("hp", [depth, 1, 48])
    consts_d = din("consts", [128, NCONST])
    out_d = nc.dram_tensor("out", [S, D], F32, kind="ExternalOutput").ap()

    qT_d = dscr("qT_s", [8, 128, S], BF16)
    kT_d = dscr("kT_s", [8, 128, S], BF16)
    V_d = dscr("V_s", [S, D], BF16)
    sg_d = dscr("sg_s", [S, D], F32)
    sz_d = dscr("sz_s", [S, D], F32)
    xsT_d = dscr("xsT_s", [D, S], F32)
    bcT_d = dscr("bcT_s", [512, S], BF16)
    mixT_d = dscr("mixT_s", [16, 128, S], BF16)
    xres_d = dscr("xres_s", [S, D], F32)
    dtraw_d = dscr("dtraw_s", [128, NT * 16], F32) if dbg else None
    hT_d = dscr("hT_s", [128, KC * S], BF16) if dbg else None

    ARENA_WORDS = 52736
    with contextlib.ExitStack() as st:
        arena_t = st.enter_context(nc.sbuf_tensor("arena", [128, ARENA_WORDS], F32))
        AR = Arena(arena_t[:, :], ARENA_WORDS)
        pbanks = [st.enter_context(nc.psum_tensor(f"pb{i}", [128, 512], F32)) for i in range(8)]
        pb = [t[:, :] for t in pbanks]
        Bpb = [Buf(f"pb{i}") for i in range(8)]
        S_ = Sched(nc)
        S_.esem = {e: st.enter_context(nc.semaphore("sem_" + e)) for e in ENGS}
        sem_pool = {}

        def dsem(name):
            if name not in sem_pool:
                sem_pool[name] = st.enter_context(nc.semaphore("d_" + name))
                S_.dma_sems[name] = sem_pool[name]
                S_.dma_cnt[name] = 0
            return name

        def eop(eng, h, fname, reads, writes, **kw):
            f = getattr(h, fname)
            xr = ()
            if eng != "pe":
                xr = [b for b in writes if b in Bpb]
                writes = [b for b in writes if b not in Bpb]
                reads = list(reads) + [b for b in writes if b not in reads]
            return S_.op(eng, (lambda f=f, kw=kw: f(**kw)), reads, writes, xr)

        def dve(fname, reads, writes, **kw):
            return eop("dve", V, fname, reads, writes, **kw)

        def act(fname, reads, writes, **kw):
            return eop("act", A, fname, reads, writes, **kw)

        def pool(fname, reads, writes, **kw):
            return eop("pool", P, fname, reads, writes, **kw)

        def pe(fname, reads, writes, **kw):
            return eop("pe", T, fname, reads, writes, **kw)

        def dma(semname, out, in_, reads=(), writes=(), eng="sp"):
            return S_.dma(eng, dsem(semname), out, in_, reads, writes)

        cst = AR.alloc((NCONST,), F32)
        Bcst = Buf("cst")
        ident_bf = AR.alloc((128,), BF16)
        bh_bf = AR.alloc((8, 256), BF16)
        e_bf = AR.alloc((16, 128), BF16)
        neg_bf = AR.alloc((128,), BF16)
        mhalf = AR.alloc((8,), F32)
        Bk = Buf("constbf")
        par = {}
        Bpar = Buf("par")
        for l in range(depth):
            par[l] = dict(
                lnwT=AR.alloc((8,), F32), cwT=AR.alloc((12, 4), F32), cbT=AR.alloc((12,), F32),
                qkw=AR.alloc((2,), F32), snwT=AR.alloc((8,), F32), hp=AR.alloc((48,), F32),
                qws=AR.alloc((1,), F32), aneg=AR.alloc((16,), F32))
        dtraw = AR.alloc((NT, 16), F32)
        Bdtraw = Buf("dtraw")

        identf = cst[:, C_IDENT:C_IDENT + 128]

        dma("cst", cst, consts_d[:, :], writes=[Bcst])
        for l in range(depth):
            pl = par[l]
            dma("par", pl["lnwT"], lnwT_d[l], writes=[Bpar])
            dma("par", pl["cwT"], cwT_d[l].rearrange("p (c k) -> p c k", k=4), writes=[Bpar])
            dma("par", pl["cbT"], cbT_d[l], writes=[Bpar])
            dma("par", pl["qkw"], qkw_d[l], writes=[Bpar])
            dma("par", pl["snwT"], snwT_d[l], writes=[Bpar])
            dma("par", pl["hp"], hp_d[l].partition_broadcast(128), writes=[Bpar])
        pool("tensor_copy", [Bcst], [Bk], out=ident_bf, in_=identf)
        pool("tensor_copy", [Bcst], [Bk], out=bh_bf, in_=cst[:, C_BH:C_BH + 2048].rearrange("p (h q) -> p h q", q=256))
        pool("tensor_copy", [Bcst], [Bk], out=e_bf, in_=cst[:, C_E:C_E + 2048].rearrange("p (n k) -> p n k", k=128))
        pool("tensor_copy", [Bcst], [Bk], out=neg_bf, in_=cst[:, C_NEG:C_NEG + 128])
        pool("memset", [], [Bk], ap=mhalf, constant=-0.5)
        for l in range(depth):
            pl = par[l]
            pool("tensor_scalar", [Bpar], [Bpar], out=pl["qws"], in0=pl["qkw"][:, 0:1], scalar1=float(128 ** -0.5), scalar2=None, op0=ALU.mult)
            act("activation", [Bpar], [Bpar], out=pl["aneg"], in_=pl["hp"][:, 16:32], func=AF.Exp)
            pool("tensor_scalar", [Bpar], [Bpar], out=pl["aneg"], in0=pl["aneg"], scalar1=-1.0, scalar2=None, op0=ALU.mult)
        S_.barrier()

        base_mark = AR.mark()
        p3_mark = base_mark

        for l in range(depth):
            pl = par[l]
            xsrc = x_in if l == 0 else xres_d
            xdst = xres_d if l < depth - 1 else out_d
            AR.release(base_mark)
            hT = AR.alloc((KC, S), BF16)
            BhT = Buf("hT")
            m1 = AR.mark()
            xt = [AR.alloc((D,), F32) for _ in range(2)]
            Bxt = [Buf("xt0"), Buf("xt1")]
            xn = [AR.alloc((D,), BF16) for _ in range(2)]
            Bxn = [Buf("xn0"), Buf("xn1")]
            junk = AR.alloc((D,), BF16)
            Bjunk = Buf("junk")
            ss1 = [AR.alloc((1,), F32) for _ in range(2)]
            Bss1 = [Buf("ss0"), Buf("ss1")]

            def p1_load(i):
                s = i % 2
                dma(f"xt{s}", xt[s], xsrc[i * 128:(i + 1) * 128, :], writes=[Bxt[s]])

            def p1_norm(i):
                s = i % 2
                act("activation", [Bxt[s]], [Bjunk, Bss1[s]], out=junk, in_=xt[s], func=AF.Square, accum_out=ss1[s])
                pool("tensor_scalar", [Bss1[s]], [Bss1[s]], out=ss1[s], in0=ss1[s], scalar1=1.0 / D, scalar2=EPS, op0=ALU.mult, op1=ALU.add)
                pool("tensor_tensor", [Bss1[s], Bk], [Bss1[s]], out=ss1[s], in0=ss1[s], in1=mhalf[:, 0:1], op=ALU.pow)
                dve("tensor_scalar", [Bxt[s], Bss1[s]], [Bxn[s]], out=xn[s], in0=xt[s], scalar1=ss1[s][:, 0:1], scalar2=None, op0=ALU.mult)

            def p1_tr(i):
                s = i % 2
                bk = 6 + (i % 2)
                pbt = pb[bk].bitcast(BF16)
                for c in range(KC):
                    pe("transpose", [Bxn[s], Bk], [Bpb[bk]], out=pbt[:, c * 128:(c + 1) * 128], in_=xn[s][:, c * 128:(c + 1) * 128], identity=ident_bf)
                dve("tensor_tensor", [Bpar], [Bpb[bk], BhT], out=hT[:, :, i * 128:(i + 1) * 128],
                    in0=pbt.rearrange("p (c t) -> p c t", t=128),
                    in1=pl["lnwT"].unsqueeze(2).to_broadcast([128, KC, 128]), op=ALU.mult)

            p1_load(0)
            for t in range(NT + 1):
                if t + 1 < NT:
                    p1_load(t + 1)
                if t < NT:
                    p1_norm(t)
                if t >= 1:
                    p1_tr(t - 1)
            if dbg and l == 0:
                dma("dbg", hT_d[:, :], hT.rearrange("p a b -> p (a b)"), reads=[BhT])
            if stop_after == "P1":
                break

            wst = [AR.alloc((KC, 512), F32) for _ in range(2)]
            Bwst = [Buf("wst0"), Buf("wst1")]
            wbf = [AR.alloc((KC, 512), BF16) for _ in range(2)]
            Bwbf = [Buf("wbf0"), Buf("wbf1")]
            NG = 14
            w_l = w_in[l]

            def gcols(g):
                c0 = g * 512
                return c0, min(512, NPROJ - c0)

            def wload(g):
                s = g % 2
                c0, ncl = gcols(g)
                dma(f"wst{s}", wst[s][:, :, 0:ncl], w_l[:, c0:c0 + ncl].rearrange("(k p) n -> p k n", p=128), writes=[Bwst[s]])

            def wconv(g):
                s = g % 2
                c0, ncl = gcols(g)
                pool("tensor_copy", [Bwst[s]], [Bwbf[s]], out=wbf[s][:, :, 0:ncl], in_=wst[s][:, :, 0:ncl])

            ss4 = [AR.alloc((4,), F32) for _ in range(2)]
            Bss4 = [Buf("ss4a"), Buf("ss4b")]
            sq = [AR.alloc((512,), F32) for _ in range(2)]
            Bsq = [Buf("sq0"), Buf("sq1")]
            qn = [AR.alloc((4, 128), BF16) for _ in range(2)]
            Bqn = [Buf("qn0"), Buf("qn1")]
            qst = [AR.alloc((4, 512), BF16) for _ in range(2)]
            Bqst = [Buf("qst0"), Buf("qst1")]
            vst = [AR.alloc((512,), BF16) for _ in range(2)]
            Bvst = [Buf("vst0"), Buf("vst1")]
            fst = [AR.alloc((512,), F32) for _ in range(2)]
            Bfst = [Buf("fst0"), Buf("fst1")]
            ubuf = [AR.alloc((515,), F32) for _ in range(2)]
            Bu = [Buf("u0"), Buf("u1")]
            cacc = [AR.alloc((512,), F32) for _ in range(2)]
            Bcacc = [Buf("cacc0"), Buf("cacc1")]
            xo_f = [AR.alloc((512,), F32) for _ in range(2)]
            xo_b = [AR.alloc((512,), BF16) for _ in range(2)]
            Bxo = [Buf("xo0"), Buf("xo1")]
            accn = [0]

            def next_acc():
                b = accn[0] % 4
                accn[0] += 1
                return b

            def mm_tok(g, i, ncl, bk):
                s = g % 2
                for k in range(KC):
                    pe("matmul", [BhT, Bwbf[s]], [Bpb[bk]], out=pb[bk][:, 0:ncl], lhsT=hT[:, k, i * 128:(i + 1) * 128],
                       rhs=wbf[s][:, k, 0:ncl], start=(k == 0), stop=(k == KC - 1))

            def do_qk(g):
                which = 0 if g < 2 else 1
                h0 = (g % 2) * 4
                dst = qT_d if which == 0 else kT_d
                wsc = pl["qws"] if which == 0 else pl["qkw"][:, 1:2]
                banks = {}

                def s0(i):
                    bk = next_acc()
                    banks[i] = bk
                    mm_tok(g, i, 512, bk)
                    s = i % 2
                    act("activation", [], [Bpb[bk], Bsq[s]], out=sq[s], in_=pb[bk], func=AF.Square)
                    dve("tensor_reduce", [Bsq[s]], [Bss4[s]], out=ss4[s], in_=sq[s].rearrange("p (h d) -> p h d", d=128), axis=AX.X, op=ALU.add)
                    pool("tensor_scalar", [], [Bss4[s]], out=ss4[s], in0=ss4[s], scalar1=1.0 / 128, scalar2=EPS, op0=ALU.mult, op1=ALU.add)
                    pool("tensor_tensor", [Bk], [Bss4[s]], out=ss4[s], in0=ss4[s], in1=mhalf[:, 0:4], op=ALU.pow)
                    dve("tensor_tensor", [Bss4[s]], [Bpb[bk], Bqn[s]], out=qn[s], in0=pb[bk].rearrange("p (h d) -> p h d", d=128),
                        in1=ss4[s].unsqueeze(2).to_broadcast([128, 4, 128]), op=ALU.mult)

                def s2(i):
                    s = i % 2
                    bk = 4 + (i % 2)
                    pbt = pb[bk].bitcast(BF16)
                    for hh in range(4):
                        pe("transpose", [Bqn[s], Bk], [Bpb[bk]], out=pbt[:, hh * 128:(hh + 1) * 128], in_=qn[s][:, hh, :], identity=ident_bf)
                    ss_ = (i // 4) % 2
                    j = i % 4
                    act("activation", [Bpar], [Bpb[bk], Bqst[ss_]], out=qst[ss_][:, :, j * 128:(j + 1) * 128],
                        in_=pbt[:, 0:512].rearrange("p (h t) -> p h t", t=128), func=AF.Copy, scale=wsc)
                    if j == 3:
                        tg = i // 4
                        dma(f"qst{ss_}", dst[h0:h0 + 4, :, tg * 512:(tg + 1) * 512].rearrange("h p t -> p h t"), qst[ss_], reads=[Bqst[ss_]])

                pipeline(NT, [(2, s2), (0, s0)])

            def do_tok(g, kind):
                c0 = (g % 2) * 512

                def s0(i):
                    bk = next_acc()
                    mm_tok(g, i, 512, bk)
                    s = i % 2
                    if kind == "v":
                        act("activation", [], [Bpb[bk], Bvst[s]], out=vst[s], in_=pb[bk], func=AF.Copy)
                        dma(f"vst{s}", V_d[i * 128:(i + 1) * 128, c0:c0 + 512], vst[s], reads=[Bvst[s]])
                    else:
                        act("activation", [], [Bpb[bk], Bfst[s]], out=fst[s], in_=pb[bk], func=AF.Silu)
                        dd = sg_d if kind == "g" else sz_d
                        dma(f"fst{s}", dd[i * 128:(i + 1) * 128, c0:c0 + 512], fst[s], reads=[Bfst[s]])

                pipeline(NT, [(0, s0)])

            def do_feat(g):
                s_w = g % 2
                nloc = 4
                items = [(cl, tg) for cl in range(nloc) for tg in range(TG)]

                def s0(ix):
                    cl, tg = items[ix]
                    cc = (g - 10) * 4 + cl
                    bk = next_acc()
                    for k in range(KC):
                        pe("matmul", [BhT, Bwbf[s_w]], [Bpb[bk]], out=pb[bk], lhsT=wbf[s_w][:, k, cl * 128:(cl + 1) * 128],
                           rhs=hT[:, k, tg * 512:(tg + 1) * 512], start=(k == 0), stop=(k == KC - 1))
                    s = ix % 2
                    if tg == 0:
                        pool("memset", [], [Bu[s]], ap=ubuf[s][:, 0:3], constant=0.0)
                    else:
                        pool("tensor_copy", [Bu[1 - s]], [Bu[s]], out=ubuf[s][:, 0:3], in_=ubuf[1 - s][:, 512:515])
                    act("activation", [], [Bpb[bk], Bu[s]], out=ubuf[s][:, 3:515], in_=pb[bk], func=AF.Copy)
                    cw = pl["cwT"]
                    act("activation", [Bu[s]], [Bcacc[s]], out=cacc[s], in_=ubuf[s][:, 3:515], func=AF.Copy, scale=cw[:, cc, 3:4])
                    for kk in (2, 1, 0):
                        dve("scalar_tensor_tensor", [Bu[s], Bpar], [Bcacc[s]], out=cacc[s], in0=ubuf[s][:, kk:kk + 512], scalar=cw[:, cc, kk:kk + 1],
                            in1=cacc[s], op0=ALU.mult, op1=ALU.add)
                    if cc < 8:
                        act("activation", [Bcacc[s], Bpar], [Bxo[s]], out=xo_f[s], in_=cacc[s], func=AF.Silu, bias=pl["cbT"][:, cc:cc + 1])
                        dma(f"xo{s}", xsT_d[cc * 128:(cc + 1) * 128, tg * 512:(tg + 1) * 512], xo_f[s], reads=[Bxo[s]])
                    else:
                        act("activation", [Bcacc[s], Bpar], [Bxo[s]], out=xo_b[s], in_=cacc[s], func=AF.Silu, bias=pl["cbT"][:, cc:cc + 1])
                        dma(f"xo{s}", bcT_d[(cc - 8) * 128:(cc - 7) * 128, tg * 512:(tg + 1) * 512], xo_b[s], reads=[Bxo[s]])

                pipeline(len(items), [(0, s0)])

            def do_dt(g):
                def s0(i):
                    bk = next_acc()
                    mm_tok(g, i, 16, bk)
                    dve("tensor_copy", [], [Bpb[bk], Bdtraw], out=dtraw[:, i, :], in_=pb[bk][:, 0:16])
                pipeline(NT, [(0, s0)])

            wload(0)
            wconv(0)
            wload(1)
            for g in range(NG):
                if g + 1 < NG:
                    wconv(g + 1)
                if g + 2 < NG:
                    wload(g + 2)
                if g < 4:
                    do_qk(g)
                elif g < 6:
                    do_tok(g, "v")
                elif g < 8:
                    do_tok(g, "g")
                elif g < 10:
                    do_tok(g, "z")
                elif g < 13:
                    do_feat(g)
                else:
                    do_dt(g)
            if dbg:
                dma("dbg", dtraw_d[:, :], dtraw.rearrange("p a b -> p (a b)"), reads=[Bdtraw])
            S_.barrier()
            if stop_after == "P2":
                break
            AR.release(p3_mark)
            qT_h = [AR.alloc((S,), BF16) for _ in range(2)]
            kT_h = [AR.alloc((S,), BF16) for _ in range(2)]
            V_h = [AR.alloc((NT, 129), BF16) for _ in range(2)]
            sg_h = [AR.alloc((NT, 128), F32) for _ in range(2)]
            Bq = [Buf("q0"), Buf("q1")]
            Bkk = [Buf("k0"), Buf("k1")]
            Bv = [Buf("v0"), Buf("v1")]
            Bsg = [Buf("sg0"), Buf("sg1")]
            attnT = [AR.alloc((S,), BF16) for _ in range(2)]
            Battn = [Buf("at0"), Buf("at1")]
            maskT = [AR.alloc((S,), BF16) for _ in range(2)]
            Bmask = [Buf("mk0"), Buf("mk1")]
            km = AR.alloc((16,), F32)
            kms = AR.alloc((16,), F32)
            kmh = [AR.alloc((16,), BF16) for _ in range(2)]
            kml = [AR.alloc((16,), BF16) for _ in range(2)]
            Bkm = Buf("km")
            Bkmhl = [Buf("kmhl0"), Buf("kmhl1")]
            gm = [AR.alloc((16,), F32) for _ in range(2)]
            top8 = [AR.alloc((8,), F32) for _ in range(2)]
            thr = [AR.alloc((1,), F32) for _ in range(2)]
            mk = [AR.alloc((16,), BF16) for _ in range(2)]
            Bgm = [Buf("gm0"), Buf("gm1")]
            Bmkk = [Buf("mkk0"), Buf("mkk1")]
            NPT = 6
            PT = [AR.alloc((512,), BF16) for _ in range(NPT)]
            BPT = [Buf(f"pt{i}") for i in range(NPT)]
            tsc = [AR.alloc((129,), F32) for _ in range(2)]
            num = [AR.alloc((129,), F32) for _ in range(2)]
            rden = [AR.alloc((1,), F32) for _ in range(2)]
            o_bf = [AR.alloc((128,), BF16) for _ in range(2)]
            Bcmb = [Buf("cmb0"), Buf("cmb1")]
            Bobf = [Buf("obf0"), Buf("obf1")]

            pool("memset", [], [Bkm], ap=km, constant=0.0)
            for s in range(2):
                pool("memset", [], [Bv[s]], ap=V_h[s][:, :, 128:129], constant=1.0)
                pool("memset", [], [Bmask[s]], ap=maskT[s], constant=0.0)

            def head_load(h):
                s = h % 2
                dma(f"hq{s}", qT_h[s], qT_d[h], writes=[Bq[s]])
                dma(f"hk{s}", kT_h[s], kT_d[h], writes=[Bkk[s]])
                dma(f"hv{s}", V_h[s][:, :, 0:128], V_d[:, h * 128:(h + 1) * 128].rearrange("(t p) d -> p t d", p=128), writes=[Bv[s]])
                dma(f"hg{s}", sg_h[s], sg_d[:, h * 128:(h + 1) * 128].rearrange("(t p) d -> p t d", p=128), writes=[Bsg[s]])

            def head_kmean(h):
                s = h % 2
                dve("tensor_reduce", [Bkk[s]], [Bkm], out=km[:, 0:NB], in_=kT_h[s].rearrange("p (n k) -> p n k", k=256), axis=AX.X, op=ALU.add)
                dve("tensor_scalar", [], [Bkm], out=kms, in0=km, scalar1=1.0 / 256, scalar2=None, op0=ALU.mult)
                dve("tensor_copy", [Bkm], [Bkmhl[s]], out=kmh[s], in_=kms)
                dve("tensor_tensor", [Bkm], [Bkmhl[s]], out=kml[s], in0=kms, in1=kmh[s], op=ALU.subtract)

            def gate_front(h, qb):
                s = h % 2
                for j in range(2):
                    i = 2 * qb + j
                    pe("matmul", [Bq[s], Bkmhl[s]], [Bpb[6]], out=pb[6][:, j * 16:(j + 1) * 16], lhsT=qT_h[s][:, i * 128:(i + 1) * 128], rhs=kmh[s], start=True, stop=False)
                    pe("matmul", [Bq[s], Bkmhl[s]], [Bpb[6]], out=pb[6][:, j * 16:(j + 1) * 16], lhsT=qT_h[s][:, i * 128:(i + 1) * 128], rhs=kml[s], start=False, stop=True)
                for j in range(2):
                    dve("tensor_tensor", [], [Bpb[6], Bgm[j]], out=gm[j], in0=pb[6][:, j * 16:(j + 1) * 16], in1=cst[:, C_PAST + qb * 16:C_PAST + (qb + 1) * 16], op=ALU.add)
                    dve("max", [], [Bgm[j]], out=top8[j], in_=gm[j])
                    dve("tensor_scalar", [], [Bgm[j]], out=thr[j], in0=top8[j][:, 2:3], scalar1=-1e29, scalar2=None, op0=ALU.max)
                    dve("tensor_scalar", [Bgm[j]], [Bmkk[j]], out=mk[j], in0=gm[j], scalar1=thr[j][:, 0:1], scalar2=NEGBIG, op0=ALU.is_lt, op1=ALU.mult)

            def gate_back(h, qb):
                s = h % 2
                pbt = pb[7].bitcast(BF16)
                for j in range(2):
                    pe("transpose", [Bmkk[j]], [Bpb[7]], out=pbt[0:16, j * 128:(j + 1) * 128], in_=mk[j], identity=ident_bf)
                dve("tensor_copy", [], [Bpb[7], Bmask[s]], out=maskT[s][0:16, qb * 256:(qb + 1) * 256], in_=pbt[0:16, 0:256])

            deferred = []

            def defer(n, fn):
                deferred.append([n, fn])

            def tick():
                for d_ in list(deferred):
                    d_[0] -= 1
                    if d_[0] <= 0:
                        deferred.remove(d_)
                        d_[1]()

            def flush():
                while deferred:
                    d_ = deferred.pop(0)
                    d_[1]()

            def attn_head(h, first, last_head):
                s = h % 2
                NP = S // 512
                items = []
                for p in range(NP):
                    for kt in range(4 * p):
                        items.append(("pc", p, kt))
                    items.append(("own0", 2 * p, 4 * p))
                    items.append(("own1", 2 * p, 4 * p + 1))
                    items.append(("po", p, 4 * p))
                    items.append(("po", p, 4 * p + 1))
                    items.append(("own0", 2 * p + 1, 4 * p + 2))
                    items.append(("own1", 2 * p + 1, 4 * p + 3))
                slots = {}
                done_b = set()
                seen_pair = set()
                seen_po = set()

                def exp_past(sb_, ps_, c0, qb, n, kt):
                    bcol = C_BIAS + h * 30 + (qb - n - 1) * 2 + (kt % 2)
                    act("activation", [], [Bpb[sb_], BPT[ps_]], out=PT[ps_][:, c0:c0 + 256], in_=pb[sb_][:, c0:c0 + 256], func=AF.Exp, bias=cst[:, bcol:bcol + 1])

                def stA(ix):
                    kind, a0, kt = items[ix]
                    sb_ = ix % 3
                    ps_ = ix % NPT
                    slots[ix] = ps_
                    if not last_head:
                        if kind in ("pc", "own0") and (a0 if kind == "pc" else a0 // 2) not in seen_pair and (kind == "pc" or a0 % 2 == 0):
                            p_ = a0 if kind == "pc" else a0 // 2
                            seen_pair.add(p_)
                            gate_front(h + 1, 2 * p_)
                            defer(4, (lambda qb=2 * p_: gate_back(h + 1, qb)))
                        if kind == "po" and a0 not in seen_po:
                            seen_po.add(a0)
                            gate_front(h + 1, 2 * a0 + 1)
                            defer(4, (lambda qb=2 * a0 + 1: gate_back(h + 1, qb)))
                    n = kt // 2
                    if kind == "pc":
                        p = a0
                        qpair = qT_h[s][:, p * 512:(p + 1) * 512]
                        pe("matmul", [Bq[s], Bkk[s]], [Bpb[sb_]], out=pb[sb_], lhsT=kT_h[s][:, kt * 128:(kt + 1) * 128], rhs=qpair, start=True, stop=False)
                        if ix - 2 >= 0:
                            stB(ix - 2)
                            done_b.add(ix - 2)
                        pe("matmul", [Bmask[s]], [Bpb[sb_]], out=pb[sb_], lhsT=e_bf[0:16, n, :], rhs=maskT[s][0:16, p * 512:(p + 1) * 512], start=False, stop=True)
                        exp_past(sb_, ps_, 0, 2 * p, n, kt)
                        exp_past(sb_, ps_, 256, 2 * p + 1, n, kt)
                    elif kind == "po":
                        qb = 2 * a0 + 1
                        qblk = qT_h[s][:, qb * 256:(qb + 1) * 256]
                        pe("matmul", [Bq[s], Bkk[s]], [Bpb[sb_]], out=pb[sb_][:, 0:256], lhsT=kT_h[s][:, kt * 128:(kt + 1) * 128], rhs=qblk, start=True, stop=False)
                        pe("matmul", [Bmask[s]], [Bpb[sb_]], out=pb[sb_][:, 0:256], lhsT=e_bf[0:16, n, :], rhs=maskT[s][0:16, qb * 256:(qb + 1) * 256], start=False, stop=True)
                        exp_past(sb_, ps_, 0, qb, n, kt)
                    elif kind == "own0":
                        qb = a0
                        qblk = qT_h[s][:, qb * 256:(qb + 1) * 256]
                        pe("matmul", [Bq[s], Bkk[s]], [Bpb[sb_]], out=pb[sb_][:, 0:256], lhsT=kT_h[s][:, kt * 128:(kt + 1) * 128], rhs=qblk, start=True, stop=False)
                        pe("matmul", [], [Bpb[sb_]], out=pb[sb_][:, 0:256], lhsT=ident_bf, rhs=bh_bf[:, h, :], start=False, stop=True)
                        act("activation", [], [Bpb[sb_], BPT[ps_]], out=PT[ps_][:, 0:256], in_=pb[sb_][:, 0:256], func=AF.Exp)
                    else:
                        qb = a0
                        qblk = qT_h[s][:, qb * 256:(qb + 1) * 256]
                        pe("matmul", [Bq[s], Bkk[s]], [Bpb[sb_]], out=pb[sb_][:, 0:128], lhsT=kT_h[s][:, kt * 128:(kt + 1) * 128], rhs=qblk[:, 128:256], start=True, stop=False)
                        pe("matmul", [], [Bpb[sb_]], out=pb[sb_][:, 0:128], lhsT=ident_bf, rhs=bh_bf[:, h, 0:128], start=False, stop=True)
                        act("activation", [], [Bpb[sb_], BPT[ps_]], out=PT[ps_][:, 0:128], in_=pb[sb_][:, 0:128], func=AF.Exp)
                    tick()

                def stB(ix):
                    kind, a0, kt = items[ix]
                    ps_ = slots[ix]
                    if kind == "pc":
                        p = a0
                        for j in range(4):
                            bk = 3 + j // 2
                            c0 = (j % 2) * 256
                            pe("matmul", [BPT[ps_], Bv[s]], [Bpb[bk]], out=pb[bk][:, c0:c0 + 129], lhsT=PT[ps_][:, j * 128:(j + 1) * 128], rhs=V_h[s][:, kt, :],
                               start=(kt == 0 and j % 2 == 0), stop=(j < 2 and kt == 4 * p - 1), skip_group_check=True)
                    elif kind == "po":
                        p = a0
                        for jj in range(2):
                            c0 = jj * 256
                            pe("matmul", [BPT[ps_], Bv[s]], [Bpb[4]], out=pb[4][:, c0:c0 + 129], lhsT=PT[ps_][:, jj * 128:(jj + 1) * 128], rhs=V_h[s][:, kt, :],
                               start=(kt == 0 and jj == 0), stop=(kt == 4 * p + 1), skip_group_check=True)
                    elif kind == "own0":
                        pe("matmul", [BPT[ps_], Bv[s]], [Bpb[5]], out=pb[5][:, 0:129], lhsT=PT[ps_][:, 0:128], rhs=V_h[s][:, kt, :], start=True, stop=True)
                        pe("matmul", [BPT[ps_], Bv[s]], [Bpb[5]], out=pb[5][:, 256:385], lhsT=PT[ps_][:, 128:256], rhs=V_h[s][:, kt, :], start=True, stop=False)
                    else:
                        qb = a0
                        pe("matmul", [BPT[ps_], Bv[s]], [Bpb[5]], out=pb[5][:, 256:385], lhsT=PT[ps_][:, 0:128], rhs=V_h[s][:, kt, :], start=False, stop=True)
                        obk = 3 + (qb % 2)
                        for j in range(2):
                            i = 2 * qb + j
                            if qb > 0:
                                gcol = C_G + h * 2 + j
                                act("activation", [], [Bpb[obk], Bcmb[j]], out=tsc[j], in_=pb[obk][:, j * 256:j * 256 + 129], func=AF.Copy, scale=cst[:, gcol:gcol + 1])
                                dve("tensor_tensor", [], [Bpb[5], Bcmb[j]], out=num[j], in0=tsc[j], in1=pb[5][:, j * 256:j * 256 + 129], op=ALU.add)
                            else:
                                dve("tensor_copy", [], [Bpb[5], Bcmb[j]], out=num[j], in_=pb[5][:, j * 256:j * 256 + 129])
                            dve("reciprocal", [], [Bcmb[j]], out=rden[j], in_=num[j][:, 128:129])
                            dve("scalar_tensor_tensor", [Bcmb[j], Bsg[s]], [Bobf[j]], out=o_bf[j], in0=num[j][:, 0:128], scalar=rden[j][:, 0:1], in1=sg_h[s][:, i, :],
                                op0=ALU.mult, op1=ALU.mult)

                        def back(qb=qb):
                            pbt = pb[7].bitcast(BF16)
                            for j in range(2):
                                pe("transpose", [Bobf[j]], [Bpb[7]], out=pbt[:, 512 + j * 128:512 + (j + 1) * 128], in_=o_bf[j], identity=ident_bf)
                            dve("tensor_copy", [], [Bpb[7], Battn[s]], out=attnT[s][:, qb * 256:(qb + 1) * 256], in_=pbt[:, 512:768])
                        defer(3, back)

                def stB_guard(ix):
                    if ix + 2 < len(items) and items[ix + 2][0] == "pc":
                        return
                    stB(ix)

                pipeline(len(items), [(2, stB_guard), (0, stA)])
                flush()
                dma(f"at{s}", mixT_d[h], attnT[s], reads=[Battn[s]])

            head_load(0)
            head_kmean(0)
            for qb in range(NB):
                gate_front(0, qb)
                gate_back(0, qb)
            for h in range(8):
                if h + 1 < 8:
                    head_load(h + 1)
                    head_kmean(h + 1)
                attn_head(h, h == 0, h == 7)
            S_.barrier()
            if stop_after == "P3":
                break
            AR.release(p3_mark)
            wo_bf = AR.alloc((16, D), BF16)
            Bwo = Buf("wo")
            wos = [AR.alloc((2, D), F32) for _ in range(2)]
            Bwos = [Buf("wos0"), Buf("wos1")]
            wo_l = w_out[l]
            p5_mark = AR.mark()
            for q8 in range(8):
                s = q8 % 2
                dma(f"wos{s}", wos[s], wo_l[q8 * 256:(q8 + 1) * 256, :].rearrange("(k p) n -> p k n", p=128), writes=[Bwos[s]])
                pool("tensor_copy", [Bwos[s]], [Bwo], out=wo_bf[:, q8 * 2:(q8 + 1) * 2, :], in_=wos[s])
            NCH = NT
            hp = pl["hp"]
            xsT_c = [AR.alloc((8, 128), F32) for _ in range(2)]
            bcT_c = [AR.alloc((4, 128), BF16) for _ in range(3)]
            sz_c = [AR.alloc((D,), F32) for _ in range(3)]
            Bxs = [Buf("xsc0"), Buf("xsc1"), Buf("xsc2")]
            Bbc = [Buf("bcc0"), Buf("bcc1"), Buf("bcc2")]
            Bsz = [Buf("szc0"), Buf("szc1"), Buf("szc2")]
            x1a = AR.alloc((NT, 16), F32)
            axa = AR.alloc((NT, 16), F32)
            lga = AR.alloc((NT, 16), F32)
            dta = AR.alloc((NT, 16), F32)
            dAa = AR.alloc((NT, 16), F32)
            acs_a = AR.alloc((NT, 16), F32)
            aend_a = AR.alloc((NT, 16), F32)
            dse_a = AR.alloc((NT, 16), F32)
            nacs_a = AR.alloc((NT, 16), F32)
            ea_a = AR.alloc((NT, 16), F32)
            cd_a = AR.alloc((NT, 16), F32)
            eds_a = AR.alloc((NT, 16), F32)
            dtw_a = AR.alloc((NT, 16), F32)
            Bdt = Buf("dtc")
            Rm = AR.alloc((16, 128), F32)
            BR = Buf("R")
            negb4 = AR.alloc((4, 128), BF16)
            decay = AR.alloc((16, 128), BF16)
            Bdecay = Buf("decay")
            MT = [AR.alloc((16, 128), BF16) for _ in range(2)]
            BMT = [Buf("MT0"), Buf("MT1")]
            cb_sb = AR.alloc((2, 128), BF16)
            Bcb = Buf("cb")
            Btok = [AR.alloc((2, 128), BF16) for _ in range(2)]
            BBtok = [Buf("btok0"), Buf("btok1")]
            xs_sb = AR.alloc((16, 64), F32)
            Bxssb = Buf("xssb")
            xdt = [AR.alloc((16, 64), BF16) for _ in range(2)]
            xw = [AR.alloc((16, 64), BF16) for _ in range(2)]
            xD = [AR.alloc((16, 64), F32) for _ in range(2)]
            Bxdt = [Buf("xdt0"), Buf("xdt1")]
            Bxw = [Buf("xw0"), Buf("xw1")]
            BxD = [Buf("xD0"), Buf("xD1")]
            prev = AR.alloc((16, 64), F32)
            prevbf = AR.alloc((16, 64), BF16)
            Bprev = Buf("prev")
            Bprevbf = Buf("prevbf")
            t1 = AR.alloc((16, 64), F32)
            Bt1 = Buf("t1")
            yg = t1
            Byg = Bt1
            ss2 = AR.alloc((2,), F32)
            Bss2 = Buf("ss2")
            yn2 = [AR.alloc((D,), BF16) for _ in range(2)]
            Byn2 = [Buf("yn0"), Buf("yn1")]
            ystage = [AR.alloc((8, 512), BF16) for _ in range(2)]
            Byst = [Buf("yst0"), Buf("yst1")]
            junk4 = AR.alloc((512,), BF16)
            Bjunk4 = Buf("junk4")
            ones_f = cst[:, C_ONES:C_ONES + 128]
            U_f = cst[:, C_U:C_U + 128]

            pool("memset", [], [Bprev], ap=prev, constant=0.0)
            pool("memset", [], [Bprevbf], ap=prevbf, constant=0.0)
            pool("tensor_copy", [], [BR], out=negb4, in_=cst[:, C_NEG:C_NEG + 128].unsqueeze(1).to_broadcast([128, 4, 128]))

            NW = NT * 16
            fl = lambda t: t.rearrange("p a b -> p (a b)")
            pool("tensor_tensor", [Bdtraw], [Bdt], out=x1a, in0=dtraw, in1=hp[:, 0:16].unsqueeze(1).to_broadcast([128, NT, 16]), op=ALU.add)
            act("activation", [], [Bdt], out=axa, in_=x1a, func=AF.Abs)
            act("activation", [], [Bdt], out=axa, in_=axa, func=AF.Exp, scale=-1.0)
            act("activation", [], [Bdt], out=lga, in_=axa, func=AF.Ln, bias=1.0)
            dve("tensor_scalar", [], [Bdt], out=x1a, in0=x1a, scalar1=0.0, scalar2=None, op0=ALU.max)
            dve("tensor_tensor", [], [Bdt], out=dta, in0=x1a, in1=lga, op=ALU.add)
            dve("tensor_tensor", [], [Bdt], out=dAa, in0=dta, in1=pl["aneg"].unsqueeze(1).to_broadcast([128, NT, 16]), op=ALU.mult)
            pe("matmul", [Bdt], [Bpb[2]], out=pb[2][:, 0:NW], lhsT=U_f, rhs=fl(dAa), start=True, stop=True)
            pe("matmul", [Bdt], [Bpb[3]], out=pb[3][:, 0:NW], lhsT=ones_f, rhs=fl(dAa), start=True, stop=True)
            act("activation", [], [Bpb[2], Bdt], out=fl(acs_a), in_=pb[2][:, 0:NW], func=AF.Copy)
            act("activation", [], [Bpb[3], Bdt], out=fl(aend_a), in_=pb[3][:, 0:NW], func=AF.Copy)
            dve("tensor_tensor", [], [Bdt], out=dse_a, in0=aend_a, in1=acs_a, op=ALU.subtract)
            dve("tensor_scalar", [], [Bdt], out=nacs_a, in0=acs_a, scalar1=-1.0, scalar2=None, op0=ALU.mult)
            act("activation", [], [Bdt], out=ea_a, in_=acs_a, func=AF.Exp)
            act("activation", [], [Bdt], out=cd_a, in_=aend_a, func=AF.Exp)
            act("activation", [], [Bdt], out=eds_a, in_=dse_a, func=AF.Exp)
            dve("tensor_tensor", [], [Bdt], out=dtw_a, in0=dta, in1=eds_a, op=ALU.mult)

            def p4_load(c):
                s = c % 3
                dma(f"xsc{c % 2}", xsT_c[c % 2], xsT_d[:, c * 128:(c + 1) * 128].rearrange("(k p) t -> p k t", p=128), writes=[Bxs[c % 2]])
                dma(f"bcc{s}", bcT_c[s], bcT_d[:, c * 128:(c + 1) * 128].rearrange("(k p) t -> p k t", p=128), writes=[Bbc[s]])
                dma(f"szc{s}", sz_c[s], sz_d[c * 128:(c + 1) * 128, :], writes=[Bsz[s]])

            def stA(c):
                s = c % 2
                s3 = c % 3
                pool("tensor_tensor", [Bdt], [BR], out=Rm, in0=U_f.unsqueeze(1).to_broadcast([128, 16, 128]),
                     in1=dAa[:, c, :].unsqueeze(2).to_broadcast([128, 16, 128]), op=ALU.mult)
                for g in range(2):
                    pe("matmul", [Bbc[s3]], [Bpb[2]], out=pb[2][:, 64 + g * 128:64 + (g + 1) * 128], lhsT=bcT_c[s3][:, g, :], rhs=bcT_c[s3][:, 2 + g, :], start=True, stop=True)
                pbt2 = pb[2].bitcast(BF16)
                for g in range(2):
                    pe("transpose", [Bbc[s3]], [Bpb[2]], out=pbt2[:, 768 + g * 128:768 + (g + 1) * 128], in_=bcT_c[s3][:, g, :], identity=ident_bf)
                act("activation", [], [Bpb[2], Bcb], out=cb_sb, in_=pb[2][:, 64:320].rearrange("p (g l) -> p g l", l=128), func=AF.Copy)
                act("activation", [], [Bpb[2], BBtok[s]], out=Btok[s], in_=pbt2[:, 768:1024].rearrange("p (g l) -> p g l", l=128), func=AF.Copy)
                Rflat = Rm.rearrange("p h l -> p (h l)")
                for qd in range(4):
                    bk = qd % 2
                    pe("matmul", [BR], [Bpb[bk]], out=pb[bk], lhsT=ones_f, rhs=Rflat[:, qd * 512:(qd + 1) * 512], start=True, stop=False)
                    pe("matmul", [BR], [Bpb[bk]], out=pb[bk], lhsT=ident_bf, rhs=negb4.rearrange("p h l -> p (h l)"), start=False, stop=True)
                    for hh in range(4):
                        h = qd * 4 + hh
                        act("activation", [Bdt], [Bpb[bk], Bdecay], out=decay[:, h, :], in_=pb[bk][:, hh * 128:(hh + 1) * 128], func=AF.Exp, bias=nacs_a[:, c, h:h + 1])
                    g = qd // 2
                    dve("tensor_tensor", [Bdecay, Bcb], [BMT[s]], out=MT[s][:, qd * 4:(qd + 1) * 4, :], in0=decay[:, qd * 4:(qd + 1) * 4, :],
                        in1=cb_sb[:, g, :].unsqueeze(1).to_broadcast([128, 4, 128]), op=ALU.mult)
                for g in range(2):
                    for cc in range(4):
                        pe("transpose", [Bxs[s]], [Bpb[3]], out=pb[3][:, cc * 128:(cc + 1) * 128], in_=xsT_c[s][:, g * 4 + cc, :], identity=identf)
                    act("activation", [], [Bpb[3], Bxssb], out=xs_sb[:, g * 8:(g + 1) * 8, :], in_=pb[3].rearrange("p (h d) -> p h d", d=64), func=AF.Copy)
                    hs = slice(g * 8, (g + 1) * 8)
                    dve("tensor_tensor", [Bxssb, Bdt], [Bxdt[s]], out=xdt[s][:, hs, :], in0=xs_sb[:, hs, :],
                        in1=dta[:, c, hs].unsqueeze(2).to_broadcast([128, 8, 64]), op=ALU.mult)
                    pool("tensor_tensor", [Bxssb, Bdt], [Bxw[s]], out=xw[s][:, hs, :], in0=xs_sb[:, hs, :],
                         in1=dtw_a[:, c, hs].unsqueeze(2).to_broadcast([128, 8, 64]), op=ALU.mult)
                    pool("tensor_tensor", [Bxssb], [BxD[s]], out=xD[s][:, hs, :], in0=xs_sb[:, hs, :],
                         in1=hp[:, 32 + g * 8:32 + (g + 1) * 8].unsqueeze(2).to_broadcast([128, 8, 64]), op=ALU.mult)

            def stB1(c):
                s = c % 2
                s3 = c % 3
                for g in range(2):
                    hs = slice(g * 8, (g + 1) * 8)
                    for hh in range(8):
                        h = g * 8 + hh
                        pe("matmul", [BMT[s], Bxdt[s]], [Bpb[4]], out=pb[4][:, hh * 64:(hh + 1) * 64], lhsT=MT[s][:, h, :], rhs=xdt[s][:, h, :], start=True, stop=True)
                    pe("matmul", [Bbc[s3], Bprevbf], [Bpb[5]], out=pb[5], lhsT=bcT_c[s3][:, 2 + g, :], rhs=prevbf[:, hs, :].rearrange("p h d -> p (h d)"), start=True, stop=True)
                    pe("matmul", [BBtok[s], Bxw[s]], [Bpb[6]], out=pb[6], lhsT=Btok[s][:, g, :], rhs=xw[s][:, hs, :].rearrange("p h d -> p (h d)"), start=True, stop=True)
                    dve("tensor_tensor", [Bdt], [Bpb[5], Bt1], out=t1[:, hs, :], in0=pb[5].rearrange("p (h d) -> p h d", d=64),
                        in1=ea_a[:, c, hs].unsqueeze(2).to_broadcast([128, 8, 64]), op=ALU.mult)
                    dve("tensor_tensor", [], [Bpb[4], Bt1], out=t1[:, hs, :], in0=t1[:, hs, :], in1=pb[4].rearrange("p (h d) -> p h d", d=64), op=ALU.add)
                    dve("tensor_tensor", [BxD[s]], [Bt1], out=t1[:, hs, :], in0=t1[:, hs, :], in1=xD[s][:, hs, :], op=ALU.add)
                    pool("tensor_tensor", [Bt1, Bsz[s3]], [Byg], out=yg[:, hs, :], in0=t1[:, hs, :], in1=sz_c[s3][:, g * 512:(g + 1) * 512].rearrange("p (h d) -> p h d", d=64), op=ALU.mult)
                    act("activation", [Byg], [Bjunk4, Bss2], out=junk4, in_=yg[:, hs, :].rearrange("p h d -> p (h d)"), func=AF.Square, accum_out=ss2[:, g:g + 1])
                    dve("tensor_tensor", [Bdt], [Bprev], out=prev[:, hs, :], in0=prev[:, hs, :],
                        in1=cd_a[:, c, hs].unsqueeze(2).to_broadcast([128, 8, 64]), op=ALU.mult)
                    dve("tensor_tensor", [], [Bpb[6], Bprev], out=prev[:, hs, :], in0=prev[:, hs, :], in1=pb[6].rearrange("p (h d) -> p h d", d=64), op=ALU.add)
                    act("activation", [Bprev], [Bprevbf], out=prevbf[:, hs, :], in_=prev[:, hs, :], func=AF.Copy)
                pool("tensor_scalar", [], [Bss2], out=ss2, in0=ss2, scalar1=1.0 / 512, scalar2=EPS, op0=ALU.mult, op1=ALU.add)
                pool("tensor_tensor", [], [Bss2], out=ss2, in0=ss2, in1=mhalf[:, 0:2], op=ALU.pow)
                for g in range(2):
                    act("activation", [Byg, Bss2], [Byn2[s]], out=yn2[s][:, g * 512:(g + 1) * 512], in_=yg[:, g * 8:(g + 1) * 8, :].rearrange("p h d -> p (h d)"),
                        func=AF.Copy, scale=ss2[:, g:g + 1])

            def stB2(c):
                s = c % 2
                pbt7 = pb[7].bitcast(BF16)
                for cc in range(8):
                    pe("transpose", [Byn2[s]], [Bpb[7]], out=pbt7[:, cc * 128:(cc + 1) * 128], in_=yn2[s][:, cc * 128:(cc + 1) * 128], identity=ident_bf)
                ys = (c // 4) % 2
                j = c % 4
                dve("tensor_tensor", [], [Bpb[7], Byst[ys]], out=ystage[ys][:, :, j * 128:(j + 1) * 128], in0=pbt7.rearrange("p (c t) -> p c t", t=128),
                    in1=pl["snwT"].unsqueeze(2).to_broadcast([128, 8, 128]), op=ALU.mult)
                if j == 3:
                    tg = c // 4
                    dma(f"yst{ys}", mixT_d[8:16, :, tg * 512:(tg + 1) * 512].rearrange("h p t -> p h t"), ystage[ys], reads=[Byst[ys]])

            p4_load(0)
            if NCH > 1:
                p4_load(1)
            for t in range(NCH + 2):
                if t < NCH:
                    stA(t)
                if 1 <= t <= NCH:
                    stB1(t - 1)
                if t + 2 < NCH:
                    p4_load(t + 2)
                if t >= 2:
                    stB2(t - 2)
            S_.barrier()
            if stop_after == "P4":
                break

            AR.release(p5_mark)
            mixs = [AR.alloc((16, 512), BF16) for _ in range(2)]
            Bmix = [Buf("mix0"), Buf("mix1")]
            xr = [AR.alloc((D,), F32) for _ in range(2)]
            Bxr = [Buf("xr0"), Buf("xr1")]
            ot = [AR.alloc((D,), F32) for _ in range(2)]
            Bot = [Buf("ot0"), Buf("ot1")]

            def p5_loadmix(tg):
                s = tg % 2
                dma(f"mix{s}", mixs[s], mixT_d[:, :, tg * 512:(tg + 1) * 512].rearrange("c p t -> p c t"), writes=[Bmix[s]])

            def p5_loadx(i):
                s = i % 2
                dma(f"xr{s}", xr[s], xsrc[i * 128:(i + 1) * 128, :], writes=[Bxr[s]])

            def p5_tile(i):
                s = i % 2
                tg = i // 4
                j = i % 4
                ms = tg % 2
                for half in range(2):
                    bk = (2 * i + half) % 4
                    for cch in range(16):
                        pe("matmul", [Bmix[ms], Bwo], [Bpb[bk]], out=pb[bk], lhsT=mixs[ms][:, cch, j * 128:(j + 1) * 128], rhs=wo_bf[:, cch, half * 512:(half + 1) * 512],
                           start=(cch == 0), stop=(cch == 15))
                    dve("tensor_tensor", [Bxr[s]], [Bpb[bk], Bot[s]], out=ot[s][:, half * 512:(half + 1) * 512], in0=pb[bk], in1=xr[s][:, half * 512:(half + 1) * 512], op=ALU.add)
                dma(f"ot{s}", xdst[i * 128:(i + 1) * 128, :], ot[s], reads=[Bot[s]])

            p5_loadmix(0)
            p5_loadx(0)
            for i in range(NT):
                if i % 4 == 0 and i // 4 + 1 < TG:
                    p5_loadmix(i // 4 + 1)
                if i + 1 < NT:
                    p5_loadx(i + 1)
                p5_tile(i)
            S_.barrier()
            if stop_after == "L0":
                break

        S_.barrier()
        S_.finalize()
        with nc.Block() as block:
            @block.sync
            def _(e):
                S_.replay("sp")

            @block.tensor
            def _(e):
                S_.replay("pe")

            @block.vector
            def _(e):
                S_.replay("dve")

            @block.scalar
            def _(e):
                S_.replay("act")

            @block.gpsimd
            def _(e):
                S_.replay("pool")
    return nc


def host_inputs(inputs, b, S, depth=2):
    f = np.float32
    d = {}
    d["x"] = np.ascontiguousarray(inputs["x"][b, :S]).astype(f)
    d["w_in"] = np.ascontiguousarray(inputs["w_in"]).astype(f)
    d["w_out"] = np.ascontiguousarray(inputs["w_out"]).astype(f)
    d["lnwT"] = np.ascontiguousarray(inputs["ln_w"].reshape(depth, 8, 128).transpose(0, 2, 1)).astype(f)
    cw = inputs["conv_w"].reshape(depth, 4, 12, 128).transpose(0, 3, 2, 1)
    d["cwT"] = np.ascontiguousarray(cw.reshape(depth, 128, 48)).astype(f)
    d["cbT"] = np.ascontiguousarray(inputs["conv_b"].reshape(depth, 12, 128).transpose(0, 2, 1)).astype(f)
    d["qkw"] = np.ascontiguousarray(np.stack([inputs["q_norm_w"], inputs["k_norm_w"]], axis=-1)).astype(f)
    d["snwT"] = np.ascontiguousarray(inputs["ssd_norm_w"].reshape(depth, 8, 128).transpose(0, 2, 1)).astype(f)
    d["hp"] = np.ascontiguousarray(np.concatenate([inputs["dt_bias"], inputs["a_log"], inputs["d_skip"]], axis=-1).reshape(depth, 1, 48)).astype(f)
    d["consts"] = make_consts()
    return d


BATCH = 8
SEQ = 4096


def kernel(**inputs):
    inputs = {k: np.asarray(v) for k, v in inputs.items()}
    nc = build(SEQ, depth=2, dbg=False)
    in_maps = [host_inputs(inputs, b, SEQ) for b in range(BATCH)]
    res = run_bass_kernel_spmd(nc, in_maps, core_ids=list(range(BATCH)))
    out = np.stack([np.asarray(r["out"], dtype=np.float32) for r in res.results], axis=0)
    return out
```

```python
import contextlib
import numpy as np
import concourse.bass as bass
import concourse.mybir as mybir
from concourse.bass_utils import run_bass_kernel_spmd

F32 = mybir.dt.float32
BF16 = mybir.dt.bfloat16
AF = mybir.ActivationFunctionType
ALU = mybir.AluOpType
AX = mybir.AxisListType

ENGS = ("pe", "act", "dve", "pool", "sp")
SAME_ENGINE_SYNC = {"pe": False, "act": True, "dve": True, "pool": True, "sp": False}


class Ev:
    __slots__ = ("eng", "idx", "sem", "val", "needed")

    def __init__(self, eng, idx, sem=None, val=None):
        self.eng = eng
        self.idx = idx
        self.sem = sem
        self.val = val
        self.needed = False


def ev_is_dma(ev):
    return ev.sem is not None and not isinstance(ev.sem, tuple)


class Buf:
    __slots__ = ("name", "w", "rs")

    def __init__(self, name=""):
        self.name = name
        self.w = None
        self.rs = []


class Op:
    __slots__ = ("fn", "waits", "ev", "is_dma")

    def __init__(self, fn, waits, ev, is_dma):
        self.fn = fn
        self.waits = waits
        self.ev = ev
        self.is_dma = is_dma


class Sched:
    def __init__(self, nc):
        self.nc = nc
        self.ops = {e: [] for e in ENGS}
        self.handles = {"pe": nc.tensor, "act": nc.scalar, "dve": nc.vector, "pool": nc.gpsimd, "sp": nc.sync}
        self.esem = {}
        self.dma_sems = {}
        self.dma_cnt = {}
        self.all_dma_ev = {}

    def _deps(self, eng, reads, writes, xreads=()):
        out = []

        def add(ev, raw):
            if ev.eng == eng and not ev_is_dma(ev):
                if not (raw and SAME_ENGINE_SYNC[eng]):
                    return
            out.append(ev)

        for b in reads:
            if b.w is not None:
                add(b.w, True)
        for b in xreads:
            if b.w is not None:
                add(b.w, True)
            for r in b.rs:
                add(r, False)
        for b in writes:
            if b.w is not None:
                add(b.w, False)
            for r in b.rs:
                add(r, False)
        return out

    def _upd(self, ev, reads, writes):
        for b in reads:
            b.rs.append(ev)
        for b in writes:
            b.w = ev
            b.rs = []

    def op(self, eng, fn, reads=(), writes=(), xreads=()):
        waits = self._deps(eng, reads, writes, xreads)
        ev = Ev(eng, len(self.ops[eng]))
        self.ops[eng].append(Op(fn, waits, ev, False))
        self._upd(ev, list(reads) + list(xreads), writes)
        return ev

    def dma(self, eng, semname, out, in_, reads=(), writes=(), **kw):
        waits = self._deps(eng, reads, writes)
        self.dma_cnt[semname] += 16
        ev = Ev(eng, len(self.ops[eng]), sem=semname, val=self.dma_cnt[semname])
        h = self.handles[eng]
        fn = (lambda h=h, out=out, in_=in_, kw=kw: h.dma_start(out=out, in_=in_, **kw))
        self.ops[eng].append(Op(fn, waits, ev, True))
        self.all_dma_ev[semname] = ev
        self._upd(ev, reads, writes)
        return ev

    def barrier(self):
        lasts = []
        for e in ENGS:
            for o in reversed(self.ops[e]):
                if not o.is_dma and o.fn is not None:
                    lasts.append(o.ev)
                    break
        lasts.extend(self.all_dma_ev.values())
        for e in ENGS:
            waits = [ev for ev in lasts if not (ev.eng == e and not ev_is_dma(ev))]
            ev = Ev(e, len(self.ops[e]))
            self.ops[e].append(Op(None, waits, ev, False))

    def finalize(self):
        for e in ENGS:
            for o in self.ops[e]:
                for ev in o.waits:
                    ev.needed = True
        for e in ENGS:
            cnt = 0
            for o in self.ops[e]:
                if o.is_dma:
                    continue
                if o.ev.needed:
                    assert o.fn is not None
                    cnt += 1
                    o.ev.sem = ("E", e)
                    o.ev.val = cnt

    def _sem(self, key):
        if isinstance(key, tuple):
            return self.esem[key[1]]
        return self.dma_sems[key]

    def replay(self, eng):
        h = self.handles[eng]
        seen = {}
        for o in self.ops[eng]:
            need = {}
            for ev in o.waits:
                if need.get(ev.sem, 0) < ev.val:
                    need[ev.sem] = ev.val
            for k, v in need.items():
                if seen.get(k, 0) < v:
                    h.wait_ge(self._sem(k), v)
                    seen[k] = v
            if o.fn is None:
                continue
            ins = o.fn()
            if o.is_dma:
                ins.then_inc(self._sem(o.ev.sem), 16)
            elif o.ev.needed:
                ins.then_inc(self._sem(o.ev.sem), 1)


class Arena:
    def __init__(self, base, nwords):
        self.base = base
        self.cap = nwords * 4
        self.off = 0

    def mark(self):
        return self.off

    def release(self, m):
        self.off = m

    def alloc(self, shape, dt):
        n = int(np.prod(shape))
        esz = 4 if dt == F32 else 2
        nbytes = (n * esz + 63) // 64 * 64
        st = self.off
        self.off += nbytes
        assert self.off <= self.cap, f"SBUF arena overflow {self.off} > {self.cap}"
        w0 = st // 4
        if dt == F32:
            v = self.base[:, w0:w0 + n]
        else:
            v = self.base[:, w0:w0 + nbytes // 4].bitcast(BF16)[:, 0:n]
        if len(shape) == 2:
            v = v.rearrange("p (a b) -> p a b", a=shape[0], b=shape[1])
        elif len(shape) == 3:
            v = v.rearrange("p (a b c) -> p a b c", a=shape[0], b=shape[1], c=shape[2])
        return v


D = 1024
KC = 8
NPROJ = 6672
EPS = 1e-6
NEGBIG = -30000.0

C_IDENT = 0
C_U = 128
C_NEGU = 256
C_NEG = 384
C_ONES = 512
C_BH = 640
C_BIAS = C_BH + 2048
C_G = C_BIAS + 240
C_PAST = C_G + 16
C_E = C_PAST + 256
NCONST = C_E + 2048


def make_consts():
    c = np.zeros((128, NCONST), np.float32)
    p = np.arange(128)
    c[:, C_IDENT:C_IDENT + 128] = np.eye(128)
    U = (p[:, None] <= p[None, :]).astype(np.float32)
    c[:, C_U:C_U + 128] = U
    c[:, C_NEGU:C_NEGU + 128] = -U
    c[:, C_NEG:C_NEG + 128] = np.where(p[None, :] < p[:, None], NEGBIG, 0.0)
    c[:, C_ONES:C_ONES + 128] = 1.0
    slopes = 2.0 ** (-8.0 * np.arange(1, 9) / 8.0)
    qrel = np.arange(256)
    for h in range(8):
        d = qrel[None, :] - p[:, None]
        c[:, C_BH + h * 256:C_BH + (h + 1) * 256] = np.where(d >= 0, -slopes[h] * d, NEGBIG)
        for dd in range(1, 16):
            for kt in range(2):
                c[:, C_BIAS + h * 30 + (dd - 1) * 2 + kt] = -slopes[h] * (dd * 256 - kt * 128 - p)
        for hf in range(2):
            c[:, C_G + h * 2 + hf] = np.exp(-slopes[h] * (hf * 128 + p))
    for qb in range(16):
        for n in range(16):
            c[:, C_PAST + qb * 16 + n] = 0.0 if n < qb else -1e30
    for n in range(16):
        c[n, C_E + n * 128:C_E + (n + 1) * 128] = 1.0
    return c


def pipeline(n, stages):
    maxlag = max(l for l, _ in stages)
    for t in range(n + maxlag):
        for lag, fn in stages:
            i = t - lag
            if 0 <= i < n:
                fn(i)


def build(S, depth=2, dbg=False, stop_after=None):
    NT = S // 128
    NB = S // 256
    TG = S // 512
    assert S % 512 == 0 and NB <= 16
    nc = bass.Bass("TRN2", target_bir_lowering=False)
    V, A, P, T = nc.vector, nc.scalar, nc.gpsimd, nc.tensor

    def din(name, shape, dt=F32):
        return nc.dram_tensor(name, list(shape), dt, kind="ExternalInput").ap()

    def dscr(name, shape, dt):
        kind = "ExternalOutput" if dbg else "Internal"
        return nc.dram_tensor(name, list(shape), dt, kind=kind).ap()

    x_in = din("x", [S, D])
    w_in = din("w_in", [depth, D, NPROJ])
    w_out = din("w_out", [depth, 2 * D, D])
    lnwT_d = din("lnwT", [depth, 128, 8])
    cwT_d = din("cwT", [depth, 128, 12 * 4])
    cbT_d = din("cbT", [depth, 128, 12])
    qkw_d = din("qkw", [depth, 128, 2])
    snwT_d = din("snwT", [depth, 128, 8])
    hp_d = din("hp", [depth, 1, 48])
    consts_d = din("consts", [128, NCONST])
    out_d = nc.dram_tensor("out", [S, D], F32, kind="ExternalOutput").ap()

    qT_d = dscr("qT_s", [8, 128, S], BF16)
    kT_d = dscr("kT_s", [8, 128, S], BF16)
    V_d = dscr("V_s", [S, D], BF16)
    sg_d = dscr("sg_s", [S, D], F32)
    sz_d = dscr("sz_s", [S, D], F32)
    xsT_d = dscr("xsT_s", [D, S], F32)
    bcT_d = dscr("bcT_s", [512, S], BF16)
    mixT_d = dscr("mixT_s", [16, 128, S], BF16)
    xres_d = dscr("xres_s", [S, D], F32)
    dtraw_d = dscr("dtraw_s", [128, NT * 16], F32) if dbg else None
    hT_d = dscr("hT_s", [128, KC * S], BF16) if dbg else None

    ARENA_WORDS = 52736
    with contextlib.ExitStack() as st:
        arena_t = st.enter_context(nc.sbuf_tensor("arena", [128, ARENA_WORDS], F32))
        AR = Arena(arena_t[:, :], ARENA_WORDS)
        pbanks = [st.enter_context(nc.psum_tensor(f"pb{i}", [128, 512], F32)) for i in range(8)]
        pb = [t[:, :] for t in pbanks]
        Bpb = [Buf(f"pb{i}") for i in range(8)]
        S_ = Sched(nc)
        S_.esem = {e: st.enter_context(nc.semaphore("sem_" + e)) for e in ENGS}
        sem_pool = {}

        def dsem(name):
            if name not in sem_pool:
                sem_pool[name] = st.enter_context(nc.semaphore("d_" + name))
                S_.dma_sems[name] = sem_pool[name]
                S_.dma_cnt[name] = 0
            return name

        def eop(eng, h, fname, reads, writes, **kw):
            f = getattr(h, fname)
            xr = ()
            if eng != "pe":
                xr = [b for b in writes if b in Bpb]
                writes = [b for b in writes if b not in Bpb]
                reads = list(reads) + [b for b in writes if b not in reads]
            return S_.op(eng, (lambda f=f, kw=kw: f(**kw)), reads, writes, xr)

        def dve(fname, reads, writes, **kw):
            return eop("dve", V, fname, reads, writes, **kw)

        def act(fname, reads, writes, **kw):
            return eop("act", A, fname, reads, writes, **kw)

        def pool(fname, reads, writes, **kw):
            return eop("pool", P, fname, reads, writes, **kw)

        def pe(fname, reads, writes, **kw):
            return eop("pe", T, fname, reads, writes, **kw)

        def dma(semname, out, in_, reads=(), writes=(), eng="sp"):
            return S_.dma(eng, dsem(semname), out, in_, reads, writes)

        cst = AR.alloc((NCONST,), F32)
        Bcst = Buf("cst")
        ident_bf = AR.alloc((128,), BF16)
        bh_bf = AR.alloc((8, 256), BF16)
        e_bf = AR.alloc((16, 128), BF16)
        neg_bf = AR.alloc((128,), BF16)
        mhalf = AR.alloc((8,), F32)
        Bk = Buf("constbf")
        par = {}
        Bpar = Buf("par")
        for l in range(depth):
            par[l] = dict(
                lnwT=AR.alloc((8,), F32), cwT=AR.alloc((12, 4), F32), cbT=AR.alloc((12,), F32),
                qkw=AR.alloc((2,), F32), snwT=AR.alloc((8,), F32), hp=AR.alloc((48,), F32),
                qws=AR.alloc((1,), F32), aneg=AR.alloc((16,), F32))
        dtraw = AR.alloc((NT, 16), F32)
        Bdtraw = Buf("dtraw")

        identf = cst[:, C_IDENT:C_IDENT + 128]

        dma("cst", cst, consts_d[:, :], writes=[Bcst])
        for l in range(depth):
            pl = par[l]
            dma("par", pl["lnwT"], lnwT_d[l], writes=[Bpar])
            dma("par", pl["cwT"], cwT_d[l].rearrange("p (c k) -> p c k", k=4), writes=[Bpar])
            dma("par", pl["cbT"], cbT_d[l], writes=[Bpar])
            dma("par", pl["qkw"], qkw_d[l], writes=[Bpar])
            dma("par", pl["snwT"], snwT_d[l], writes=[Bpar])
            dma("par", pl["hp"], hp_d[l].partition_broadcast(128), writes=[Bpar])
        pool("tensor_copy", [Bcst], [Bk], out=ident_bf, in_=identf)
        pool("tensor_copy", [Bcst], [Bk], out=bh_bf, in_=cst[:, C_BH:C_BH + 2048].rearrange("p (h q) -> p h q", q=256))
        pool("tensor_copy", [Bcst], [Bk], out=e_bf, in_=cst[:, C_E:C_E + 2048].rearrange("p (n k) -> p n k", k=128))
        pool("tensor_copy", [Bcst], [Bk], out=neg_bf, in_=cst[:, C_NEG:C_NEG + 128])
        pool("memset", [], [Bk], ap=mhalf, constant=-0.5)
        for l in range(depth):
            pl = par[l]
            pool("tensor_scalar", [Bpar], [Bpar], out=pl["qws"], in0=pl["qkw"][:, 0:1], scalar1=float(128 ** -0.5), scalar2=None, op0=ALU.mult)
            act("activation", [Bpar], [Bpar], out=pl["aneg"], in_=pl["hp"][:, 16:32], func=AF.Exp)
            pool("tensor_scalar", [Bpar], [Bpar], out=pl["aneg"], in0=pl["aneg"], scalar1=-1.0, scalar2=None, op0=ALU.mult)
        S_.barrier()

        base_mark = AR.mark()
        p3_mark = base_mark

        for l in range(depth):
            pl = par[l]
            xsrc = x_in if l == 0 else xres_d
            xdst = xres_d if l < depth - 1 else out_d
            AR.release(base_mark)
            hT = AR.alloc((KC, S), BF16)
            BhT = Buf("hT")
            m1 = AR.mark()
            xt = [AR.alloc((D,), F32) for _ in range(2)]
            Bxt = [Buf("xt0"), Buf("xt1")]
            xn = [AR.alloc((D,), BF16) for _ in range(2)]
            Bxn = [Buf("xn0"), Buf("xn1")]
            junk = AR.alloc((D,), BF16)
            Bjunk = Buf("junk")
            ss1 = [AR.alloc((1,), F32) for _ in range(2)]
            Bss1 = [Buf("ss0"), Buf("ss1")]

            def p1_load(i):
                s = i % 2
                dma(f"xt{s}", xt[s], xsrc[i * 128:(i + 1) * 128, :], writes=[Bxt[s]])

            def p1_norm(i):
                s = i % 2
                act("activation", [Bxt[s]], [Bjunk, Bss1[s]], out=junk, in_=xt[s], func=AF.Square, accum_out=ss1[s])
                pool("tensor_scalar", [Bss1[s]], [Bss1[s]], out=ss1[s], in0=ss1[s], scalar1=1.0 / D, scalar2=EPS, op0=ALU.mult, op1=ALU.add)
                pool("tensor_tensor", [Bss1[s], Bk], [Bss1[s]], out=ss1[s], in0=ss1[s], in1=mhalf[:, 0:1], op=ALU.pow)
                act("activation", [Bxt[s], Bss1[s]], [Bxn[s]], out=xn[s], in_=xt[s], func=AF.Copy, scale=ss1[s][:, 0:1])

            def p1_tr(i):
                s = i % 2
                bk = 6 + (i % 2)
                pbt = pb[bk].bitcast(BF16)
                for c in range(KC):
                    pe("transpose", [Bxn[s], Bk], [Bpb[bk]], out=pbt[:, c * 128:(c + 1) * 128], in_=xn[s][:, c * 128:(c + 1) * 128], identity=ident_bf)
                dve("tensor_tensor", [Bpar], [Bpb[bk], BhT], out=hT[:, :, i * 128:(i + 1) * 128],
                    in0=pbt.rearrange("p (c t) -> p c t", t=128),
                    in1=pl["lnwT"].unsqueeze(2).to_broadcast([128, KC, 128]), op=ALU.mult)

            p1_load(0)
            for t in range(NT + 1):
                if t + 1 < NT:
                    p1_load(t + 1)
                if t < NT:
                    p1_norm(t)
                if t >= 1:
                    p1_tr(t - 1)
            if dbg and l == 0:
                dma("dbg", hT_d[:, :], hT.rearrange("p a b -> p (a b)"), reads=[BhT])
            if stop_after == "P1":
                break

            wst = [AR.alloc((KC, 512), F32) for _ in range(2)]
            Bwst = [Buf("wst0"), Buf("wst1")]
            wbf = [AR.alloc((KC, 512), BF16) for _ in range(2)]
            Bwbf = [Buf("wbf0"), Buf("wbf1")]
            NG = 14
            w_l = w_in[l]

            def gcols(g):
                c0 = g * 512
                return c0, min(512, NPROJ - c0)

            def wload(g):
                s = g % 2
                c0, ncl = gcols(g)
                dma(f"wst{s}", wst[s][:, :, 0:ncl], w_l[:, c0:c0 + ncl].rearrange("(k p) n -> p k n", p=128), writes=[Bwst[s]])

            def wconv(g):
                s = g % 2
                c0, ncl = gcols(g)
                pool("tensor_copy", [Bwst[s]], [Bwbf[s]], out=wbf[s][:, :, 0:ncl], in_=wst[s][:, :, 0:ncl])

            ss4 = [AR.alloc((4,), F32) for _ in range(2)]
            Bss4 = [Buf("ss4a"), Buf("ss4b")]
            sq = [AR.alloc((512,), F32) for _ in range(2)]
            Bsq = [Buf("sq0"), Buf("sq1")]
            qn = [AR.alloc((4, 128), BF16) for _ in range(2)]
            Bqn = [Buf("qn0"), Buf("qn1")]
            qst = [AR.alloc((4, 512), BF16) for _ in range(2)]
            Bqst = [Buf("qst0"), Buf("qst1")]
            vst = [AR.alloc((512,), BF16) for _ in range(2)]
            Bvst = [Buf("vst0"), Buf("vst1")]
            fst = [AR.alloc((512,), F32) for _ in range(2)]
            Bfst = [Buf("fst0"), Buf("fst1")]
            ubuf = [AR.alloc((515,), F32) for _ in range(2)]
            Bu = [Buf("u0"), Buf("u1")]
            cacc = [AR.alloc((512,), F32) for _ in range(2)]
            Bcacc = [Buf("cacc0"), Buf("cacc1")]
            xo_f = [AR.alloc((512,), F32) for _ in range(2)]
            xo_b = [AR.alloc((512,), BF16) for _ in range(2)]
            Bxo = [Buf("xo0"), Buf("xo1")]
            accn = [0]

            def next_acc():
                b = accn[0] % 4
                accn[0] += 1
                return b

            def mm_tok(g, i, ncl, bk):
                s = g % 2
                for k in range(KC):
                    pe("matmul", [BhT, Bwbf[s]], [Bpb[bk]], out=pb[bk][:, 0:ncl], lhsT=hT[:, k, i * 128:(i + 1) * 128],
                       rhs=wbf[s][:, k, 0:ncl], start=(k == 0), stop=(k == KC - 1))

            def do_qk(g):
                which = 0 if g < 2 else 1
                h0 = (g % 2) * 4
                dst = qT_d if which == 0 else kT_d
                wsc = pl["qws"] if which == 0 else pl["qkw"][:, 1:2]
                banks = {}

                def s0(i):
                    bk = next_acc()
                    banks[i] = bk
                    mm_tok(g, i, 512, bk)
                    s = i % 2
                    act("activation", [], [Bpb[bk], Bsq[s]], out=sq[s], in_=pb[bk], func=AF.Square)
                    dve("tensor_reduce", [Bsq[s]], [Bss4[s]], out=ss4[s], in_=sq[s].rearrange("p (h d) -> p h d", d=128), axis=AX.X, op=ALU.add)
                    pool("tensor_scalar", [], [Bss4[s]], out=ss4[s], in0=ss4[s], scalar1=1.0 / 128, scalar2=EPS, op0=ALU.mult, op1=ALU.add)
                    pool("tensor_tensor", [Bk], [Bss4[s]], out=ss4[s], in0=ss4[s], in1=mhalf[:, 0:4], op=ALU.pow)
                    dve("tensor_tensor", [Bss4[s]], [Bpb[bk], Bqn[s]], out=qn[s], in0=pb[bk].rearrange("p (h d) -> p h d", d=128),
                        in1=ss4[s].unsqueeze(2).to_broadcast([128, 4, 128]), op=ALU.mult)

                def s2(i):
                    s = i % 2
                    bk = 4 + (i % 2)
                    pbt = pb[bk].bitcast(BF16)
                    for hh in range(4):
                        pe("transpose", [Bqn[s], Bk], [Bpb[bk]], out=pbt[:, hh * 128:(hh + 1) * 128], in_=qn[s][:, hh, :], identity=ident_bf)
                    ss_ = (i // 4) % 2
                    j = i % 4
                    act("activation", [Bpar], [Bpb[bk], Bqst[ss_]], out=qst[ss_][:, :, j * 128:(j + 1) * 128],
                        in_=pbt[:, 0:512].rearrange("p (h t) -> p h t", t=128), func=AF.Copy, scale=wsc)
                    if j == 3:
                        tg = i // 4
                        dma(f"qst{ss_}", dst[h0:h0 + 4, :, tg * 512:(tg + 1) * 512].rearrange("h p t -> p h t"), qst[ss_], reads=[Bqst[ss_]])

                pipeline(NT, [(2, s2), (0, s0)])

            def do_tok(g, kind):
                c0 = (g % 2) * 512

                def s0(i):
                    bk = next_acc()
                    mm_tok(g, i, 512, bk)
                    s = i % 2
                    if kind == "v":
                        act("activation", [], [Bpb[bk], Bvst[s]], out=vst[s], in_=pb[bk], func=AF.Copy)
                        dma(f"vst{s}", V_d[i * 128:(i + 1) * 128, c0:c0 + 512], vst[s], reads=[Bvst[s]])
                    else:
                        act("activation", [], [Bpb[bk], Bfst[s]], out=fst[s], in_=pb[bk], func=AF.Silu)
                        dd = sg_d if kind == "g" else sz_d
                        dma(f"fst{s}", dd[i * 128:(i + 1) * 128, c0:c0 + 512], fst[s], reads=[Bfst[s]])

                pipeline(NT, [(0, s0)])

            def do_feat(g):
                s_w = g % 2
                nloc = 4
                items = [(cl, tg) for cl in range(nloc) for tg in range(TG)]

                def s0(ix):
                    cl, tg = items[ix]
                    cc = (g - 10) * 4 + cl
                    bk = next_acc()
                    for k in range(KC):
                        pe("matmul", [BhT, Bwbf[s_w]], [Bpb[bk]], out=pb[bk], lhsT=wbf[s_w][:, k, cl * 128:(cl + 1) * 128],
                           rhs=hT[:, k, tg * 512:(tg + 1) * 512], start=(k == 0), stop=(k == KC - 1))
                    s = ix % 2
                    if tg == 0:
                        pool("memset", [], [Bu[s]], ap=ubuf[s][:, 0:3], constant=0.0)
                    else:
                        pool("tensor_copy", [Bu[1 - s]], [Bu[s]], out=ubuf[s][:, 0:3], in_=ubuf[1 - s][:, 512:515])
                    act("activation", [], [Bpb[bk], Bu[s]], out=ubuf[s][:, 3:515], in_=pb[bk], func=AF.Copy)
                    cw = pl["cwT"]
                    act("activation", [Bu[s]], [Bcacc[s]], out=cacc[s], in_=ubuf[s][:, 3:515], func=AF.Copy, scale=cw[:, cc, 3:4])
                    for kk in (2, 1, 0):
                        dve("scalar_tensor_tensor", [Bu[s], Bpar], [Bcacc[s]], out=cacc[s], in0=ubuf[s][:, kk:kk + 512], scalar=cw[:, cc, kk:kk + 1],
                            in1=cacc[s], op0=ALU.mult, op1=ALU.add)
                    if cc < 8:
                        act("activation", [Bcacc[s], Bpar], [Bxo[s]], out=xo_f[s], in_=cacc[s], func=AF.Silu, bias=pl["cbT"][:, cc:cc + 1])
                        dma(f"xo{s}", xsT_d[cc * 128:(cc + 1) * 128, tg * 512:(tg + 1) * 512], xo_f[s], reads=[Bxo[s]])
                    else:
                        act("activation", [Bcacc[s], Bpar], [Bxo[s]], out=xo_b[s], in_=cacc[s], func=AF.Silu, bias=pl["cbT"][:, cc:cc + 1])
                        dma(f"xo{s}", bcT_d[(cc - 8) * 128:(cc - 7) * 128, tg * 512:(tg + 1) * 512], xo_b[s], reads=[Bxo[s]])

                pipeline(len(items), [(0, s0)])

            def do_dt(g):
                def s0(i):
                    bk = next_acc()
                    mm_tok(g, i, 16, bk)
                    dve("tensor_copy", [], [Bpb[bk], Bdtraw], out=dtraw[:, i, :], in_=pb[bk][:, 0:16])
                pipeline(NT, [(0, s0)])

            wload(0)
            wconv(0)
            wload(1)
            for g in range(NG):
                if g + 1 < NG:
                    wconv(g + 1)
                if g + 2 < NG:
                    wload(g + 2)
                if g < 4:
                    do_qk(g)
                elif g < 6:
                    do_tok(g, "v")
                elif g < 8:
                    do_tok(g, "g")
                elif g < 10:
                    do_tok(g, "z")
                elif g < 13:
                    do_feat(g)
                else:
                    do_dt(g)
            if dbg:
                dma("dbg", dtraw_d[:, :], dtraw.rearrange("p a b -> p (a b)"), reads=[Bdtraw])
            S_.barrier()
            if stop_after == "P2":
                break
            AR.release(p3_mark)
            qT_h = [AR.alloc((S,), BF16) for _ in range(2)]
            kT_h = [AR.alloc((S,), BF16) for _ in range(2)]
            V_h = [AR.alloc((NT, 129), BF16) for _ in range(2)]
            sg_h = [AR.alloc((NT, 128), F32) for _ in range(2)]
            Bq = [Buf("q0"), Buf("q1")]
            Bkk = [Buf("k0"), Buf("k1")]
            Bv = [Buf("v0"), Buf("v1")]
            Bsg = [Buf("sg0"), Buf("sg1")]
            attnT = [AR.alloc((S,), BF16) for _ in range(2)]
            Battn = [Buf("at0"), Buf("at1")]
            maskT = [AR.alloc((S,), BF16) for _ in range(2)]
            Bmask = [Buf("mk0"), Buf("mk1")]
            km = AR.alloc((16,), F32)
            kms = AR.alloc((16,), F32)
            kmh = [AR.alloc((16,), BF16) for _ in range(2)]
            kml = [AR.alloc((16,), BF16) for _ in range(2)]
            Bkm = Buf("km")
            Bkmhl = [Buf("kmhl0"), Buf("kmhl1")]
            gm = [AR.alloc((16,), F32) for _ in range(2)]
            top8 = [AR.alloc((8,), F32) for _ in range(2)]
            thr = [AR.alloc((1,), F32) for _ in range(2)]
            mk = [AR.alloc((16,), BF16) for _ in range(2)]
            Bgm = [Buf("gm0"), Buf("gm1")]
            Bmkk = [Buf("mkk0"), Buf("mkk1")]
            NPT = 6
            PT = [AR.alloc((512,), BF16) for _ in range(NPT)]
            BPT = [Buf(f"pt{i}") for i in range(NPT)]
            tsc = [AR.alloc((129,), F32) for _ in range(2)]
            num = [AR.alloc((129,), F32) for _ in range(2)]
            rden = [AR.alloc((1,), F32) for _ in range(2)]
            o_bf = [AR.alloc((128,), BF16) for _ in range(2)]
            Bcmb = [Buf("cmb0"), Buf("cmb1")]
            Bobf = [Buf("obf0"), Buf("obf1")]

            pool("memset", [], [Bkm], ap=km, constant=0.0)
            for s in range(2):
                pool("memset", [], [Bv[s]], ap=V_h[s][:, :, 128:129], constant=1.0)
                pool("memset", [], [Bmask[s]], ap=maskT[s], constant=0.0)

            def head_load(h):
                s = h % 2
                dma(f"hq{s}", qT_h[s], qT_d[h], writes=[Bq[s]])
                dma(f"hk{s}", kT_h[s], kT_d[h], writes=[Bkk[s]])
                dma(f"hv{s}", V_h[s][:, :, 0:128], V_d[:, h * 128:(h + 1) * 128].rearrange("(t p) d -> p t d", p=128), writes=[Bv[s]])
                dma(f"hg{s}", sg_h[s], sg_d[:, h * 128:(h + 1) * 128].rearrange("(t p) d -> p t d", p=128), writes=[Bsg[s]])

            def head_kmean(h):
                s = h % 2
                dve("tensor_reduce", [Bkk[s]], [Bkm], out=km[:, 0:NB], in_=kT_h[s].rearrange("p (n k) -> p n k", k=256), axis=AX.X, op=ALU.add)
                dve("tensor_scalar", [], [Bkm], out=kms, in0=km, scalar1=1.0 / 256, scalar2=None, op0=ALU.mult)
                dve("tensor_copy", [Bkm], [Bkmhl[s]], out=kmh[s], in_=kms)
                dve("tensor_tensor", [Bkm], [Bkmhl[s]], out=kml[s], in0=kms, in1=kmh[s], op=ALU.subtract)

            def gate_front(h, qb):
                s = h % 2
                for j in range(2):
                    i = 2 * qb + j
                    pe("matmul", [Bq[s], Bkmhl[s]], [Bpb[6]], out=pb[6][:, j * 16:(j + 1) * 16], lhsT=qT_h[s][:, i * 128:(i + 1) * 128], rhs=kmh[s], start=True, stop=False)
                    pe("matmul", [Bq[s], Bkmhl[s]], [Bpb[6]], out=pb[6][:, j * 16:(j + 1) * 16], lhsT=qT_h[s][:, i * 128:(i + 1) * 128], rhs=kml[s], start=False, stop=True)
                for j in range(2):
                    dve("tensor_tensor", [], [Bpb[6], Bgm[j]], out=gm[j], in0=pb[6][:, j * 16:(j + 1) * 16], in1=cst[:, C_PAST + qb * 16:C_PAST + (qb + 1) * 16], op=ALU.add)
                    dve("max", [], [Bgm[j]], out=top8[j], in_=gm[j])
                    dve("tensor_scalar", [], [Bgm[j]], out=thr[j], in0=top8[j][:, 2:3], scalar1=-1e29, scalar2=None, op0=ALU.max)
                    dve("tensor_scalar", [Bgm[j]], [Bmkk[j]], out=mk[j], in0=gm[j], scalar1=thr[j][:, 0:1], scalar2=NEGBIG, op0=ALU.is_lt, op1=ALU.mult)

            def gate_back(h, qb):
                s = h % 2
                pbt = pb[7].bitcast(BF16)
                for j in range(2):
                    pe("transpose", [Bmkk[j]], [Bpb[7]], out=pbt[0:16, j * 128:(j + 1) * 128], in_=mk[j], identity=ident_bf)
                dve("tensor_copy", [], [Bpb[7], Bmask[s]], out=maskT[s][0:16, qb * 256:(qb + 1) * 256], in_=pbt[0:16, 0:256])

            deferred = []

            def defer(n, fn):
                deferred.append([n, fn])

            def tick():
                for d_ in list(deferred):
                    d_[0] -= 1
                    if d_[0] <= 0:
                        deferred.remove(d_)
                        d_[1]()

            def flush():
                while deferred:
                    d_ = deferred.pop(0)
                    d_[1]()

            def attn_head(h, first, last_head):
                s = h % 2
                NP = S // 512
                items = []
                for p in range(NP):
                    for kt in range(4 * p):
                        items.append(("pc", p, kt))
                    items.append(("own0", 2 * p, 4 * p))
                    items.append(("own1", 2 * p, 4 * p + 1))
                    items.append(("po", p, 4 * p))
                    items.append(("po", p, 4 * p + 1))
                    items.append(("own0", 2 * p + 1, 4 * p + 2))
                    items.append(("own1", 2 * p + 1, 4 * p + 3))
                slots = {}
                done_b = set()
                seen_pair = set()
                seen_po = set()

                def exp_past(sb_, ps_, c0, qb, n, kt):
                    bcol = C_BIAS + h * 30 + (qb - n - 1) * 2 + (kt % 2)
                    act("activation", [], [Bpb[sb_], BPT[ps_]], out=PT[ps_][:, c0:c0 + 256], in_=pb[sb_][:, c0:c0 + 256], func=AF.Exp, bias=cst[:, bcol:bcol + 1])

                def stA(ix):
                    kind, a0, kt = items[ix]
                    sb_ = ix % 3
                    ps_ = ix % NPT
                    slots[ix] = ps_
                    if not last_head:
                        if kind in ("pc", "own0") and (a0 if kind == "pc" else a0 // 2) not in seen_pair and (kind == "pc" or a0 % 2 == 0):
                            p_ = a0 if kind == "pc" else a0 // 2
                            seen_pair.add(p_)
                            gate_front(h + 1, 2 * p_)
                            defer(4, (lambda qb=2 * p_: gate_back(h + 1, qb)))
                        if kind == "po" and a0 not in seen_po:
                            seen_po.add(a0)
                            gate_front(h + 1, 2 * a0 + 1)
                            defer(4, (lambda qb=2 * a0 + 1: gate_back(h + 1, qb)))
                    n = kt // 2
                    if kind == "pc":
                        p = a0
                        qpair = qT_h[s][:, p * 512:(p + 1) * 512]
                        pe("matmul", [Bq[s], Bkk[s]], [Bpb[sb_]], out=pb[sb_], lhsT=kT_h[s][:, kt * 128:(kt + 1) * 128], rhs=qpair, start=True, stop=False)
                        if ix - 2 >= 0:
                            stB(ix - 2)
                            done_b.add(ix - 2)
                        pe("matmul", [Bmask[s]], [Bpb[sb_]], out=pb[sb_], lhsT=e_bf[0:16, n, :], rhs=maskT[s][0:16, p * 512:(p + 1) * 512], start=False, stop=True)
                        exp_past(sb_, ps_, 0, 2 * p, n, kt)
                        exp_past(sb_, ps_, 256, 2 * p + 1, n, kt)
                    elif kind == "po":
                        qb = 2 * a0 + 1
                        qblk = qT_h[s][:, qb * 256:(qb + 1) * 256]
                        pe("matmul", [Bq[s], Bkk[s]], [Bpb[sb_]], out=pb[sb_][:, 0:256], lhsT=kT_h[s][:, kt * 128:(kt + 1) * 128], rhs=qblk, start=True, stop=False)
                        pe("matmul", [Bmask[s]], [Bpb[sb_]], out=pb[sb_][:, 0:256], lhsT=e_bf[0:16, n, :], rhs=maskT[s][0:16, qb * 256:(qb + 1) * 256], start=False, stop=True)
                        exp_past(sb_, ps_, 0, qb, n, kt)
                    elif kind == "own0":
                        qb = a0
                        qblk = qT_h[s][:, qb * 256:(qb + 1) * 256]
                        pe("matmul", [Bq[s], Bkk[s]], [Bpb[sb_]], out=pb[sb_][:, 0:256], lhsT=kT_h[s][:, kt * 128:(kt + 1) * 128], rhs=qblk, start=True, stop=False)
                        pe("matmul", [], [Bpb[sb_]], out=pb[sb_][:, 0:256], lhsT=ident_bf, rhs=bh_bf[:, h, :], start=False, stop=True)
                        act("activation", [], [Bpb[sb_], BPT[ps_]], out=PT[ps_][:, 0:256], in_=pb[sb_][:, 0:256], func=AF.Exp)
                    else:
                        qb = a0
                        qblk = qT_h[s][:, qb * 256:(qb + 1) * 256]
                        pe("matmul", [Bq[s], Bkk[s]], [Bpb[sb_]], out=pb[sb_][:, 0:128], lhsT=kT_h[s][:, kt * 128:(kt + 1) * 128], rhs=qblk[:, 128:256], start=True, stop=False)
                        pe("matmul", [], [Bpb[sb_]], out=pb[sb_][:, 0:128], lhsT=ident_bf, rhs=bh_bf[:, h, 0:128], start=False, stop=True)
                        act("activation", [], [Bpb[sb_], BPT[ps_]], out=PT[ps_][:, 0:128], in_=pb[sb_][:, 0:128], func=AF.Exp)
                    tick()

                def stB(ix):
                    kind, a0, kt = items[ix]
                    ps_ = slots[ix]
                    if kind == "pc":
                        p = a0
                        for j in range(4):
                            bk = 3 + j // 2
                            c0 = (j % 2) * 256
                            pe("matmul", [BPT[ps_], Bv[s]], [Bpb[bk]], out=pb[bk][:, c0:c0 + 129], lhsT=PT[ps_][:, j * 128:(j + 1) * 128], rhs=V_h[s][:, kt, :],
                               start=(kt == 0 and j % 2 == 0), stop=(j < 2 and kt == 4 * p - 1), skip_group_check=True)
                    elif kind == "po":
                        p = a0
                        for jj in range(2):
                            c0 = jj * 256
                            pe("matmul", [BPT[ps_], Bv[s]], [Bpb[4]], out=pb[4][:, c0:c0 + 129], lhsT=PT[ps_][:, jj * 128:(jj + 1) * 128], rhs=V_h[s][:, kt, :],
                               start=(kt == 0 and jj == 0), stop=(kt == 4 * p + 1), skip_group_check=True)
                    elif kind == "own0":
                        pe("matmul", [BPT[ps_], Bv[s]], [Bpb[5]], out=pb[5][:, 0:129], lhsT=PT[ps_][:, 0:128], rhs=V_h[s][:, kt, :], start=True, stop=True)
                        pe("matmul", [BPT[ps_], Bv[s]], [Bpb[5]], out=pb[5][:, 256:385], lhsT=PT[ps_][:, 128:256], rhs=V_h[s][:, kt, :], start=True, stop=False)
                    else:
                        qb = a0
                        pe("matmul", [BPT[ps_], Bv[s]], [Bpb[5]], out=pb[5][:, 256:385], lhsT=PT[ps_][:, 0:128], rhs=V_h[s][:, kt, :], start=False, stop=True)
                        obk = 3 + (qb % 2)
                        for j in range(2):
                            i = 2 * qb + j
                            if qb > 0:
                                gcol = C_G + h * 2 + j
                                act("activation", [], [Bpb[obk], Bcmb[j]], out=tsc[j], in_=pb[obk][:, j * 256:j * 256 + 129], func=AF.Copy, scale=cst[:, gcol:gcol + 1])
                                dve("tensor_tensor", [], [Bpb[5], Bcmb[j]], out=num[j], in0=tsc[j], in1=pb[5][:, j * 256:j * 256 + 129], op=ALU.add)
                            else:
                                dve("tensor_copy", [], [Bpb[5], Bcmb[j]], out=num[j], in_=pb[5][:, j * 256:j * 256 + 129])
                            dve("reciprocal", [], [Bcmb[j]], out=rden[j], in_=num[j][:, 128:129])
                            dve("scalar_tensor_tensor", [Bcmb[j], Bsg[s]], [Bobf[j]], out=o_bf[j], in0=num[j][:, 0:128], scalar=rden[j][:, 0:1], in1=sg_h[s][:, i, :],
                                op0=ALU.mult, op1=ALU.mult)

                        def back(qb=qb):
                            pbt = pb[7].bitcast(BF16)
                            for j in range(2):
                                pe("transpose", [Bobf[j]], [Bpb[7]], out=pbt[:, 512 + j * 128:512 + (j + 1) * 128], in_=o_bf[j], identity=ident_bf)
                            dve("tensor_copy", [], [Bpb[7], Battn[s]], out=attnT[s][:, qb * 256:(qb + 1) * 256], in_=pbt[:, 512:768])
                        defer(3, back)

                def stB_guard(ix):
                    if ix + 2 < len(items) and items[ix + 2][0] == "pc":
                        return
                    stB(ix)

                pipeline(len(items), [(2, stB_guard), (0, stA)])
                flush()
                dma(f"at{s}", mixT_d[h], attnT[s], reads=[Battn[s]])

            head_load(0)
            head_kmean(0)
            for qb in range(NB):
                gate_front(0, qb)
                gate_back(0, qb)
            for h in range(8):
                if h + 1 < 8:
                    head_load(h + 1)
                    head_kmean(h + 1)
                attn_head(h, h == 0, h == 7)
            S_.barrier()
            if stop_after == "P3":
                break
            AR.release(p3_mark)
            wo_bf = AR.alloc((16, D), BF16)
            Bwo = Buf("wo")
            wos = [AR.alloc((2, D), F32) for _ in range(2)]
            Bwos = [Buf("wos0"), Buf("wos1")]
            wo_l = w_out[l]
            p5_mark = AR.mark()
            for q8 in range(8):
                s = q8 % 2
                dma(f"wos{s}", wos[s], wo_l[q8 * 256:(q8 + 1) * 256, :].rearrange("(k p) n -> p k n", p=128), writes=[Bwos[s]])
                pool("tensor_copy", [Bwos[s]], [Bwo], out=wo_bf[:, q8 * 2:(q8 + 1) * 2, :], in_=wos[s])
            NCH = NT
            hp = pl["hp"]
            xsT_c = [AR.alloc((8, 128), F32) for _ in range(2)]
            bcT_c = [AR.alloc((4, 128), BF16) for _ in range(3)]
            sz_c = [AR.alloc((D,), F32) for _ in range(3)]
            Bxs = [Buf("xsc0"), Buf("xsc1"), Buf("xsc2")]
            Bbc = [Buf("bcc0"), Buf("bcc1"), Buf("bcc2")]
            Bsz = [Buf("szc0"), Buf("szc1"), Buf("szc2")]
            x1a = AR.alloc((NT, 16), F32)
            axa = AR.alloc((NT, 16), F32)
            lga = AR.alloc((NT, 16), F32)
            dta = AR.alloc((NT, 16), F32)
            dAa = AR.alloc((NT, 16), F32)
            acs_a = AR.alloc((NT, 16), F32)
            aend_a = AR.alloc((NT, 16), F32)
            dse_a = AR.alloc((NT, 16), F32)
            nacs_a = AR.alloc((NT, 16), F32)
            ea_a = AR.alloc((NT, 16), F32)
            cd_a = AR.alloc((NT, 16), F32)
            eds_a = AR.alloc((NT, 16), F32)
            dtw_a = AR.alloc((NT, 16), F32)
            Bdt = Buf("dtc")
            Rm = AR.alloc((16, 128), F32)
            BR = Buf("R")
            negb4 = AR.alloc((4, 128), BF16)
            decay = AR.alloc((16, 128), BF16)
            Bdecay = Buf("decay")
            MT = [AR.alloc((16, 128), BF16) for _ in range(2)]
            BMT = [Buf("MT0"), Buf("MT1")]
            cb_sb = AR.alloc((2, 128), BF16)
            Bcb = Buf("cb")
            Btok = [AR.alloc((2, 128), BF16) for _ in range(2)]
            BBtok = [Buf("btok0"), Buf("btok1")]
            xs_sb = AR.alloc((16, 64), F32)
            Bxssb = Buf("xssb")
            xdt = [AR.alloc((16, 64), BF16) for _ in range(2)]
            xw = [AR.alloc((16, 64), BF16) for _ in range(2)]
            xD = [AR.alloc((16, 64), F32) for _ in range(2)]
            Bxdt = [Buf("xdt0"), Buf("xdt1")]
            Bxw = [Buf("xw0"), Buf("xw1")]
            BxD = [Buf("xD0"), Buf("xD1")]
            prev = AR.alloc((16, 64), F32)
            prevbf = AR.alloc((16, 64), BF16)
            Bprev = Buf("prev")
            Bprevbf = Buf("prevbf")
            t1 = AR.alloc((16, 64), F32)
            Bt1 = Buf("t1")
            yg = t1
            Byg = Bt1
            ss2 = AR.alloc((2,), F32)
            Bss2 = Buf("ss2")
            yn2 = [AR.alloc((D,), BF16) for _ in range(2)]
            Byn2 = [Buf("yn0"), Buf("yn1")]
            ystage = [AR.alloc((8, 512), BF16) for _ in range(2)]
            Byst = [Buf("yst0"), Buf("yst1")]
            junk4 = AR.alloc((512,), BF16)
            Bjunk4 = Buf("junk4")
            ones_f = cst[:, C_ONES:C_ONES + 128]
            U_f = cst[:, C_U:C_U + 128]

            pool("memset", [], [Bprev], ap=prev, constant=0.0)
            pool("memset", [], [Bprevbf], ap=prevbf, constant=0.0)
            pool("tensor_copy", [], [BR], out=negb4, in_=cst[:, C_NEG:C_NEG + 128].unsqueeze(1).to_broadcast([128, 4, 128]))

            NW = NT * 16
            fl = lambda t: t.rearrange("p a b -> p (a b)")
            pool("tensor_tensor", [Bdtraw], [Bdt], out=x1a, in0=dtraw, in1=hp[:, 0:16].unsqueeze(1).to_broadcast([128, NT, 16]), op=ALU.add)
            act("activation", [], [Bdt], out=axa, in_=x1a, func=AF.Abs)
            act("activation", [], [Bdt], out=axa, in_=axa, func=AF.Exp, scale=-1.0)
            act("activation", [], [Bdt], out=lga, in_=axa, func=AF.Ln, bias=1.0)
            dve("tensor_scalar", [], [Bdt], out=x1a, in0=x1a, scalar1=0.0, scalar2=None, op0=ALU.max)
            dve("tensor_tensor", [], [Bdt], out=dta, in0=x1a, in1=lga, op=ALU.add)
            dve("tensor_tensor", [], [Bdt], out=dAa, in0=dta, in1=pl["aneg"].unsqueeze(1).to_broadcast([128, NT, 16]), op=ALU.mult)
            pe("matmul", [Bdt], [Bpb[2]], out=pb[2][:, 0:NW], lhsT=U_f, rhs=fl(dAa), start=True, stop=True)
            pe("matmul", [Bdt], [Bpb[3]], out=pb[3][:, 0:NW], lhsT=ones_f, rhs=fl(dAa), start=True, stop=True)
            act("activation", [], [Bpb[2], Bdt], out=fl(acs_a), in_=pb[2][:, 0:NW], func=AF.Copy)
            act("activation", [], [Bpb[3], Bdt], out=fl(aend_a), in_=pb[3][:, 0:NW], func=AF.Copy)
            dve("tensor_tensor", [], [Bdt], out=dse_a, in0=aend_a, in1=acs_a, op=ALU.subtract)
            dve("tensor_scalar", [], [Bdt], out=nacs_a, in0=acs_a, scalar1=-1.0, scalar2=None, op0=ALU.mult)
            act("activation", [], [Bdt], out=ea_a, in_=acs_a, func=AF.Exp)
            act("activation", [], [Bdt], out=cd_a, in_=aend_a, func=AF.Exp)
            act("activation", [], [Bdt], out=eds_a, in_=dse_a, func=AF.Exp)
            dve("tensor_tensor", [], [Bdt], out=dtw_a, in0=dta, in1=eds_a, op=ALU.mult)

            def p4_load(c):
                s = c % 3
                dma(f"xsc{c % 2}", xsT_c[c % 2], xsT_d[:, c * 128:(c + 1) * 128].rearrange("(k p) t -> p k t", p=128), writes=[Bxs[c % 2]])
                dma(f"bcc{s}", bcT_c[s], bcT_d[:, c * 128:(c + 1) * 128].rearrange("(k p) t -> p k t", p=128), writes=[Bbc[s]])
                dma(f"szc{s}", sz_c[s], sz_d[c * 128:(c + 1) * 128, :], writes=[Bsz[s]])

            def stA(c):
                s = c % 2
                s3 = c % 3
                pool("tensor_tensor", [Bdt], [BR], out=Rm, in0=U_f.unsqueeze(1).to_broadcast([128, 16, 128]),
                     in1=dAa[:, c, :].unsqueeze(2).to_broadcast([128, 16, 128]), op=ALU.mult)
                for g in range(2):
                    pe("matmul", [Bbc[s3]], [Bpb[2]], out=pb[2][:, 64 + g * 128:64 + (g + 1) * 128], lhsT=bcT_c[s3][:, g, :], rhs=bcT_c[s3][:, 2 + g, :], start=True, stop=True)
                pbt2 = pb[2].bitcast(BF16)
                for g in range(2):
                    pe("transpose", [Bbc[s3]], [Bpb[2]], out=pbt2[:, 768 + g * 128:768 + (g + 1) * 128], in_=bcT_c[s3][:, g, :], identity=ident_bf)
                act("activation", [], [Bpb[2], Bcb], out=cb_sb, in_=pb[2][:, 64:320].rearrange("p (g l) -> p g l", l=128), func=AF.Copy)
                act("activation", [], [Bpb[2], BBtok[s]], out=Btok[s], in_=pbt2[:, 768:1024].rearrange("p (g l) -> p g l", l=128), func=AF.Copy)
                Rflat = Rm.rearrange("p h l -> p (h l)")
                for qd in range(4):
                    bk = qd % 2
                    pe("matmul", [BR], [Bpb[bk]], out=pb[bk], lhsT=ones_f, rhs=Rflat[:, qd * 512:(qd + 1) * 512], start=True, stop=False)
                    pe("matmul", [BR], [Bpb[bk]], out=pb[bk], lhsT=ident_bf, rhs=negb4.rearrange("p h l -> p (h l)"), start=False, stop=True)
                    for hh in range(4):
                        h = qd * 4 + hh
                        act("activation", [Bdt], [Bpb[bk], Bdecay], out=decay[:, h, :], in_=pb[bk][:, hh * 128:(hh + 1) * 128], func=AF.Exp, bias=nacs_a[:, c, h:h + 1])
                    g = qd // 2
                    dve("tensor_tensor", [Bdecay, Bcb], [BMT[s]], out=MT[s][:, qd * 4:(qd + 1) * 4, :], in0=decay[:, qd * 4:(qd + 1) * 4, :],
                        in1=cb_sb[:, g, :].unsqueeze(1).to_broadcast([128, 4, 128]), op=ALU.mult)
                for g in range(2):
                    for cc in range(4):
                        pe("transpose", [Bxs[s]], [Bpb[3]], out=pb[3][:, cc * 128:(cc + 1) * 128], in_=xsT_c[s][:, g * 4 + cc, :], identity=identf)
                    act("activation", [], [Bpb[3], Bxssb], out=xs_sb[:, g * 8:(g + 1) * 8, :], in_=pb[3].rearrange("p (h d) -> p h d", d=64), func=AF.Copy)
                    hs = slice(g * 8, (g + 1) * 8)
                    dve("tensor_tensor", [Bxssb, Bdt], [Bxdt[s]], out=xdt[s][:, hs, :], in0=xs_sb[:, hs, :],
                        in1=dta[:, c, hs].unsqueeze(2).to_broadcast([128, 8, 64]), op=ALU.mult)
                    pool("tensor_tensor", [Bxssb, Bdt], [Bxw[s]], out=xw[s][:, hs, :], in0=xs_sb[:, hs, :],
                         in1=dtw_a[:, c, hs].unsqueeze(2).to_broadcast([128, 8, 64]), op=ALU.mult)
                    pool("tensor_tensor", [Bxssb], [BxD[s]], out=xD[s][:, hs, :], in0=xs_sb[:, hs, :],
                         in1=hp[:, 32 + g * 8:32 + (g + 1) * 8].unsqueeze(2).to_broadcast([128, 8, 64]), op=ALU.mult)

            def stB1(c):
                s = c % 2
                s3 = c % 3
                for g in range(2):
                    hs = slice(g * 8, (g + 1) * 8)
                    for hh in range(8):
                        h = g * 8 + hh
                        pe("matmul", [BMT[s], Bxdt[s]], [Bpb[4]], out=pb[4][:, hh * 64:(hh + 1) * 64], lhsT=MT[s][:, h, :], rhs=xdt[s][:, h, :], start=True, stop=True)
                    pe("matmul", [Bbc[s3], Bprevbf], [Bpb[5]], out=pb[5], lhsT=bcT_c[s3][:, 2 + g, :], rhs=prevbf[:, hs, :].rearrange("p h d -> p (h d)"), start=True, stop=True)
                    pe("matmul", [BBtok[s], Bxw[s]], [Bpb[6]], out=pb[6], lhsT=Btok[s][:, g, :], rhs=xw[s][:, hs, :].rearrange("p h d -> p (h d)"), start=True, stop=True)
                    dve("tensor_tensor", [Bdt], [Bpb[5], Bt1], out=t1[:, hs, :], in0=pb[5].rearrange("p (h d) -> p h d", d=64),
                        in1=ea_a[:, c, hs].unsqueeze(2).to_broadcast([128, 8, 64]), op=ALU.mult)
                    dve("tensor_tensor", [], [Bpb[4], Bt1], out=t1[:, hs, :], in0=t1[:, hs, :], in1=pb[4].rearrange("p (h d) -> p h d", d=64), op=ALU.add)
                    dve("tensor_tensor", [BxD[s]], [Bt1], out=t1[:, hs, :], in0=t1[:, hs, :], in1=xD[s][:, hs, :], op=ALU.add)
                    pool("tensor_tensor", [Bt1, Bsz[s3]], [Byg], out=yg[:, hs, :], in0=t1[:, hs, :], in1=sz_c[s3][:, g * 512:(g + 1) * 512].rearrange("p (h d) -> p h d", d=64), op=ALU.mult)
                    act("activation", [Byg], [Bjunk4, Bss2], out=junk4, in_=yg[:, hs, :].rearrange("p h d -> p (h d)"), func=AF.Square, accum_out=ss2[:, g:g + 1])
                    dve("tensor_tensor", [Bdt], [Bprev], out=prev[:, hs, :], in0=prev[:, hs, :],
                        in1=cd_a[:, c, hs].unsqueeze(2).to_broadcast([128, 8, 64]), op=ALU.mult)
                    dve("tensor_tensor", [], [Bpb[6], Bprev], out=prev[:, hs, :], in0=prev[:, hs, :], in1=pb[6].rearrange("p (h d) -> p h d", d=64), op=ALU.add)
                    act("activation", [Bprev], [Bprevbf], out=prevbf[:, hs, :], in_=prev[:, hs, :], func=AF.Copy)
                pool("tensor_scalar", [], [Bss2], out=ss2, in0=ss2, scalar1=1.0 / 512, scalar2=EPS, op0=ALU.mult, op1=ALU.add)
                pool("tensor_tensor", [], [Bss2], out=ss2, in0=ss2, in1=mhalf[:, 0:2], op=ALU.pow)
                for g in range(2):
                    act("activation", [Byg, Bss2], [Byn2[s]], out=yn2[s][:, g * 512:(g + 1) * 512], in_=yg[:, g * 8:(g + 1) * 8, :].rearrange("p h d -> p (h d)"),
                        func=AF.Copy, scale=ss2[:, g:g + 1])

            def stB2(c):
                s = c % 2
                pbt7 = pb[7].bitcast(BF16)
                for cc in range(8):
                    pe("transpose", [Byn2[s]], [Bpb[7]], out=pbt7[:, cc * 128:(cc + 1) * 128], in_=yn2[s][:, cc * 128:(cc + 1) * 128], identity=ident_bf)
                ys = (c // 4) % 2
                j = c % 4
                dve("tensor_tensor", [], [Bpb[7], Byst[ys]], out=ystage[ys][:, :, j * 128:(j + 1) * 128], in0=pbt7.rearrange("p (c t) -> p c t", t=128),
                    in1=pl["snwT"].unsqueeze(2).to_broadcast([128, 8, 128]), op=ALU.mult)
                if j == 3:
                    tg = c // 4
                    dma(f"yst{ys}", mixT_d[8:16, :, tg * 512:(tg + 1) * 512].rearrange("h p t -> p h t"), ystage[ys], reads=[Byst[ys]])

            p4_load(0)
            if NCH > 1:
                p4_load(1)
            for t in range(NCH + 2):
                if t < NCH:
                    stA(t)
                if 1 <= t <= NCH:
                    stB1(t - 1)
                if t + 2 < NCH:
                    p4_load(t + 2)
                if t >= 2:
                    stB2(t - 2)
            S_.barrier()
            if stop_after == "P4":
                break

            AR.release(p5_mark)
            mixs = [AR.alloc((16, 512), BF16) for _ in range(2)]
            Bmix = [Buf("mix0"), Buf("mix1")]
            xr = [AR.alloc((D,), F32) for _ in range(2)]
            Bxr = [Buf("xr0"), Buf("xr1")]
            ot = [AR.alloc((D,), F32) for _ in range(2)]
            Bot = [Buf("ot0"), Buf("ot1")]

            def p5_loadmix(tg):
                s = tg % 2
                dma(f"mix{s}", mixs[s], mixT_d[:, :, tg * 512:(tg + 1) * 512].rearrange("c p t -> p c t"), writes=[Bmix[s]])

            def p5_loadx(i):
                s = i % 2
                dma(f"xr{s}", xr[s], xsrc[i * 128:(i + 1) * 128, :], writes=[Bxr[s]])

            def p5_tile(i):
                s = i % 2
                tg = i // 4
                j = i % 4
                ms = tg % 2
                for half in range(2):
                    bk = (2 * i + half) % 4
                    for cch in range(16):
                        pe("matmul", [Bmix[ms], Bwo], [Bpb[bk]], out=pb[bk], lhsT=mixs[ms][:, cch, j * 128:(j + 1) * 128], rhs=wo_bf[:, cch, half * 512:(half + 1) * 512],
                           start=(cch == 0), stop=(cch == 15))
                    dve("tensor_tensor", [Bxr[s]], [Bpb[bk], Bot[s]], out=ot[s][:, half * 512:(half + 1) * 512], in0=pb[bk], in1=xr[s][:, half * 512:(half + 1) * 512], op=ALU.add)
                dma(f"ot{s}", xdst[i * 128:(i + 1) * 128, :], ot[s], reads=[Bot[s]])

            p5_loadmix(0)
            p5_loadx(0)
            for i in range(NT):
                if i % 4 == 0 and i // 4 + 1 < TG:
                    p5_loadmix(i // 4 + 1)
                if i + 1 < NT:
                    p5_loadx(i + 1)
                p5_tile(i)
            S_.barrier()
            if stop_after == "L0":
                break

        S_.barrier()
        S_.finalize()
        with nc.Block() as block:
            @block.sync
            def _(e):
                S_.replay("sp")

            @block.tensor
            def _(e):
                S_.replay("pe")

            @block.vector
            def _(e):
                S_.replay("dve")

            @block.scalar
            def _(e):
                S_.replay("act")

            @block.gpsimd
            def _(e):
                S_.replay("pool")
    return nc


def host_inputs(inputs, b, S, depth=2):
    f = np.float32
    d = {}
    d["x"] = np.ascontiguousarray(inputs["x"][b, :S]).astype(f)
    d["w_in"] = np.ascontiguousarray(inputs["w_in"]).astype(f)
    d["w_out"] = np.ascontiguousarray(inputs["w_out"]).astype(f)
    d["lnwT"] = np.ascontiguousarray(inputs["ln_w"].reshape(depth, 8, 128).transpose(0, 2, 1)).astype(f)
    cw = inputs["conv_w"].reshape(depth, 4, 12, 128).transpose(0, 3, 2, 1)
    d["cwT"] = np.ascontiguousarray(cw.reshape(depth, 128, 48)).astype(f)
    d["cbT"] = np.ascontiguousarray(inputs["conv_b"].reshape(depth, 12, 128).transpose(0, 2, 1)).astype(f)
    d["qkw"] = np.ascontiguousarray(np.stack([inputs["q_norm_w"], inputs["k_norm_w"]], axis=-1)).astype(f)
    d["snwT"] = np.ascontiguousarray(inputs["ssd_norm_w"].reshape(depth, 8, 128).transpose(0, 2, 1)).astype(f)
    d["hp"] = np.ascontiguousarray(np.concatenate([inputs["dt_bias"], inputs["a_log"], inputs["d_skip"]], axis=-1).reshape(depth, 1, 48)).astype(f)
    d["consts"] = make_consts()
    return d


BATCH = 8
SEQ = 4096


def kernel(**inputs):
    inputs = {k: np.asarray(v) for k, v in inputs.items()}
    nc = build(SEQ, depth=2, dbg=False)
    in_maps = [host_inputs(inputs, b, SEQ) for b in range(BATCH)]
    res = run_bass_kernel_spmd(nc, in_maps, core_ids=list(range(BATCH)))
    out = np.stack([np.asarray(r["out"], dtype=np.float32) for r in res.results], axis=0)
    return out
```
